# Optimizing a Trainium2 kernel written in Bass

```python
import math
import jax
import jax.numpy as jnp
from jax import lax
import numpy as np

D_MODEL = 1024
BATCH = 16
SEQ = 2048
DEPTH = 4

N_MIXERS = 2
N_ATTN_LAYERS = (DEPTH + 1) // 2
N_RET_LAYERS = DEPTH // 2

DA_HEAD_DIM = 64
DA_HEADS = D_MODEL // (2 * DA_HEAD_DIM)
DA_VALUE_DIM = 2 * DA_HEAD_DIM
ROT_DIM = DA_HEAD_DIM // 4
ROPE_THETA = 500000.0
Q_BLOCK = 128

RET_KEY_DIM = 256
RET_HEADS = D_MODEL // RET_KEY_DIM
RET_VALUE_DIM = 2 * RET_KEY_DIM
RET_CHUNK = 128
RET_THETA = 10000.0

FFN_DIM = 256 * ((8 * D_MODEL // 3 + 255) // 256)
CONV_WIDTH = 3
NORM_EPS = 1e-6

kernel_name = "hybrid_diffattn_retention_convffn"


def rms_norm(x, g):
    xf = x.astype(jnp.float32)
    y = xf * lax.rsqrt(jnp.mean(xf * xf, axis=-1, keepdims=True) + NORM_EPS)
    return (y * g.astype(jnp.float32)).astype(x.dtype)


def head_rms_norm(x):
    xf = x.astype(jnp.float32)
    return (xf * lax.rsqrt(jnp.mean(xf * xf, axis=-1, keepdims=True) + NORM_EPS)).astype(x.dtype)


def partial_rope(x, cos, sin):
    half = ROT_DIM // 2
    x1 = x[..., :half]
    x2 = x[..., half:ROT_DIM]
    return jnp.concatenate([x1 * cos - x2 * sin, x2 * cos + x1 * sin, x[..., ROT_DIM:]], axis=-1)


def diff_attention(h, positions, w_qkv, w_o, lq1, lk1, lq2, lk2, subln_g, lambda_init):
    B, S, _ = h.shape
    q, k, v = jnp.split(h @ w_qkv, 3, axis=-1)
    q = q.reshape(B, S, DA_HEADS, 2, DA_HEAD_DIM)
    k = k.reshape(B, S, DA_HEADS, 2, DA_HEAD_DIM)
    v = v.reshape(B, S, DA_HEADS, DA_VALUE_DIM)

    inv_freq = ROPE_THETA ** (-jnp.arange(0, ROT_DIM, 2, dtype=jnp.float32) / ROT_DIM)
    ang = positions.astype(jnp.float32)[..., None] * inv_freq
    cos = jnp.cos(ang)[:, :, None, None, :].astype(h.dtype)
    sin = jnp.sin(ang)[:, :, None, None, :].astype(h.dtype)
    q = partial_rope(q, cos, sin) * (DA_HEAD_DIM ** -0.5)
    k = partial_rope(k, cos, sin)

    lam = (jnp.exp(jnp.sum(lq1.astype(jnp.float32) * lk1.astype(jnp.float32)))
           - jnp.exp(jnp.sum(lq2.astype(jnp.float32) * lk2.astype(jnp.float32)))
           + lambda_init)

    nb = S // Q_BLOCK
    q_blocks = q.reshape(B, nb, Q_BLOCK, DA_HEADS, 2, DA_HEAD_DIM).transpose(1, 0, 2, 3, 4, 5)
    starts = jnp.arange(nb, dtype=jnp.int32) * Q_BLOCK
    key_idx = jnp.arange(S, dtype=jnp.int32)
    neg = jnp.finfo(jnp.float32).min

    def block(args):
        qb, start = args
        s = jnp.einsum('bqhcd,bkhcd->bhcqk', qb, k).astype(jnp.float32)
        causal = key_idx[None, :] <= (start + jnp.arange(Q_BLOCK, dtype=jnp.int32))[:, None]
        s = jnp.where(causal, s, neg)
        p = jax.nn.softmax(s, axis=-1)
        a = p[:, :, 0] - lam * p[:, :, 1]
        return jnp.einsum('bhqk,bkhe->bqhe', a.astype(v.dtype), v)

    o = lax.map(block, (q_blocks, starts))
    o = o.transpose(1, 0, 2, 3, 4).reshape(B, S, DA_HEADS, DA_VALUE_DIM)
    o = rms_norm(o, subln_g) * (1.0 - lambda_init)
    return o.reshape(B, S, DA_HEADS * DA_VALUE_DIM) @ w_o


def retention(h, positions, w_qkvg, w_o):
    B, S, _ = h.shape
    dk_all = RET_HEADS * RET_KEY_DIM
    dv_all = RET_HEADS * RET_VALUE_DIM
    q, k, v, g = jnp.split(h @ w_qkvg, [dk_all, 2 * dk_all, 2 * dk_all + dv_all], axis=-1)
    q = q.reshape(B, S, RET_HEADS, RET_KEY_DIM)
    k = k.reshape(B, S, RET_HEADS, RET_KEY_DIM)
    v = v.reshape(B, S, RET_HEADS, RET_VALUE_DIM)

    angle = 1.0 / (RET_THETA ** jnp.linspace(0.0, 1.0, RET_KEY_DIM // 2, dtype=jnp.float32))
    ang = positions.astype(jnp.float32)[..., None] * angle
    cos = jnp.cos(ang)[:, :, None, :].astype(h.dtype)
    sin = jnp.sin(ang)[:, :, None, :].astype(h.dtype)

    def rotate(t):
        te = t[..., 0::2]
        to = t[..., 1::2]
        return jnp.stack([te * cos - to * sin, to * cos + te * sin], axis=-1).reshape(t.shape)

    q = rotate(q)
    k = rotate(k) * (RET_KEY_DIM ** -0.5)

    log_gamma = jnp.log1p(-(2.0 ** (-5.0 - jnp.arange(RET_HEADS, dtype=jnp.float32))))
    C = RET_CHUNK
    idx = jnp.arange(C, dtype=jnp.float32)
    rel = idx[:, None] - idx[None, :]
    inner_decay = jnp.where(rel[None] >= 0,
                            jnp.exp(jnp.maximum(rel, 0.0)[None] * log_gamma[:, None, None]), 0.0)
    q_decay = jnp.exp((idx + 1.0)[:, None] * log_gamma[None, :])
    k_decay = jnp.exp((C - 1.0 - idx)[:, None] * log_gamma[None, :])
    chunk_decay = jnp.exp(C * log_gamma)

    nc = S // C
    qc = q.reshape(B, nc, C, RET_HEADS, RET_KEY_DIM).transpose(1, 0, 2, 3, 4)
    kc = k.reshape(B, nc, C, RET_HEADS, RET_KEY_DIM).transpose(1, 0, 2, 3, 4)
    vc = v.reshape(B, nc, C, RET_HEADS, RET_VALUE_DIM).transpose(1, 0, 2, 3, 4)

    def step(state, xs):
        qb, kb, vb = xs
        inner = jnp.einsum('bihd,bjhd->bhij', qb, kb) * inner_decay
        o_inner = jnp.einsum('bhij,bjhe->bihe', inner, vb)
        o_cross = jnp.einsum('bihd,bhde->bihe', qb * q_decay[..., None], state)
        state = (state * chunk_decay[None, :, None, None]
                 + jnp.einsum('bjhd,bjhe->bhde', kb * k_decay[..., None], vb))
        return state, o_inner + o_cross

    state0 = jnp.zeros((B, RET_HEADS, RET_KEY_DIM, RET_VALUE_DIM), jnp.float32)
    _, o = lax.scan(step, state0, (qc, kc, vc))
    o = o.transpose(1, 0, 2, 3, 4).reshape(B, S, RET_HEADS, RET_VALUE_DIM)
    o = head_rms_norm(o).reshape(B, S, dv_all)
    return (jax.nn.silu(g) * o) @ w_o


def conv_ffn(h, w_in, conv_w, conv_b, w_out):
    S = h.shape[1]
    gate, up = jnp.split(h @ w_in, 2, axis=-1)
    gp = jnp.pad(gate, ((0, 0), (CONV_WIDTH - 1, 0), (0, 0)))
    gate = conv_b + sum(gp[:, j:j + S] * conv_w[j] for j in range(CONV_WIDTH))
    return (jax.nn.silu(gate) * up) @ w_out


def setup_inputs(seed: int = 0) -> dict:
    key = jax.random.key(seed)
    ks = jax.random.split(key, 20)
    f32 = jnp.float32
    D = D_MODEL
    nrm = lambda k, shape, scale: jax.random.normal(k, shape, f32) * scale
    x = jax.random.normal(ks[0], (BATCH, SEQ, D), f32)
    offsets = jax.random.randint(ks[1], (BATCH, 1), 0, 4096, dtype=jnp.int32)
    positions = offsets + jnp.arange(SEQ, dtype=jnp.int32)[None, :]
    return {
        "x": x,
        "positions": positions,
        "norm_mix_g": 1.0 + nrm(ks[2], (DEPTH, D), 0.02),
        "norm_ffn_g": 1.0 + nrm(ks[3], (DEPTH, D), 0.02),
        "final_norm_g": 1.0 + nrm(ks[4], (D,), 0.02),
        "attn_w_qkv": nrm(ks[5], (N_ATTN_LAYERS, D, 3 * D), D ** -0.5),
        "attn_w_o": nrm(ks[6], (N_ATTN_LAYERS, DA_HEADS * DA_VALUE_DIM, D), (DA_HEADS * DA_VALUE_DIM) ** -0.5),
        "attn_lambda_q1": nrm(ks[7], (N_ATTN_LAYERS, DA_HEAD_DIM), 0.1),
        "attn_lambda_k1": nrm(ks[8], (N_ATTN_LAYERS, DA_HEAD_DIM), 0.1),
        "attn_lambda_q2": nrm(ks[9], (N_ATTN_LAYERS, DA_HEAD_DIM), 0.1),
        "attn_lambda_k2": nrm(ks[10], (N_ATTN_LAYERS, DA_HEAD_DIM), 0.1),
        "attn_subln_g": 1.0 + nrm(ks[11], (N_ATTN_LAYERS, DA_VALUE_DIM), 0.02),
        "ret_w_qkvg": nrm(ks[12], (N_RET_LAYERS, D, 2 * RET_HEADS * RET_KEY_DIM + 2 * RET_HEADS * RET_VALUE_DIM), D ** -0.5),
        "ret_w_o": nrm(ks[13], (N_RET_LAYERS, RET_HEADS * RET_VALUE_DIM, D), (RET_HEADS * RET_VALUE_DIM) ** -0.5),
        "ffn_w_in": nrm(ks[14], (DEPTH, D, 2 * FFN_DIM), D ** -0.5),
        "ffn_conv_w": nrm(ks[15], (DEPTH, CONV_WIDTH, FFN_DIM), CONV_WIDTH ** -0.5),
        "ffn_conv_b": nrm(ks[16], (DEPTH, FFN_DIM), 0.01),
        "ffn_w_out": nrm(ks[17], (DEPTH, FFN_DIM, D), FFN_DIM ** -0.5),
    }


def reference(x, positions, norm_mix_g, norm_ffn_g, final_norm_g, attn_w_qkv, attn_w_o,
              attn_lambda_q1, attn_lambda_k1, attn_lambda_q2, attn_lambda_k2, attn_subln_g,
              ret_w_qkvg, ret_w_o, ffn_w_in, ffn_conv_w, ffn_conv_b, ffn_w_out):
    for i in range(DEPTH):
        h = rms_norm(x, norm_mix_g[i])
        j = i // N_MIXERS
        if i % N_MIXERS == 0:
            lambda_init = 0.8 - 0.6 * math.exp(-0.3 * i)
            x = x + diff_attention(h, positions, attn_w_qkv[j], attn_w_o[j],
                                   attn_lambda_q1[j], attn_lambda_k1[j],
                                   attn_lambda_q2[j], attn_lambda_k2[j],
                                   attn_subln_g[j], lambda_init)
        else:
            x = x + retention(h, positions, ret_w_qkvg[j], ret_w_o[j])
        x = x + conv_ffn(rms_norm(x, norm_ffn_g[i]), ffn_w_in[i], ffn_conv_w[i], ffn_conv_b[i], ffn_w_out[i])
    return rms_norm(x, final_norm_g)
```

```python
import math
import numpy as np
import ml_dtypes
import concourse.bass as bass
import concourse.mybir as mybir
from concourse.bass_utils import run_bass_kernel_spmd

dt = mybir.dt
F32, BF16, I32 = dt.float32, dt.bfloat16, dt.int32
AF = mybir.ActivationFunctionType
ALU = mybir.AluOpType
COMPUTE = ("pe", "act", "dve", "pool")

D = 1024
S = 2048
NT = 16
NSEQ = 2
DEPTH = 4
FFN = 2816
NFC = 22
EPS = 1e-6
N_CORES = 8
PI = math.pi


class Buf:
    __slots__ = ("name", "w", "r")

    def __init__(self, name):
        self.name = name
        self.w = {}
        self.r = {}


class Op:
    __slots__ = ("eng", "fn", "deps", "sig", "val", "key", "is_dma", "order")

    def __init__(self, eng, fn, is_dma=False, key=None):
        self.eng = eng
        self.fn = fn
        self.deps = {}
        self.sig = False
        self.val = 0
        self.key = key if key is not None else eng
        self.is_dma = is_dma


class Prog:
    def __init__(self):
        self.streams = {e: [] for e in ("pe", "act", "dve", "pool", "sp")}
        self.dma_cnt = {}
        self.all_ops = []

    def op(self, eng, fn, R=(), W=(), WA=()):
        o = Op(eng, fn)
        self._track(o, R, W, WA)
        return o

    def dma(self, eng, fn, key, R=(), W=(), WA=()):
        o = Op(eng, fn, is_dma=True, key="dma:" + key)
        self.dma_cnt[key] = self.dma_cnt.get(key, 0) + 1
        o.val = 16 * self.dma_cnt[key]
        o.sig = True
        self._track(o, R, W, WA)
        return o

    def _track(self, o, R, W, WA):
        o.order = len(self.all_ops)
        self.all_ops.append(o)
        self.streams[o.eng].append(o)
        for b in R:
            for s in b.w.values():
                self._add(o, s, True)
        for b in list(W) + list(WA):
            for s in b.w.values():
                self._add(o, s, False)
            for s in b.r.values():
                self._add(o, s, False)
        for b in R:
            b.r[o.key] = o
        for b in W:
            b.w = {o.key: o}
            b.r = {}
        for b in WA:
            b.w[o.key] = o

    def _add(self, o, s, raw):
        if s is o:
            return
        if (not s.is_dma) and (not o.is_dma) and s.eng == o.eng:
            if not raw or s.eng == "pe":
                return
        cur = o.deps.get(s.key)
        if cur is None or cur.order < s.order:
            o.deps[s.key] = s

    def finalize(self):
        for o in self.all_ops:
            for s in o.deps.values():
                s.sig = True
        cnt = {e: 0 for e in COMPUTE}
        for o in self.all_ops:
            if not o.is_dma and o.sig:
                cnt[o.eng] += 1
                o.val = cnt[o.eng]

    def replay(self, name, eng, sems, dma_sems):
        waited = {}
        for o in self.streams[name]:
            for k, s in o.deps.items():
                if waited.get(k, 0) < s.val:
                    sem = dma_sems[k[4:]] if s.is_dma else sems[s.eng]
                    eng.wait_ge(sem, s.val)
                    waited[k] = s.val
            if o.fn is None:
                continue
            ins = o.fn(eng)
            if o.is_dma:
                ins.then_inc(dma_sems[o.key[4:]], 16)
            elif o.sig:
                ins.then_inc(sems[o.eng], 1)


class Builder:
    def __init__(self, layers, nseq, final_norm):
        self.layers = layers
        self.nseq = nseq
        self.final_norm = final_norm
        self.P = Prog()
        self.nc = bass.Bass("TRN2", target_bir_lowering=False)
        self.bufs = {}

    def B(self, name):
        b = self.bufs.get(name)
        if b is None:
            b = self.bufs[name] = Buf(name)
        return b

    def Bs(self, names):
        return [self.B(n) for n in names]

    def xb(self, *aps):
        out = []
        for a in aps:
            try:
                if a.tensor.name != "psum":
                    continue
            except AttributeError:
                continue
            es = 4 if a.dtype in (F32, I32) else 2
            b = (a.offset * es) // 2048
            bb = self.B(f"xbank{b}")
            if bb not in out:
                out.append(bb)
        return out

    def dram_in(self, name, shape, d=F32):
        return self.nc.dram_tensor(name, list(shape), d, kind="ExternalInput").ap()

    def MM(self, out, lhsT, rhs, start, stop, R, W=(), WA=()):
        self.P.op("pe", lambda e: e.matmul(out, lhsT=lhsT, rhs=rhs, start=start, stop=stop,
                                           skip_group_check=True), R=R, W=list(W) + self.xb(out), WA=WA)

    def TR(self, out, in_, R, W=(), WA=()):
        ident = self.ident
        self.P.op("pe", lambda e: e.transpose(out=out, in_=in_, identity=ident), R=list(R) + [self.B("consts")], W=list(W) + self.xb(out), WA=WA)

    def ACT(self, out, in_, func, R, W=(), WA=(), scale=None, bias=None, accum=None):
        kw = {}
        if scale is not None:
            kw["scale"] = scale
        if bias is not None:
            kw["bias"] = bias
        if accum is not None:
            kw["accum_out"] = accum
        self.P.op("act", lambda e: e.activation(out=out, in_=in_, func=func, **kw), R=R, W=list(W) + self.xb(out, in_), WA=WA)

    def TT(self, eng, out, in0, in1, op, R, W=(), WA=()):
        self.P.op(eng, lambda e: e.tensor_tensor(out=out, in0=in0, in1=in1, op=op), R=R, W=list(W) + self.xb(out, in0, in1), WA=WA)

    def TS(self, eng, out, in0, s1, s2, op0, op1, R, W=(), WA=()):
        if op1 is None:
            self.P.op(eng, lambda e: e.tensor_scalar(out=out, in0=in0, scalar1=s1, scalar2=None, op0=op0), R=R, W=list(W) + self.xb(out, in0), WA=WA)
        else:
            self.P.op(eng, lambda e: e.tensor_scalar(out=out, in0=in0, scalar1=s1, scalar2=s2, op0=op0, op1=op1), R=R, W=list(W) + self.xb(out, in0), WA=WA)

    def STT(self, out, in0, scalar, in1, op0, op1, R, W=(), WA=(), accum=None):
        if accum is None:
            self.P.op("dve", lambda e: e.scalar_tensor_tensor(out=out, in0=in0, scalar=scalar, in1=in1, op0=op0, op1=op1), R=R, W=list(W) + self.xb(out, in0, in1), WA=WA)
        else:
            self.P.op("dve", lambda e: e.scalar_tensor_tensor(out=out, in0=in0, scalar=scalar, in1=in1, op0=op0, op1=op1, accum_out=accum), R=R, W=list(W) + self.xb(out, in0, in1), WA=WA)

    def CP(self, eng, out, in_, R, W=(), WA=()):
        if eng == "act":
            self.P.op("act", lambda e: e.copy(out=out, in_=in_), R=R, W=list(W) + self.xb(out, in_), WA=WA)
        else:
            self.P.op(eng, lambda e: e.tensor_copy(out=out, in_=in_), R=R, W=list(W) + self.xb(out, in_), WA=WA)

    def MEMSET(self, eng, ap, val, W=(), WA=()):
        self.P.op(eng, lambda e: e.memset(ap, val), W=W, WA=WA)

    def DMA(self, eng, out, in_, key, R=(), W=(), WA=()):
        self.P.dma(eng, lambda e: e.dma_start(out=out, in_=in_), key, R=R, W=W, WA=WA)

    def barrier(self):
        allb = list(self.bufs.values())
        P = self.P
        for e in ("pe", "act", "dve", "pool", "sp"):
            o = Op(e, None)
            o.order = len(P.all_ops)
            for b in allb:
                for src in list(b.w.values()) + list(b.r.values()):
                    if src.fn is None:
                        continue
                    if (not src.is_dma) and src.eng == e:
                        continue
                    cur = o.deps.get(src.key)
                    if cur is None or cur.order < src.order:
                        o.deps[src.key] = src
            P.all_ops.append(o)
            P.streams[e].append(o)

    def arena_reset(self):
        self.aoff = 0

    def carve(self, shape, d):
        n = 1
        for v in shape:
            n *= v
        nbytes = n * (4 if d in (F32, I32) else 2)
        nbytes = (nbytes + 31) // 32 * 32
        o2 = self.aoff // 2
        assert self.aoff + nbytes <= self.arena_bytes, (self.aoff, nbytes, self.arena_bytes)
        v = self.arena[:, o2:o2 + nbytes // 2]
        self.aoff += nbytes
        if d != BF16:
            v = v.bitcast(d)
        v = v[:, 0:n]
        if len(shape) == 2:
            v = v.rearrange("p (a b) -> p a b", b=shape[1])
        elif len(shape) == 3:
            v = v.rearrange("p (a b c) -> p a b c", b=shape[1], c=shape[2])
        return v

    def pbank(self, i, d=F32):
        v = self.psum[:, i * 512:(i + 1) * 512]
        if d == BF16:
            v = v.bitcast(BF16)
        return v

    def build(self):
        nc = self.nc
        ns = self.nseq
        self.x_d = self.dram_in("x", [ns, S, D])
        self.pos_d = self.dram_in("pos", [ns, 128, NT], I32)
        self.nmg_d = self.dram_in("nmg", [128, DEPTH * 8])
        self.nfg_d = self.dram_in("nfg", [128, DEPTH * 8])
        self.fng_d = self.dram_in("fng", [128, D])
        self.lam_d = self.dram_in("lam_in", [128, 2 * 256])
        self.subln_d = self.dram_in("subln", [128, 2])
        self.cw_d = self.dram_in("f_cw", [128, DEPTH * NFC * 3])
        self.cb_d = self.dram_in("f_cb", [128, DEPTH * NFC])
        ents = [e if isinstance(e, tuple) else (e, True, True) for e in self.layers]
        self.attn_js = sorted({li // 2 for li, m, f in ents if m and li % 2 == 0})
        self.ret_js = sorted({li // 2 for li, m, f in ents if m and li % 2 == 1})
        self.ffn_ls = sorted({li for li, m, f in ents if f})
        if self.attn_js:
            self.a_wqkv = self.dram_in("a_wqkv", [len(self.attn_js), D, 3 * D])
            self.a_wo = self.dram_in("a_wo", [len(self.attn_js), D, D])
        if self.ret_js:
            self.r_wqkvg = self.dram_in("r_wqkvg", [len(self.ret_js), D, 6144])
            self.r_wo = self.dram_in("r_wo", [len(self.ret_js), 2048, D])
        if self.ffn_ls:
            self.f_win = self.dram_in("f_win", [len(self.ffn_ls), D, 2 * FFN])
            self.f_wout = self.dram_in("f_wout", [len(self.ffn_ls), FFN, D])
        self.cbf_d = self.dram_in("c_bf", [128, 256], BF16)
        self.cf_d = self.dram_in("c_f32", [128, 8 + 128 + 512 + 16])
        self.out_d = nc.dram_tensor("out", [ns, S, D], F32, kind="ExternalOutput").ap()

        self.X = nc.alloc_sbuf_tensor("X", [128, NT, D], F32)[:]
        self.HT = nc.alloc_sbuf_tensor("HT", [128, 8, S], BF16)[:]
        self.cbf = nc.alloc_sbuf_tensor("cbf", [128, 256], BF16)[:]
        self.cf = nc.alloc_sbuf_tensor("cf", [128, 664], F32)[:]
        self.ident = self.cbf[:, 0:128]
        self.cmask = self.cbf[:, 128:256]
        self.afreq = self.cf[:, 0:8]
        self.rfreq = self.cf[:, 8:136]
        self.rmask = self.cf[:, 136:648].rearrange("p (h i) -> p h i", i=128)
        self.rcol = self.cf[:, 648:664]
        self.params = nc.alloc_sbuf_tensor("params", [128, 32 + 32 + 512 + 2 + 264 + 88], F32)[:]
        o = 0
        self.nmg = self.params[:, o:o + 32]; o += 32
        self.nfg = self.params[:, o:o + 32]; o += 32
        self.lamin = self.params[:, o:o + 512]; o += 512
        self.subln = self.params[:, o:o + 2]; o += 2
        self.cw = self.params[:, o:o + 264]; o += 264
        self.cb = self.params[:, o:o + 88]; o += 88
        self.rcs = nc.alloc_sbuf_tensor("rcs", [128, 2, NT, 128], F32)[:]
        self.acs = nc.alloc_sbuf_tensor("acs", [128, 2, NT, 8], F32)[:]
        self.posf = nc.alloc_sbuf_tensor("posf", [128, NT], F32)[:]
        self.posi = nc.alloc_sbuf_tensor("posi", [128, NT], I32)[:]
        self.small = nc.alloc_sbuf_tensor("small", [128, 64], F32)[:]
        self.arena_bytes = (nc.sbuf_bytes_remaining - 256) // 64 * 64
        self.arena = nc.alloc_sbuf_tensor("arena", [128, self.arena_bytes // 2], BF16)[:]
        self.psum = nc.alloc_psum_tensor("psum", [128, 4096], F32)[:]

        cB = self.B("consts")
        self.DMA("sp", self.cbf, self.cbf_d, "consts", W=[cB])
        self.DMA("sp", self.cf, self.cf_d, "consts", WA=[cB])
        self.DMA("sp", self.nmg, self.nmg_d, "consts", WA=[cB])
        self.DMA("sp", self.nfg, self.nfg_d, "consts", WA=[cB])
        self.DMA("sp", self.lamin, self.lam_d, "consts", WA=[cB])
        self.DMA("sp", self.subln, self.subln_d, "consts", WA=[cB])
        self.DMA("sp", self.cw, self.cw_d, "consts", WA=[cB])
        self.DMA("sp", self.cb, self.cb_d, "consts", WA=[cB])
        self.MEMSET("dve", self.small[:, 0:1], EPS, WA=[cB])

        for s in range(ns):
            self.emit_seq(s)
        self.P.op("sp", None, R=[self.B("out")])
        self.P.finalize()

        from contextlib import ExitStack
        with ExitStack() as es:
            sems = {e: es.enter_context(nc.semaphore("s_" + e)) for e in COMPUTE}
            dsems = {k: es.enter_context(nc.semaphore("d_" + k)) for k in self.P.dma_cnt}
            P = self.P
            with nc.Block() as block:
                @block.tensor
                def _(e):
                    P.replay("pe", e, sems, dsems)

                @block.scalar
                def _(e):
                    P.replay("act", e, sems, dsems)

                @block.vector
                def _(e):
                    P.replay("dve", e, sems, dsems)

                @block.gpsimd
                def _(e):
                    P.replay("pool", e, sems, dsems)

                @block.sync
                def _(e):
                    P.replay("sp", e, sems, dsems)
        return nc

    def emit_seq(self, s):
        BX = [self.B(f"X{t}") for t in range(NT)]
        self.barrier()
        for t in range(NT):
            self.DMA("sp", self.X[:, t, :], self.x_d[s, t * 128:(t + 1) * 128, :], f"X{t}", W=[BX[t]])
        self.DMA("sp", self.posi, self.pos_d[s], "pos", W=[self.B("posi")])
        self.CP("dve", self.posf, self.posi, R=[self.B("posi")], W=[self.B("posf")])
        self.arena_reset()
        self.emit_sincos(self.afreq, 8, self.acs, "acs")
        self.arena_reset()
        self.emit_sincos(self.rfreq, 128, self.rcs, "rcs")
        for ent in self.layers:
            li, do_mix, do_ffn = ent if isinstance(ent, tuple) else (ent, True, True)
            if do_mix:
                self.barrier()
                self.arena_reset()
                if li % 2 == 0:
                    self.emit_attn(li)
                else:
                    self.emit_ret(li)
            if do_ffn:
                self.barrier()
                self.arena_reset()
                self.emit_ffn(li)
        self.barrier()
        self.arena_reset()
        self.emit_out(s)

    def emit_sincos(self, freq, F, dst, name):
        cB = self.B("consts")
        Bt = self.B("sc_tmp")
        Bd = self.B(name)
        ang = self.carve([NT, F], F32)
        ki = self.carve([NT, F], I32)
        kf = self.carve([NT, F], F32)
        y = self.carve([NT, F], F32)
        fb = freq.unsqueeze(1).to_broadcast([128, NT, F])
        pb = self.posf.unsqueeze(2).to_broadcast([128, NT, F])
        self.TT("dve", ang, fb, pb, ALU.mult, R=[cB, self.B("posf")], W=[Bt])
        self.TS("dve", ki, ang, 1.0 / (2 * PI), None, ALU.mult, None, R=[Bt], WA=[Bt])
        self.CP("dve", kf, ki, R=[Bt], WA=[Bt])
        self.STT(ang, kf, -2 * PI, ang, ALU.mult, ALU.add, R=[Bt], WA=[Bt])
        for idx, shift in ((1, 0.0), (0, PI / 2)):
            self.TS("dve", y, ang, shift, None, ALU.add, None, R=[Bt], WA=[Bt])
            self.TS("dve", kf, y, PI, -2 * PI, ALU.is_gt, ALU.mult, R=[Bt], WA=[Bt])
            self.TT("dve", y, y, kf, ALU.add, R=[Bt], WA=[Bt])
            self.TS("dve", kf, y, -PI, 2 * PI, ALU.is_lt, ALU.mult, R=[Bt], WA=[Bt])
            self.TT("dve", y, y, kf, ALU.add, R=[Bt], WA=[Bt])
            self.ACT(dst[:, idx], y, AF.Sin, R=[Bt], WA=[Bd])

    def emit_rstd(self, out, in_, scale, R, W):
        self.ACT(out, in_, AF.Ln, R=list(R) + [self.B("consts")], W=W, scale=scale, bias=self.small[:, 0:1])
        self.ACT(out, out, AF.Exp, R=W, WA=W, scale=-0.5)

    def emit_norm(self, gcol):
        cB = self.B("consts")
        junk = self.carve([D], BF16)
        xn = [self.carve([D], BF16) for _ in range(2)]
        ss = self.carve([4], F32)
        gb = gcol.unsqueeze(2).to_broadcast([128, 8, 128])
        for t in range(NT):
            p = t % 2
            BXt = self.B(f"X{t}")
            Bss = self.B(f"n_ss{p}")
            Bxn = self.B(f"n_xn{p}")
            Bpt = self.B(f"bank{4 + p}")
            self.ACT(junk, self.X[:, t, :], AF.Square, R=[BXt], W=[Bss], WA=[self.B("n_junk")], accum=ss[:, p:p + 1])
            self.emit_rstd(ss[:, 2 + p:3 + p], ss[:, p:p + 1], 1.0 / D, R=[Bss], W=[self.B(f"n_rs{p}")])
            self.ACT(xn[p], self.X[:, t, :], AF.Copy, R=[BXt, self.B(f"n_rs{p}")], W=[Bxn], scale=ss[:, 2 + p:3 + p])
            pt = self.pbank(4 + p, BF16).rearrange("p (c t) -> p c t", t=128)
            for c in range(8):
                self.TR(pt[:, c, :], xn[p][:, c * 128:(c + 1) * 128], R=[Bxn], W=[Bpt] if c == 0 else (), WA=() if c == 0 else [Bpt])
            self.TT("dve", self.HT[:, :, t * 128:(t + 1) * 128], pt, gb, ALU.mult, R=[Bpt, cB], W=[self.B(f"HT{t}")])

    def emit_attn(self, li):
        j = li // 2
        lambda_init = 0.8 - 0.6 * math.exp(-0.3 * li)
        cB = self.B("consts")
        X, HT = self.X, self.HT
        WA = [self.carve([8, 768], BF16) for _ in range(2)]
        WB = [self.carve([2, D], BF16) for _ in range(2)]
        qT = self.carve([2, S], BF16)
        kT = self.carve([2, S], BF16)
        V = self.carve([NT, 2, 129], BF16)
        onT = self.carve([2, S], BF16)
        Pt = [[self.carve([512], BF16) for _ in range(2)] for _ in range(2)]
        qkf = [self.carve([512], F32) for _ in range(2)]
        qkb = [self.carve([512], BF16) for _ in range(2)]
        rt = [self.carve([8, 8], F32) for _ in range(4)]
        of = [self.carve([128], F32) for _ in range(2)]
        onb = [self.carve([128], BF16) for _ in range(2)]
        junk2 = self.carve([128], F32)
        sm = self.carve([32], F32)
        lamj = self.carve([64], F32)
        Bl = self.B("a_lam")
        li0 = self.lamin[:, j * 256:(j + 1) * 256]
        self.STT(lamj, li0[:, 0:64], 1.0, li0[:, 64:128], ALU.mult, ALU.mult, R=[cB], W=[Bl], accum=sm[:, 0:1])
        self.STT(lamj, li0[:, 128:192], 1.0, li0[:, 192:256], ALU.mult, ALU.mult, R=[cB], WA=[Bl], accum=sm[:, 1:2])
        self.ACT(sm[:, 2:4], sm[:, 0:2], AF.Exp, R=[Bl], WA=[Bl])
        self.TT("dve", sm[:, 4:5], sm[:, 3:4], sm[:, 2:3], ALU.subtract, R=[Bl], WA=[Bl])
        self.TS("dve", sm[:, 5:6], sm[:, 4:5], -lambda_init, None, ALU.add, None, R=[Bl], WA=[Bl])
        neglam = sm[:, 5:6]
        self.TS("dve", sm[:, 6:7], self.subln[:, j:j + 1], 1.0 - lambda_init, None, ALU.mult, None, R=[cB], WA=[Bl])
        sgcol = sm[:, 6:7]
        self.MEMSET("pool", V[:, :, :, 128:129], 1.0, W=[self.B("a_Vones")])

        self.emit_norm(self.nmg[:, li * 8:(li + 1) * 8])

        wq = self.a_wqkv[self.attn_js.index(j)]
        wo = self.a_wo[self.attn_js.index(j)]

        def load_w(g):
            sl = g % 2
            Bw = self.B(f"a_WA{sl}")
            for blk in range(3):
                src = wq[:, blk * D + g * 256: blk * D + (g + 1) * 256].rearrange("(k p) c -> p k c", p=128)
                self.DMA("pool", WA[sl][:, :, blk * 256:(blk + 1) * 256], src, f"a_WA{sl}", W=[Bw] if blk == 0 else (), WA=() if blk == 0 else [Bw])
            src = wo[g * 256:(g + 1) * 256, :].rearrange("(k p) c -> p k c", p=128)
            self.DMA("pool", WB[sl], src, f"a_WB{sl}", W=[self.B(f"a_WB{sl}")])

        acos = self.acs[:, 0]
        asin = self.acs[:, 1]
        load_w(0)
        for g in range(4):
            sl = g % 2
            if g + 1 < 4:
                load_w(g + 1)
            Bw = self.B(f"a_WA{sl}")
            Bwo = self.B(f"a_WB{sl}")
            for t in range(NT):
                p = t % 2
                tsl = slice(t * 128, (t + 1) * 128)
                BHt = self.B(f"HT{t}")
                ps_qk = self.pbank(2 * p)
                ps_v = self.pbank(2 * p + 1)[:, 0:256]
                Bqk = self.B(f"bank{2 * p}")
                Bv = self.B(f"bank{2 * p + 1}")
                for k in range(8):
                    self.MM(ps_qk, HT[:, k, tsl], WA[sl][:, k, 0:512], k == 0, k == 7, R=[BHt, Bw], W=[Bqk] if k == 0 else (), WA=() if k == 0 else [Bqk])
                for k in range(8):
                    self.MM(ps_v, HT[:, k, tsl], WA[sl][:, k, 512:768], k == 0, k == 7, R=[BHt, Bw], W=[Bv] if k == 0 else (), WA=() if k == 0 else [Bv])
                BVt = self.B(f"a_V{t}")
                self.CP("act", V[:, t, :, 0:128], ps_v.rearrange("p (h e) -> p h e", e=128), R=[Bv, self.B("a_Vones")], W=[BVt])
                Bf = self.B(f"a_qkf{p}")
                self.ACT(qkf[p][:, 0:256], ps_qk[:, 0:256], AF.Copy, R=[Bqk], W=[Bf], scale=0.125)
                self.CP("act", qkf[p][:, 256:512], ps_qk[:, 256:512], R=[Bqk], WA=[Bf])
                v3 = qkf[p].rearrange("p (g d) -> p g d", d=64)
                x1 = v3[:, :, 0:8]
                x2 = v3[:, :, 8:16]
                cb_ = acos[:, t, :].unsqueeze(1).to_broadcast([128, 8, 8])
                sb_ = asin[:, t, :].unsqueeze(1).to_broadcast([128, 8, 8])
                Brt = self.B("a_rt")
                Bacs = self.B("acs")
                self.TT("dve", rt[0], x1, cb_, ALU.mult, R=[Bf, Bacs], W=[Brt])
                self.TT("dve", rt[1], x2, sb_, ALU.mult, R=[Bf, Bacs], WA=[Brt])
                self.TT("dve", rt[2], x2, cb_, ALU.mult, R=[Bf, Bacs], WA=[Brt])
                self.TT("dve", rt[3], x1, sb_, ALU.mult, R=[Bf, Bacs], WA=[Brt])
                self.TT("dve", x1, rt[0], rt[1], ALU.subtract, R=[Brt], WA=[Bf])
                self.TT("dve", x2, rt[2], rt[3], ALU.add, R=[Brt], WA=[Bf])
                Bb = self.B(f"a_qkb{p}")
                self.CP("pool", qkb[p], qkf[p], R=[Bf], W=[Bb])
                ptb = self.pbank(7, BF16)[:, 0:512].rearrange("p (i t) -> p i t", t=128)
                Bpt = self.B("bank7")
                for i in range(4):
                    self.TR(ptb[:, i, :], qkb[p][:, i * 128:(i + 1) * 128], R=[Bb], W=[Bpt] if i == 0 else (), WA=() if i == 0 else [Bpt])
                self.CP("dve", qT[:, :, tsl], ptb[:, 0:2, :], R=[Bpt], W=[self.B(f"a_qT{t}")])
                self.CP("act", kT[:, :, tsl], ptb[:, 2:4, :], R=[Bpt], W=[self.B(f"a_kT{t}")])
            it = 0
            for hl in range(2):
                for qt in range(4):
                    started = [False, False, False]
                    Bacc = [self.B(f"bank{4 + b}") for b in range(3)]
                    BqTs = [self.B(f"a_qT{4 * qt + i}") for i in range(4)]
                    nkb = 4 * qt + 4

                    def acc_ap(c, qb):
                        a = c * 4 + qb
                        return self.pbank(4 + a // 3)[:, (a % 3) * 129:(a % 3) * 129 + 129], a // 3

                    for kb in range(nkb):
                        jd = kb - 4 * qt
                        c0 = max(jd, 0) * 128
                        par = it % 2
                        it += 1
                        for c in range(2):
                            Bs = self.B(f"bank{2 * par + c}")
                            self.MM(self.pbank(2 * par + c)[:, c0:512], kT[c * 64:(c + 1) * 64, hl, kb * 128:(kb + 1) * 128],
                                    qT[c * 64:(c + 1) * 64, hl, qt * 512 + c0:(qt + 1) * 512], True, True,
                                    R=[self.B(f"a_kT{kb}")] + BqTs, W=[Bs])
                        for c in range(2):
                            Bs = self.B(f"bank{2 * par + c}")
                            Bp = self.B(f"a_P{par}{c}")
                            self.ACT(Pt[par][c][:, c0:512], self.pbank(2 * par + c)[:, c0:512], AF.Exp, R=[Bs], W=[Bp])
                            if jd >= 0:
                                self.TT("pool", Pt[par][c][:, c0:c0 + 128], Pt[par][c][:, c0:c0 + 128], self.cmask, ALU.mult, R=[Bp, cB], WA=[Bp])
                        for c in range(2):
                            Bp = self.B(f"a_P{par}{c}")
                            for qb in range(max(jd, 0), 4):
                                ap_, b = acc_ap(c, qb)
                                st = not started[b]
                                started[b] = True
                                self.MM(ap_, Pt[par][c][:, qb * 128:(qb + 1) * 128], V[:, kb, hl, :], st, kb == 4 * qt + qb,
                                        R=[Bp, self.B(f"a_V{kb}")], W=[Bacc[b]] if st else (), WA=() if st else [Bacc[b]])
                    for qb in range(4):
                        t = 4 * qt + qb
                        p = qb % 2
                        a1, b1 = acc_ap(0, qb)
                        a2, b2 = acc_ap(1, qb)
                        Bsm = self.B(f"a_sm{p}")
                        o_ = 8 + p * 8
                        self.P.op("dve", (lambda a1=a1, o_=o_: lambda e: e.reciprocal(out=sm[:, o_:o_ + 1], in_=a1[:, 128:129]))(), R=[Bacc[b1]], W=[Bsm] + self.xb(a1))
                        self.P.op("dve", (lambda a2=a2, o_=o_: lambda e: e.reciprocal(out=sm[:, o_ + 1:o_ + 2], in_=a2[:, 128:129]))(), R=[Bacc[b2]], W=self.xb(a2), WA=[Bsm])
                        self.TT("dve", sm[:, o_ + 1:o_ + 2], sm[:, o_ + 1:o_ + 2], neglam, ALU.mult, R=[Bsm, Bl], WA=[Bsm])
                        Bo = self.B(f"a_of{p}")
                        self.TS("dve", of[p], a1[:, 0:128], sm[:, o_:o_ + 1], None, ALU.mult, None, R=[Bacc[b1], Bsm], W=[Bo])
                        self.STT(of[p], a2[:, 0:128], sm[:, o_ + 1:o_ + 2], of[p], ALU.mult, ALU.add, R=[Bacc[b2], Bsm, Bo], WA=[Bo])
                        self.STT(junk2, of[p], 1.0, of[p], ALU.mult, ALU.mult, R=[Bo], WA=[Bsm], accum=sm[:, o_ + 2:o_ + 3])
                        self.emit_rstd(sm[:, o_ + 3:o_ + 4], sm[:, o_ + 2:o_ + 3], 1.0 / 128, R=[Bsm], W=[self.B(f"a_rs{p}")])
                        Bon = self.B(f"a_on{p}")
                        self.TS("dve", onb[p], of[p], sm[:, o_ + 3:o_ + 4], None, ALU.mult, None, R=[Bo, self.B(f"a_rs{p}")], W=[Bon])
                        pt2 = self.pbank(7, BF16)[:, 512 + p * 128:512 + (p + 1) * 128]
                        Bp2 = self.B(f"bank7b{p}")
                        self.TR(pt2, onb[p], R=[Bon], W=[Bp2])
                        self.TS("dve", onT[:, hl, t * 128:(t + 1) * 128], pt2, sgcol, None, ALU.mult, None, R=[Bp2, Bl], W=[self.B(f"a_onT{hl}_{t}")])
            for t in range(NT):
                p = t % 2
                tsl = slice(t * 128, (t + 1) * 128)
                for dh in range(2):
                    Bk = self.B(f"bank{2 * p + dh}")
                    for hl in range(2):
                        self.MM(self.pbank(2 * p + dh), onT[:, hl, tsl], WB[sl][:, hl, dh * 512:(dh + 1) * 512], hl == 0, hl == 1,
                                R=[self.B(f"a_onT{hl}_{t}"), Bwo], W=[Bk] if hl == 0 else (), WA=() if hl == 0 else [Bk])
                    BXt = self.B(f"X{t}")
                    xs = X[:, t, dh * 512:(dh + 1) * 512]
                    self.TT("dve", xs, xs, self.pbank(2 * p + dh), ALU.add, R=[Bk, BXt], WA=[BXt])

    def emit_ffn(self, li):
        cB = self.B("consts")
        X, HT = self.X, self.HT
        Win = [self.carve([8, 1024], BF16) for _ in range(2)]
        Wout = [self.carve([4, D], BF16) for _ in range(2)]
        gb = [self.carve([516], F32) for _ in range(2)]
        tb = [self.carve([512], F32) for _ in range(2)]
        sl_ = [self.carve([512], F32) for _ in range(2)]
        actT = [self.carve([4, 512], BF16) for _ in range(2)]
        carry = self.carve([4, 2], F32)
        self.emit_norm(self.nfg[:, li * 8:(li + 1) * 8])
        win = self.f_win[self.ffn_ls.index(li)]
        wout = self.f_wout[self.ffn_ls.index(li)]
        groups = [(0, 4), (512, 4), (1024, 4), (1536, 4), (2048, 4), (2560, 2)]
        cw = self.cw[:, li * 66:(li + 1) * 66].rearrange("p (f j) -> p f j", j=3)
        cb = self.cb[:, li * 22:(li + 1) * 22]

        def load_w(gi):
            f0, nch = groups[gi]
            nf = nch * 128
            s_ = gi % 2
            Bw = self.B(f"f_Win{s_}")
            self.DMA("pool", Win[s_][:, :, 0:nf], win[:, f0:f0 + nf].rearrange("(k p) c -> p k c", p=128), f"f_Win{s_}", W=[Bw])
            self.DMA("pool", Win[s_][:, :, 512:512 + nf], win[:, FFN + f0:FFN + f0 + nf].rearrange("(k p) c -> p k c", p=128), f"f_Win{s_}", WA=[Bw])
            self.DMA("pool", Wout[s_][:, 0:nch, :], wout[f0:f0 + nf, :].rearrange("(c p) d -> p c d", p=128), f"f_Wout{s_}", W=[self.B(f"f_Wout{s_}")])

        load_w(0)
        it = 0
        for gi, (f0, nch) in enumerate(groups):
            s_ = gi % 2
            if gi + 1 < len(groups):
                load_w(gi + 1)
            Bw = self.B(f"f_Win{s_}")
            Bwo = self.B(f"f_Wout{s_}")
            for T in range(4):
                ap_ = T % 2
                BHs = [self.B(f"HT{4 * T + i}") for i in range(4)]
                Bact = self.B(f"f_act{ap_}")
                for fc in range(nch):
                    par = it % 2
                    it += 1
                    fi = f0 // 128 + fc
                    psG = self.pbank(2 * par)
                    psU = self.pbank(2 * par + 1)
                    BG = self.B(f"bank{2 * par}")
                    BU = self.B(f"bank{2 * par + 1}")
                    for k in range(8):
                        self.MM(psG, Win[s_][:, k, fc * 128:(fc + 1) * 128], HT[:, k, T * 512:(T + 1) * 512], k == 0, k == 7,
                                R=BHs + [Bw], W=[BG] if k == 0 else (), WA=() if k == 0 else [BG])
                    for k in range(8):
                        self.MM(psU, Win[s_][:, k, 512 + fc * 128:512 + (fc + 1) * 128], HT[:, k, T * 512:(T + 1) * 512], k == 0, k == 7,
                                R=BHs + [Bw], W=[BU] if k == 0 else (), WA=() if k == 0 else [BU])
                    Bgb = self.B(f"f_gb{par}")
                    Bc = self.B(f"f_carry{fc}")
                    if T == 0:
                        self.MEMSET("pool", gb[par][:, 0:2], 0.0, W=[Bgb])
                    else:
                        self.CP("pool", gb[par][:, 0:2], carry[:, fc, :], R=[Bc], W=[Bgb])
                    self.CP("act", gb[par][:, 2:514], psG, R=[BG], WA=[Bgb])
                    self.CP("pool", carry[:, fc, :], gb[par][:, 512:514], R=[Bgb], W=[Bc])
                    Btb = self.B(f"f_tb{par}")
                    self.TS("pool", tb[par], gb[par][:, 2:514], cw[:, fi, 2:3], cb[:, fi:fi + 1], ALU.mult, ALU.add, R=[Bgb, cB], W=[Btb])
                    self.STT(tb[par], gb[par][:, 1:513], cw[:, fi, 1:2], tb[par], ALU.mult, ALU.add, R=[Bgb, cB, Btb], WA=[Btb])
                    self.STT(tb[par], gb[par][:, 0:512], cw[:, fi, 0:1], tb[par], ALU.mult, ALU.add, R=[Bgb, cB, Btb], WA=[Btb])
                    Bsl = self.B(f"f_sl{par}")
                    self.ACT(sl_[par], tb[par], AF.Silu, R=[Btb], W=[Bsl])
                    self.TT("dve", actT[ap_][:, fc, :], sl_[par], psU, ALU.mult, R=[Bsl, BU], W=[Bact] if fc == 0 else (), WA=() if fc == 0 else [Bact])
                for tb_ in range(4):
                    t = 4 * T + tb_
                    p2 = t % 2
                    BXt = self.B(f"X{t}")
                    for dh in range(2):
                        bk = 4 + 2 * p2 + dh
                        Bk = self.B(f"bank{bk}")
                        for fc in range(nch):
                            self.MM(self.pbank(bk), actT[ap_][:, fc, tb_ * 128:(tb_ + 1) * 128], Wout[s_][:, fc, dh * 512:(dh + 1) * 512],
                                    fc == 0, fc == nch - 1, R=[Bact, Bwo], W=[Bk] if fc == 0 else (), WA=() if fc == 0 else [Bk])
                        xs = X[:, t, dh * 512:(dh + 1) * 512]
                        self.TT("dve", xs, xs, self.pbank(bk), ALU.add, R=[Bk, BXt], WA=[BXt])

    def emit_ret(self, li):
        j = li // 2
        cB = self.B("consts")
        X, HT = self.X, self.HT
        WA = self.carve([8, 1536], BF16)
        WB = [self.carve([4, D], BF16) for _ in range(2)]
        St = self.carve([2, 512], F32)
        Sbf = [self.carve([2, 512], BF16) for _ in range(2)]
        rt = [self.carve([2, 128], F32) for _ in range(4)]
        qkr = [self.carve([2, 2, 128], BF16) for _ in range(2)]
        vbf = [self.carve([512], BF16) for _ in range(2)]
        sg = [self.carve([512], F32) for _ in range(2)]
        kd = [self.carve([256], BF16) for _ in range(2)]
        qkT = [self.carve([4, 128], BF16) for _ in range(2)]
        innT = [self.carve([128], BF16) for _ in range(2)]
        gated = [self.carve([512], BF16) for _ in range(2)]
        goT = [self.carve([4, 128], BF16) for _ in range(2)]
        junk = self.carve([512], BF16)
        sm = self.carve([16], F32)
        self.emit_norm(self.nmg[:, li * 8:(li + 1) * 8])
        wq = self.r_wqkvg[self.ret_js.index(j)]
        wo = self.r_wo[self.ret_js.index(j)]
        rcos = self.rcs[:, 0]
        rsin = self.rcs[:, 1]
        Brcs = self.B("rcs")
        gam = [1.0 - 2.0 ** (-5.0 - h) for h in range(4)]

        def load_w(h):
            blocks = [(h * 256, 256, 0), (1024 + h * 256, 256, 256), (2048 + h * 512, 512, 512), (4096 + h * 512, 512, 1024)]
            names = ["r_Wqk", "r_Wqk", "r_Wv", "r_Wg"]
            first = {"r_Wqk": True, "r_Wv": True, "r_Wg": True}
            for (c0, n, d0), nm in zip(blocks, names):
                src = wq[:, c0:c0 + n].rearrange("(k p) c -> p k c", p=128)
                Bw = self.B(nm)
                self.DMA("pool", WA[:, :, d0:d0 + n], src, nm, W=[Bw] if first[nm] else (), WA=() if first[nm] else [Bw])
                first[nm] = False
            s_ = h % 2
            self.DMA("pool", WB[s_], wo[h * 512:(h + 1) * 512, :].rearrange("(c p) d -> p c d", p=128), f"r_WB{s_}", W=[self.B(f"r_WB{s_}")])

        for h in range(4):
            load_w(h)
            s_ = h % 2
            Bwo = self.B(f"r_WB{s_}")
            cd = gam[h] ** 128
            qd = self.rcol[:, h:h + 1]
            qd2 = self.rcol[:, 4 + h:5 + h]
            kdc = self.rcol[:, 8 + h:9 + h]
            maskT = self.rmask[:, h, :]

            def stageA(n):
                p = n % 2
                tsl = slice(n * 128, (n + 1) * 128)
                BHt = self.B(f"HT{n}")
                names = ["r_Wqk", "r_Wv", "r_Wg"]
                for b in range(3):
                    Bk = self.B(f"bank{b}")
                    Bw = self.B(names[b])
                    for k in range(8):
                        self.MM(self.pbank(b), HT[:, k, tsl], WA[:, k, b * 512:(b + 1) * 512], k == 0, k == 7, R=[BHt, Bw],
                                W=[Bk] if k == 0 else (), WA=() if k == 0 else [Bk])
                B0, B1, B2 = self.B("bank0"), self.B("bank1"), self.B("bank2")
                v4 = self.pbank(0).rearrange("p (a i e) -> p a i e", i=128, e=2)
                E = v4[:, :, :, 0]
                O = v4[:, :, :, 1]
                cb_ = rcos[:, n, :].unsqueeze(1).to_broadcast([128, 2, 128])
                sb_ = rsin[:, n, :].unsqueeze(1).to_broadcast([128, 2, 128])
                Brt = self.B("r_rt")
                self.TT("dve", rt[0], E, cb_, ALU.mult, R=[B0, Brcs], W=[Brt])
                self.TT("dve", rt[1], O, sb_, ALU.mult, R=[B0, Brcs], WA=[Brt])
                self.TT("dve", rt[2], O, cb_, ALU.mult, R=[B0, Brcs], WA=[Brt])
                self.TT("dve", rt[3], E, sb_, ALU.mult, R=[B0, Brcs], WA=[Brt])
                Bqr = self.B(f"r_qkr{p}")
                self.TT("dve", qkr[p][:, :, 0, :], rt[0], rt[1], ALU.subtract, R=[Brt], W=[Bqr])
                self.TT("dve", qkr[p][:, :, 1, :], rt[2], rt[3], ALU.add, R=[Brt], WA=[Bqr])
                self.CP("act", vbf[p], self.pbank(1), R=[B1], W=[self.B(f"r_v{p}")])
                self.ACT(sg[p], self.pbank(2), AF.Silu, R=[B2], W=[self.B(f"r_sg{p}")])
                kflat = qkr[p][:, 1].rearrange("p a b -> p (a b)")
                self.ACT(kd[p], kflat, AF.Copy, R=[Bqr, cB], W=[self.B(f"r_kd{p}")], scale=kdc)
                ptb = self.pbank(3, BF16)[:, 0:512].rearrange("p (i t) -> p i t", t=128)
                Bpt = self.B("bank3a")
                qflat = qkr[p].rearrange("p a b c -> p (a b c)")
                for i in range(4):
                    self.TR(ptb[:, i, :], qflat[:, i * 128:(i + 1) * 128], R=[Bqr], W=[Bpt] if i == 0 else (), WA=() if i == 0 else [Bpt])
                self.CP("act", qkT[p], ptb, R=[Bpt], W=[self.B(f"r_qkT{p}")])

            def stageB(n):
                p = n % 2
                BqkT = self.B(f"r_qkT{p}")
                Bv = self.B(f"r_v{p}")
                pin = self.pbank(3)[:, 256:384]
                Bin = self.B("bank3b")
                for dc in range(2):
                    self.MM(pin, qkT[p][:, 2 + dc, :], qkT[p][:, dc, :], dc == 0, dc == 1, R=[BqkT], W=[Bin] if dc == 0 else (), WA=() if dc == 0 else [Bin])
                Bit = self.B(f"r_innT{p}")
                self.TT("dve", innT[p], pin, maskT, ALU.mult, R=[Bin, cB], W=[Bit])
                BO = self.B("bank4")
                pO = self.pbank(4)
                sp_ = (n - 1) % 2
                self.MM(pO, innT[p], vbf[p], True, n == 0, R=[Bit, Bv], W=[BO])
                if n > 0:
                    Bs = self.B(f"r_Sbf{sp_}")
                    for dc in range(2):
                        self.MM(pO, qkT[p][:, dc, :], Sbf[sp_][:, dc, :], False, dc == 1, R=[BqkT, Bs], WA=[BO])
                Bkd = self.B(f"r_kd{p}")
                BSt = self.B("r_St")
                if n < NT - 1:
                    for dc in range(2):
                        Bd = self.B(f"bank{5 + dc}")
                        self.MM(self.pbank(5 + dc), kd[p][:, dc * 128:(dc + 1) * 128], vbf[p], True, True, R=[Bkd, Bv], W=[Bd])
                        if n == 0:
                            self.CP("dve", St[:, dc, :], self.pbank(5 + dc), R=[Bd], W=[BSt] if dc == 0 else (), WA=() if dc == 0 else [BSt])
                        else:
                            self.STT(St[:, dc, :], St[:, dc, :], cd, self.pbank(5 + dc), ALU.mult, ALU.add, R=[Bd, BSt], WA=[BSt])
                    self.CP("pool", Sbf[p], St, R=[BSt], W=[self.B(f"r_Sbf{p}")])
                Bsm = self.B(f"r_sm{p}")
                o_ = p * 8
                self.ACT(junk, pO, AF.Square, R=[BO], W=[Bsm], WA=[self.B("r_junk")], accum=sm[:, o_:o_ + 1])
                self.TT("dve", sm[:, o_ + 1:o_ + 2], sm[:, o_:o_ + 1], qd2, ALU.mult, R=[Bsm, cB], WA=[Bsm])
                self.emit_rstd(sm[:, o_ + 2:o_ + 3], sm[:, o_ + 1:o_ + 2], 1.0, R=[Bsm], W=[self.B(f"r_rs{p}")])
                self.TT("dve", sm[:, o_ + 3:o_ + 4], sm[:, o_ + 2:o_ + 3], qd, ALU.mult, R=[self.B(f"r_rs{p}"), cB], W=[self.B(f"r_rq{p}")])
                Bg = self.B(f"r_gated{p}")
                self.STT(gated[p], pO, sm[:, o_ + 3:o_ + 4], sg[p], ALU.mult, ALU.mult, R=[BO, self.B(f"r_rq{p}"), self.B(f"r_sg{p}")], W=[Bg])
                ptb = self.pbank(7, BF16)[:, 0:512].rearrange("p (i t) -> p i t", t=128)
                Bpt = self.B("bank7")
                for i in range(4):
                    self.TR(ptb[:, i, :], gated[p][:, i * 128:(i + 1) * 128], R=[Bg], W=[Bpt] if i == 0 else (), WA=() if i == 0 else [Bpt])
                BgT = self.B(f"r_goT{p}")
                self.CP("act", goT[p], ptb, R=[Bpt], W=[BgT])
                BXt = self.B(f"X{n}")
                tslx = n
                for dh in range(2):
                    Bk = self.B("bank6") if dh == 0 else self.B("bank5")
                    bk = 6 if dh == 0 else 5
                    if dh == 1:
                        bk = 6
                        Bk = self.B("bank6")
                    for ec in range(4):
                        self.MM(self.pbank(bk), goT[p][:, ec, :], WB[s_][:, ec, dh * 512:(dh + 1) * 512], ec == 0, ec == 3, R=[BgT, Bwo],
                                W=[Bk] if ec == 0 else (), WA=() if ec == 0 else [Bk])
                    xs = X[:, tslx, dh * 512:(dh + 1) * 512]
                    self.TT("dve", xs, xs, self.pbank(bk), ALU.add, R=[Bk, BXt], WA=[BXt])

            stageA(0)
            for n in range(NT):
                if n + 1 < NT:
                    stageA(n + 1)
                stageB(n)

    def emit_out(self, s):
        junk = self.carve([D], BF16)
        ob = [self.carve([D], F32) for _ in range(2)]
        ss = self.carve([4], F32)
        gfull = self.carve([D], F32)
        Bg = self.B("o_g")
        self.DMA("sp", gfull, self.fng_d, "o_g", W=[Bg])
        for t in range(NT):
            p = t % 2
            BXt = self.B(f"X{t}")
            Bob = self.B(f"o_b{p}")
            if self.final_norm:
                Bss = self.B(f"o_ss{p}")
                self.ACT(junk, self.X[:, t, :], AF.Square, R=[BXt], W=[Bss], WA=[self.B("o_junk")], accum=ss[:, p:p + 1])
                self.emit_rstd(ss[:, 2 + p:3 + p], ss[:, p:p + 1], 1.0 / D, R=[Bss], W=[self.B(f"o_rs{p}")])
                self.STT(ob[p], self.X[:, t, :], ss[:, 2 + p:3 + p], gfull, ALU.mult, ALU.mult, R=[BXt, self.B(f"o_rs{p}"), Bg], W=[Bob])
            else:
                self.CP("dve", ob[p], self.X[:, t, :], R=[BXt], W=[Bob])
            self.DMA("sp", self.out_d[s, t * 128:(t + 1) * 128, :], ob[p], "out", R=[Bob], WA=[self.B("out")])


def _consts():
    ident = np.eye(128, dtype=np.float32)
    cm = (np.arange(128)[:, None] <= np.arange(128)[None, :]).astype(np.float32)
    cbf = np.concatenate([ident, cm], axis=1).astype(ml_dtypes.bfloat16)
    afreq = (500000.0 ** (-np.arange(0, 16, 2, dtype=np.float32) / np.float32(16))).astype(np.float32)
    rfreq = (1.0 / (10000.0 ** np.linspace(0.0, 1.0, 128, dtype=np.float32))).astype(np.float32)
    cf = np.zeros((128, 664), np.float32)
    cf[:, 0:8] = afreq[None, :]
    cf[:, 8:136] = rfreq[None, :]
    idx = np.arange(128, dtype=np.float64)
    for h in range(4):
        gam = 1.0 - 2.0 ** (-5.0 - h)
        m = np.where(idx[None, :] >= idx[:, None], (gam ** (-(idx[:, None] + 1.0))) / 16.0, 0.0)
        cf[:, 136 + h * 128:136 + (h + 1) * 128] = m
        qd = gam ** (idx + 1.0)
        cf[:, 648 + h] = qd
        cf[:, 652 + h] = qd * qd / 512.0
        cf[:, 656 + h] = gam ** (127.0 - idx) / 16.0
    return cbf, cf


_NC_CACHE = {}


def _get_nc(layers, nseq, final_norm):
    key = (tuple(layers), nseq, final_norm)
    if key not in _NC_CACHE:
        _NC_CACHE[key] = Builder(list(layers), nseq, final_norm).build()
    return _NC_CACHE[key]


PLAN = [[(0, True, False)], [(0, False, True)], [(1, True, False)], [(1, False, True)],
        [(2, True, False)], [(2, False, True)], [(3, True, False)], [(3, False, True)]]


def _norm_layers(layers):
    return tuple(e if isinstance(e, tuple) else (e, True, True) for e in layers)


def _run(inputs, layers=None, final_norm=True, plan=None):
    f = lambda a: np.ascontiguousarray(np.asarray(a, dtype=np.float32))
    x = f(inputs["x"])
    pos = np.ascontiguousarray(np.asarray(inputs["positions"], dtype=np.int32))
    cbf, cf = _consts()
    fm = lambda g: np.ascontiguousarray(f(g).reshape(DEPTH, 8, 128).transpose(2, 0, 1).reshape(128, DEPTH * 8))
    lam = np.concatenate([f(inputs["attn_lambda_q1"]), f(inputs["attn_lambda_k1"]),
                          f(inputs["attn_lambda_q2"]), f(inputs["attn_lambda_k2"])], axis=1)
    lam = np.ascontiguousarray(np.broadcast_to(lam.reshape(1, 512), (128, 512)))
    subln = np.ascontiguousarray(f(inputs["attn_subln_g"]).T)
    cw = f(inputs["ffn_conv_w"]).reshape(DEPTH, 3, NFC, 128).transpose(3, 0, 2, 1)
    cw = np.ascontiguousarray(cw.reshape(128, DEPTH * NFC * 3))
    cb = np.ascontiguousarray(f(inputs["ffn_conv_b"]).reshape(DEPTH, NFC, 128).transpose(2, 0, 1).reshape(128, DEPTH * NFC))
    small = dict(
        nmg=fm(inputs["norm_mix_g"]), nfg=fm(inputs["norm_ffn_g"]),
        fng=np.ascontiguousarray(np.broadcast_to(f(inputs["final_norm_g"]).reshape(1, D), (128, D))),
        lam_in=lam, subln=subln, f_cw=cw, f_cb=cb, c_bf=cbf, c_f32=cf,
    )
    if plan is None:
        plan = [list(layers)] if layers is not None else PLAN
    pcs = [np.ascontiguousarray(pos[c * NSEQ:(c + 1) * NSEQ].reshape(NSEQ, NT, 128).transpose(0, 2, 1)) for c in range(N_CORES)]
    cur = x
    for li_, lay in enumerate(plan):
        lay = _norm_layers(lay)
        fn = final_norm and (li_ == len(plan) - 1)
        key = (lay, NSEQ, fn)
        if key not in _NC_CACHE:
            b = Builder(list(lay), NSEQ, fn)
            _NC_CACHE[key] = (b.build(), b)
        nc, b = _NC_CACHE[key]
        shared = dict(small)
        if b.attn_js:
            shared["a_wqkv"] = f(inputs["attn_w_qkv"])[b.attn_js]
            shared["a_wo"] = f(inputs["attn_w_o"])[b.attn_js]
        if b.ret_js:
            shared["r_wqkvg"] = f(inputs["ret_w_qkvg"])[b.ret_js]
            shared["r_wo"] = f(inputs["ret_w_o"])[b.ret_js]
        if b.ffn_ls:
            shared["f_win"] = f(inputs["ffn_w_in"])[b.ffn_ls]
            shared["f_wout"] = f(inputs["ffn_w_out"])[b.ffn_ls]
        in_maps = []
        for c in range(N_CORES):
            m = dict(shared)
            m["x"] = np.ascontiguousarray(cur[c * NSEQ:(c + 1) * NSEQ])
            m["pos"] = pcs[c]
            in_maps.append(m)
        res = run_bass_kernel_spmd(nc, in_maps, core_ids=list(range(N_CORES)))
        cur = np.concatenate([r["out"] for r in res.results], axis=0)
    return cur


def kernel(**inputs):
    return _run(inputs)
```

```python
import math
import numpy as np
import ml_dtypes
import concourse.bass as bass
import concourse.mybir as mybir
from concourse.bass_utils import run_bass_kernel_spmd

dt = mybir.dt
F32, BF16, I32 = dt.float32, dt.bfloat16, dt.int32
AF = mybir.ActivationFunctionType
ALU = mybir.AluOpType
COMPUTE = ("pe", "act", "dve", "pool")

D = 1024
S = 2048
NT = 16
NSEQ = 2
DEPTH = 4
FFN = 2816
NFC = 22
EPS = 1e-6
N_CORES = 8
PI = math.pi
EPOCH = 1024


class Buf:
    __slots__ = ("name", "w", "r")

    def __init__(self, name):
        self.name = name
        self.w = {}
        self.r = {}


class Op:
    __slots__ = ("eng", "fn", "deps", "sig", "val", "key", "is_dma", "order", "epoch")

    def __init__(self, eng, fn, is_dma=False, key=None):
        self.eng = eng
        self.fn = fn
        self.deps = {}
        self.sig = False
        self.val = 0
        self.key = key if key is not None else eng
        self.is_dma = is_dma


class Prog:
    def __init__(self):
        self.streams = {e: [] for e in ("pe", "act", "dve", "pool", "sp")}
        self.dma_cnt = {}
        self.all_ops = []

    def op(self, eng, fn, R=(), W=(), WA=()):
        o = Op(eng, fn)
        self._track(o, R, W, WA)
        return o

    def dma(self, eng, fn, key, R=(), W=(), WA=()):
        o = Op(eng, fn, is_dma=True, key="dma:" + key)
        self.dma_cnt[key] = self.dma_cnt.get(key, 0) + 1
        o.val = 16 * self.dma_cnt[key]
        o.sig = True
        self._track(o, R, W, WA)
        return o

    def _track(self, o, R, W, WA):
        o.order = len(self.all_ops)
        self.all_ops.append(o)
        self.streams[o.eng].append(o)
        for b in R:
            for s in b.w.values():
                self._add(o, s, True)
        for b in list(W) + list(WA):
            for s in b.w.values():
                self._add(o, s, False)
            for s in b.r.values():
                self._add(o, s, False)
        for b in R:
            b.r[o.key] = o
        for b in W:
            b.w = {o.key: o}
            b.r = {}
        for b in WA:
            b.w[o.key] = o

    def _add(self, o, s, raw):
        if s is o:
            return
        if (not s.is_dma) and (not o.is_dma) and s.eng == o.eng:
            if not raw or s.eng == "pe":
                return
        cur = o.deps.get(s.key)
        if cur is None or cur.order < s.order:
            o.deps[s.key] = s

    def finalize(self):
        for o in self.all_ops:
            for s in o.deps.values():
                s.sig = True
        cnt = {e: 0 for e in COMPUTE}
        for o in self.all_ops:
            if not o.is_dma and o.sig:
                cnt[o.eng] += 1
                o.epoch = (cnt[o.eng] - 1) // EPOCH
                o.val = (cnt[o.eng] - 1) % EPOCH + 1
        self.n_epochs = {e: (cnt[e] + EPOCH - 1) // EPOCH for e in COMPUTE}

    def replay(self, name, eng, sems, dma_sems):
        waited = {}
        for o in self.streams[name]:
            for k, s in o.deps.items():
                if s.is_dma:
                    if waited.get(k, 0) < s.val:
                        eng.wait_ge(dma_sems[k[4:]], s.val)
                        waited[k] = s.val
                else:
                    tv = (s.epoch, s.val)
                    if waited.get(k, (-1, 0)) < tv:
                        eng.wait_ge(sems[(s.eng, s.epoch)], s.val)
                        waited[k] = tv
            if o.fn is None:
                continue
            ins = o.fn(eng)
            if o.is_dma:
                ins.then_inc(dma_sems[o.key[4:]], 16)
            elif o.sig:
                ins.then_inc(sems[(o.eng, o.epoch)], 1)


class Builder:
    def __init__(self, layers, nseq, final_norm):
        self.layers = layers
        self.nseq = nseq
        self.final_norm = final_norm
        self.P = Prog()
        self.nc = bass.Bass("TRN2", target_bir_lowering=False)
        self.bufs = {}

    def B(self, name):
        b = self.bufs.get(name)
        if b is None:
            b = self.bufs[name] = Buf(name)
        return b

    def Bs(self, names):
        return [self.B(n) for n in names]

    def xb(self, *aps):
        out = []
        for a in aps:
            try:
                if a.tensor.name != "psum":
                    continue
            except AttributeError:
                continue
            es = 4 if a.dtype in (F32, I32) else 2
            b = (a.offset * es) // 2048
            bb = self.B(f"xbank{b}")
            if bb not in out:
                out.append(bb)
        return out

    def dram_in(self, name, shape, d=F32):
        return self.nc.dram_tensor(name, list(shape), d, kind="ExternalInput").ap()

    def MM(self, out, lhsT, rhs, start, stop, R, W=(), WA=()):
        self.P.op("pe", lambda e: e.matmul(out, lhsT=lhsT, rhs=rhs, start=start, stop=stop,
                                           skip_group_check=True), R=R, W=list(W) + self.xb(out), WA=WA)

    def TR(self, out, in_, R, W=(), WA=()):
        ident = self.ident
        self.P.op("pe", lambda e: e.transpose(out=out, in_=in_, identity=ident), R=list(R) + [self.B("consts")], W=list(W) + self.xb(out), WA=WA)

    def ACT(self, out, in_, func, R, W=(), WA=(), scale=None, bias=None, accum=None):
        kw = {}
        if scale is not None:
            kw["scale"] = scale
        if bias is not None:
            kw["bias"] = bias
        if accum is not None:
            kw["accum_out"] = accum
        self.P.op("act", lambda e: e.activation(out=out, in_=in_, func=func, **kw), R=R, W=list(W) + self.xb(out, in_), WA=WA)

    def TT(self, eng, out, in0, in1, op, R, W=(), WA=()):
        self.P.op(eng, lambda e: e.tensor_tensor(out=out, in0=in0, in1=in1, op=op), R=R, W=list(W) + self.xb(out, in0, in1), WA=WA)

    def TS(self, eng, out, in0, s1, s2, op0, op1, R, W=(), WA=()):
        if op1 is None:
            self.P.op(eng, lambda e: e.tensor_scalar(out=out, in0=in0, scalar1=s1, scalar2=None, op0=op0), R=R, W=list(W) + self.xb(out, in0), WA=WA)
        else:
            self.P.op(eng, lambda e: e.tensor_scalar(out=out, in0=in0, scalar1=s1, scalar2=s2, op0=op0, op1=op1), R=R, W=list(W) + self.xb(out, in0), WA=WA)

    def STT(self, out, in0, scalar, in1, op0, op1, R, W=(), WA=(), accum=None):
        if accum is None:
            self.P.op("dve", lambda e: e.scalar_tensor_tensor(out=out, in0=in0, scalar=scalar, in1=in1, op0=op0, op1=op1), R=R, W=list(W) + self.xb(out, in0, in1), WA=WA)
        else:
            self.P.op("dve", lambda e: e.scalar_tensor_tensor(out=out, in0=in0, scalar=scalar, in1=in1, op0=op0, op1=op1, accum_out=accum), R=R, W=list(W) + self.xb(out, in0, in1), WA=WA)

    def CP(self, eng, out, in_, R, W=(), WA=()):
        if eng == "act":
            self.P.op("act", lambda e: e.copy(out=out, in_=in_), R=R, W=list(W) + self.xb(out, in_), WA=WA)
        else:
            self.P.op(eng, lambda e: e.tensor_copy(out=out, in_=in_), R=R, W=list(W) + self.xb(out, in_), WA=WA)

    def MEMSET(self, eng, ap, val, W=(), WA=()):
        self.P.op(eng, lambda e: e.memset(ap, val), W=W, WA=WA)

    def DMA(self, eng, out, in_, key, R=(), W=(), WA=()):
        self.P.dma(eng, lambda e: e.dma_start(out=out, in_=in_), key, R=R, W=W, WA=WA)

    def barrier(self):
        allb = list(self.bufs.values())
        P = self.P
        for e in ("pe", "act", "dve", "pool", "sp"):
            o = Op(e, None)
            o.order = len(P.all_ops)
            for b in allb:
                for src in list(b.w.values()) + list(b.r.values()):
                    if src.fn is None:
                        continue
                    if (not src.is_dma) and src.eng == e:
                        continue
                    cur = o.deps.get(src.key)
                    if cur is None or cur.order < src.order:
                        o.deps[src.key] = src
            P.all_ops.append(o)
            P.streams[e].append(o)

    def arena_reset(self):
        self.aoff = 0

    def carve(self, shape, d):
        n = 1
        for v in shape:
            n *= v
        nbytes = n * (4 if d in (F32, I32) else 2)
        nbytes = (nbytes + 31) // 32 * 32
        o2 = self.aoff // 2
        assert self.aoff + nbytes <= self.arena_bytes, (self.aoff, nbytes, self.arena_bytes)
        v = self.arena[:, o2:o2 + nbytes // 2]
        self.aoff += nbytes
        if d != BF16:
            v = v.bitcast(d)
        v = v[:, 0:n]
        if len(shape) == 2:
            v = v.rearrange("p (a b) -> p a b", b=shape[1])
        elif len(shape) == 3:
            v = v.rearrange("p (a b c) -> p a b c", b=shape[1], c=shape[2])
        return v

    def pbank(self, i, d=F32):
        v = self.psum[:, i * 512:(i + 1) * 512]
        if d == BF16:
            v = v.bitcast(BF16)
        return v

    def build(self):
        nc = self.nc
        ns = self.nseq
        self.x_d = self.dram_in("x", [ns, S, D])
        self.pos_d = self.dram_in("pos", [ns, 128, NT], I32)
        self.nmg_d = self.dram_in("nmg", [128, DEPTH * 8])
        self.nfg_d = self.dram_in("nfg", [128, DEPTH * 8])
        self.fng_d = self.dram_in("fng", [128, D])
        self.lam_d = self.dram_in("lam_in", [128, 2 * 256])
        self.subln_d = self.dram_in("subln", [128, 2])
        self.cw_d = self.dram_in("f_cw", [128, DEPTH * NFC * 3])
        self.cb_d = self.dram_in("f_cb", [128, DEPTH * NFC])
        ents = [e if isinstance(e, tuple) else (e, True, True) for e in self.layers]
        self.attn_js = sorted({li // 2 for li, m, f in ents if m and li % 2 == 0})
        self.ret_js = sorted({li // 2 for li, m, f in ents if m and li % 2 == 1})
        self.ffn_ls = sorted({li for li, m, f in ents if f})
        if self.attn_js:
            self.a_wqkv = self.dram_in("a_wqkv", [len(self.attn_js), D, 3 * D])
            self.a_wo = self.dram_in("a_wo", [len(self.attn_js), D, D])
        if self.ret_js:
            self.r_wqkvg = self.dram_in("r_wqkvg", [len(self.ret_js), D, 6144])
            self.r_wo = self.dram_in("r_wo", [len(self.ret_js), 2048, D])
        if self.ffn_ls:
            self.f_win = self.dram_in("f_win", [len(self.ffn_ls), D, 2 * FFN])
            self.f_wout = self.dram_in("f_wout", [len(self.ffn_ls), FFN, D])
        self.cbf_d = self.dram_in("c_bf", [128, 256], BF16)
        self.cf_d = self.dram_in("c_f32", [128, 8 + 128 + 512 + 16])
        self.out_d = nc.dram_tensor("out", [ns, S, D], F32, kind="ExternalOutput").ap()

        self.X = nc.alloc_sbuf_tensor("X", [128, NT, D], F32)[:]
        self.HT = nc.alloc_sbuf_tensor("HT", [128, 8, S], BF16)[:]
        self.cbf = nc.alloc_sbuf_tensor("cbf", [128, 256], BF16)[:]
        self.cf = nc.alloc_sbuf_tensor("cf", [128, 664], F32)[:]
        self.ident = self.cbf[:, 0:128]
        self.cmask = self.cbf[:, 128:256]
        self.afreq = self.cf[:, 0:8]
        self.rfreq = self.cf[:, 8:136]
        self.rmask = self.cf[:, 136:648].rearrange("p (h i) -> p h i", i=128)
        self.rcol = self.cf[:, 648:664]
        self.params = nc.alloc_sbuf_tensor("params", [128, 32 + 32 + 512 + 2 + 264 + 88], F32)[:]
        o = 0
        self.nmg = self.params[:, o:o + 32]; o += 32
        self.nfg = self.params[:, o:o + 32]; o += 32
        self.lamin = self.params[:, o:o + 512]; o += 512
        self.subln = self.params[:, o:o + 2]; o += 2
        self.cw = self.params[:, o:o + 264]; o += 264
        self.cb = self.params[:, o:o + 88]; o += 88
        self.rcs = nc.alloc_sbuf_tensor("rcs", [128, 2, NT, 128], F32)[:]
        self.acs = nc.alloc_sbuf_tensor("acs", [128, 2, NT, 8], F32)[:]
        self.posf = nc.alloc_sbuf_tensor("posf", [128, NT], F32)[:]
        self.posi = nc.alloc_sbuf_tensor("posi", [128, NT], I32)[:]
        self.small = nc.alloc_sbuf_tensor("small", [128, 64], F32)[:]
        self.arena_bytes = (nc.sbuf_bytes_remaining - 256) // 64 * 64
        self.arena = nc.alloc_sbuf_tensor("arena", [128, self.arena_bytes // 2], BF16)[:]
        self.psum = nc.alloc_psum_tensor("psum", [128, 4096], F32)[:]

        cB = self.B("consts")
        self.DMA("sp", self.cbf, self.cbf_d, "consts", W=[cB])
        self.DMA("sp", self.cf, self.cf_d, "consts", WA=[cB])
        self.DMA("sp", self.nmg, self.nmg_d, "consts", WA=[cB])
        self.DMA("sp", self.nfg, self.nfg_d, "consts", WA=[cB])
        self.DMA("sp", self.lamin, self.lam_d, "consts", WA=[cB])
        self.DMA("sp", self.subln, self.subln_d, "consts", WA=[cB])
        self.DMA("sp", self.cw, self.cw_d, "consts", WA=[cB])
        self.DMA("sp", self.cb, self.cb_d, "consts", WA=[cB])
        self.MEMSET("dve", self.small[:, 0:1], EPS, WA=[cB])

        for s in range(ns):
            self.emit_seq(s)
        self.P.op("sp", None, R=[self.B("out")])
        self.P.finalize()

        from contextlib import ExitStack
        with ExitStack() as es:
            sems = {(e, k): es.enter_context(nc.semaphore(f"s_{e}{k}")) for e in COMPUTE for k in range(self.P.n_epochs[e])}
            dsems = {k: es.enter_context(nc.semaphore("d_" + k)) for k in self.P.dma_cnt}
            P = self.P
            with nc.Block() as block:
                @block.tensor
                def _(e):
                    P.replay("pe", e, sems, dsems)

                @block.scalar
                def _(e):
                    P.replay("act", e, sems, dsems)

                @block.vector
                def _(e):
                    P.replay("dve", e, sems, dsems)

                @block.gpsimd
                def _(e):
                    P.replay("pool", e, sems, dsems)

                @block.sync
                def _(e):
                    P.replay("sp", e, sems, dsems)
        return nc

    def emit_seq(self, s):
        BX = [self.B(f"X{t}") for t in range(NT)]
        self.barrier()
        for t in range(NT):
            self.DMA("sp", self.X[:, t, :], self.x_d[s, t * 128:(t + 1) * 128, :], f"X{t}", W=[BX[t]])
        self.DMA("sp", self.posi, self.pos_d[s], "pos", W=[self.B("posi")])
        self.CP("dve", self.posf, self.posi, R=[self.B("posi")], W=[self.B("posf")])
        self.arena_reset()
        self.emit_sincos(self.afreq, 8, self.acs, "acs")
        self.arena_reset()
        self.emit_sincos(self.rfreq, 128, self.rcs, "rcs")
        for ent in self.layers:
            li, do_mix, do_ffn = ent if isinstance(ent, tuple) else (ent, True, True)
            if do_mix:
                self.barrier()
                self.arena_reset()
                if li % 2 == 0:
                    self.emit_attn(li)
                else:
                    self.emit_ret(li)
            if do_ffn:
                self.barrier()
                self.arena_reset()
                self.emit_ffn(li)
        self.barrier()
        self.arena_reset()
        self.emit_out(s)

    def emit_sincos(self, freq, F, dst, name):
        cB = self.B("consts")
        Bt = self.B("sc_tmp")
        Bd = self.B(name)
        ang = self.carve([NT, F], F32)
        ki = self.carve([NT, F], I32)
        kf = self.carve([NT, F], F32)
        y = self.carve([NT, F], F32)
        fb = freq.unsqueeze(1).to_broadcast([128, NT, F])
        pb = self.posf.unsqueeze(2).to_broadcast([128, NT, F])
        self.TT("dve", ang, fb, pb, ALU.mult, R=[cB, self.B("posf")], W=[Bt])
        self.TS("dve", ki, ang, 1.0 / (2 * PI), None, ALU.mult, None, R=[Bt], WA=[Bt])
        self.CP("dve", kf, ki, R=[Bt], WA=[Bt])
        self.STT(ang, kf, -2 * PI, ang, ALU.mult, ALU.add, R=[Bt], WA=[Bt])
        for idx, shift in ((1, 0.0), (0, PI / 2)):
            self.TS("dve", y, ang, shift, None, ALU.add, None, R=[Bt], WA=[Bt])
            self.TS("dve", kf, y, PI, -2 * PI, ALU.is_gt, ALU.mult, R=[Bt], WA=[Bt])
            self.TT("dve", y, y, kf, ALU.add, R=[Bt], WA=[Bt])
            self.TS("dve", kf, y, -PI, 2 * PI, ALU.is_lt, ALU.mult, R=[Bt], WA=[Bt])
            self.TT("dve", y, y, kf, ALU.add, R=[Bt], WA=[Bt])
            self.ACT(dst[:, idx], y, AF.Sin, R=[Bt], WA=[Bd])

    def emit_rstd(self, out, in_, scale, R, W):
        self.ACT(out, in_, AF.Ln, R=list(R) + [self.B("consts")], W=W, scale=scale, bias=self.small[:, 0:1])
        self.ACT(out, out, AF.Exp, R=W, WA=W, scale=-0.5)

    def emit_norm(self, gcol):
        cB = self.B("consts")
        junk = self.carve([D], BF16)
        xn = [self.carve([D], BF16) for _ in range(2)]
        ss = self.carve([4], F32)
        gb = gcol.unsqueeze(2).to_broadcast([128, 8, 128])
        for t in range(NT):
            p = t % 2
            BXt = self.B(f"X{t}")
            Bss = self.B(f"n_ss{p}")
            Bxn = self.B(f"n_xn{p}")
            Bpt = self.B(f"bank{4 + p}")
            self.ACT(junk, self.X[:, t, :], AF.Square, R=[BXt], W=[Bss], WA=[self.B("n_junk")], accum=ss[:, p:p + 1])
            self.emit_rstd(ss[:, 2 + p:3 + p], ss[:, p:p + 1], 1.0 / D, R=[Bss], W=[self.B(f"n_rs{p}")])
            self.ACT(xn[p], self.X[:, t, :], AF.Copy, R=[BXt, self.B(f"n_rs{p}")], W=[Bxn], scale=ss[:, 2 + p:3 + p])
            pt = self.pbank(4 + p, BF16).rearrange("p (c t) -> p c t", t=128)
            for c in range(8):
                self.TR(pt[:, c, :], xn[p][:, c * 128:(c + 1) * 128], R=[Bxn], W=[Bpt] if c == 0 else (), WA=() if c == 0 else [Bpt])
            self.TT("dve", self.HT[:, :, t * 128:(t + 1) * 128], pt, gb, ALU.mult, R=[Bpt, cB], W=[self.B(f"HT{t}")])

    def emit_attn(self, li):
        j = li // 2
        lambda_init = 0.8 - 0.6 * math.exp(-0.3 * li)
        cB = self.B("consts")
        X, HT = self.X, self.HT
        WA = [self.carve([8, 768], BF16) for _ in range(2)]
        WB = [self.carve([2, D], BF16) for _ in range(2)]
        qT = self.carve([2, S], BF16)
        kT = self.carve([2, S], BF16)
        V = self.carve([NT, 2, 129], BF16)
        onT = self.carve([2, S], BF16)
        Pt = [[self.carve([512], BF16) for _ in range(2)] for _ in range(2)]
        qkf = [self.carve([512], F32) for _ in range(2)]
        qkb = [self.carve([512], BF16) for _ in range(2)]
        rt = [self.carve([8, 8], F32) for _ in range(4)]
        of = [self.carve([128], F32) for _ in range(2)]
        onb = [self.carve([128], BF16) for _ in range(2)]
        junk2 = self.carve([128], F32)
        sm = self.carve([32], F32)
        lamj = self.carve([64], F32)
        Bl = self.B("a_lam")
        li0 = self.lamin[:, j * 256:(j + 1) * 256]
        self.STT(lamj, li0[:, 0:64], 1.0, li0[:, 64:128], ALU.mult, ALU.mult, R=[cB], W=[Bl], accum=sm[:, 0:1])
        self.STT(lamj, li0[:, 128:192], 1.0, li0[:, 192:256], ALU.mult, ALU.mult, R=[cB], WA=[Bl], accum=sm[:, 1:2])
        self.ACT(sm[:, 2:4], sm[:, 0:2], AF.Exp, R=[Bl], WA=[Bl])
        self.TT("dve", sm[:, 4:5], sm[:, 3:4], sm[:, 2:3], ALU.subtract, R=[Bl], WA=[Bl])
        self.TS("dve", sm[:, 5:6], sm[:, 4:5], -lambda_init, None, ALU.add, None, R=[Bl], WA=[Bl])
        neglam = sm[:, 5:6]
        self.TS("dve", sm[:, 6:7], self.subln[:, j:j + 1], 1.0 - lambda_init, None, ALU.mult, None, R=[cB], WA=[Bl])
        sgcol = sm[:, 6:7]
        self.MEMSET("pool", V[:, :, :, 128:129], 1.0, W=[self.B("a_Vones")])

        self.emit_norm(self.nmg[:, li * 8:(li + 1) * 8])

        wq = self.a_wqkv[self.attn_js.index(j)]
        wo = self.a_wo[self.attn_js.index(j)]

        def load_w(g):
            sl = g % 2
            Bw = self.B(f"a_WA{sl}")
            for blk in range(3):
                src = wq[:, blk * D + g * 256: blk * D + (g + 1) * 256].rearrange("(k p) c -> p k c", p=128)
                self.DMA("pool", WA[sl][:, :, blk * 256:(blk + 1) * 256], src, f"a_WA{sl}", W=[Bw] if blk == 0 else (), WA=() if blk == 0 else [Bw])
            src = wo[g * 256:(g + 1) * 256, :].rearrange("(k p) c -> p k c", p=128)
            self.DMA("pool", WB[sl], src, f"a_WB{sl}", W=[self.B(f"a_WB{sl}")])

        acos = self.acs[:, 0]
        asin = self.acs[:, 1]
        load_w(0)
        for g in range(4):
            sl = g % 2
            if g + 1 < 4:
                load_w(g + 1)
            Bw = self.B(f"a_WA{sl}")
            Bwo = self.B(f"a_WB{sl}")
            for t in range(NT):
                p = t % 2
                tsl = slice(t * 128, (t + 1) * 128)
                BHt = self.B(f"HT{t}")
                ps_qk = self.pbank(2 * p)
                ps_v = self.pbank(2 * p + 1)[:, 0:256]
                Bqk = self.B(f"bank{2 * p}")
                Bv = self.B(f"bank{2 * p + 1}")
                for k in range(8):
                    self.MM(ps_qk, HT[:, k, tsl], WA[sl][:, k, 0:512], k == 0, k == 7, R=[BHt, Bw], W=[Bqk] if k == 0 else (), WA=() if k == 0 else [Bqk])
                for k in range(8):
                    self.MM(ps_v, HT[:, k, tsl], WA[sl][:, k, 512:768], k == 0, k == 7, R=[BHt, Bw], W=[Bv] if k == 0 else (), WA=() if k == 0 else [Bv])
                BVt = self.B(f"a_V{t}")
                self.CP("act", V[:, t, :, 0:128], ps_v.rearrange("p (h e) -> p h e", e=128), R=[Bv, self.B("a_Vones")], W=[BVt])
                Bf = self.B(f"a_qkf{p}")
                self.ACT(qkf[p][:, 0:256], ps_qk[:, 0:256], AF.Copy, R=[Bqk], W=[Bf], scale=0.125)
                self.CP("act", qkf[p][:, 256:512], ps_qk[:, 256:512], R=[Bqk], WA=[Bf])
                v3 = qkf[p].rearrange("p (g d) -> p g d", d=64)
                x1 = v3[:, :, 0:8]
                x2 = v3[:, :, 8:16]
                cb_ = acos[:, t, :].unsqueeze(1).to_broadcast([128, 8, 8])
                sb_ = asin[:, t, :].unsqueeze(1).to_broadcast([128, 8, 8])
                Brt = self.B("a_rt")
                Bacs = self.B("acs")
                self.TT("dve", rt[0], x1, cb_, ALU.mult, R=[Bf, Bacs], W=[Brt])
                self.TT("dve", rt[1], x2, sb_, ALU.mult, R=[Bf, Bacs], WA=[Brt])
                self.TT("dve", rt[2], x2, cb_, ALU.mult, R=[Bf, Bacs], WA=[Brt])
                self.TT("dve", rt[3], x1, sb_, ALU.mult, R=[Bf, Bacs], WA=[Brt])
                self.TT("dve", x1, rt[0], rt[1], ALU.subtract, R=[Brt], WA=[Bf])
                self.TT("dve", x2, rt[2], rt[3], ALU.add, R=[Brt], WA=[Bf])
                Bb = self.B(f"a_qkb{p}")
                self.CP("pool", qkb[p], qkf[p], R=[Bf], W=[Bb])
                ptb = self.pbank(7, BF16)[:, 0:512].rearrange("p (i t) -> p i t", t=128)
                Bpt = self.B("bank7")
                for i in range(4):
                    self.TR(ptb[:, i, :], qkb[p][:, i * 128:(i + 1) * 128], R=[Bb], W=[Bpt] if i == 0 else (), WA=() if i == 0 else [Bpt])
                self.CP("dve", qT[:, :, tsl], ptb[:, 0:2, :], R=[Bpt], W=[self.B(f"a_qT{t}")])
                self.CP("act", kT[:, :, tsl], ptb[:, 2:4, :], R=[Bpt], W=[self.B(f"a_kT{t}")])
            it = 0
            for hl in range(2):
                for qt in range(4):
                    started = [False, False, False]
                    Bacc = [self.B(f"bank{4 + b}") for b in range(3)]
                    BqTs = [self.B(f"a_qT{4 * qt + i}") for i in range(4)]
                    nkb = 4 * qt + 4

                    def acc_ap(c, qb):
                        a = c * 4 + qb
                        return self.pbank(4 + a // 3)[:, (a % 3) * 129:(a % 3) * 129 + 129], a // 3

                    for kb in range(nkb):
                        jd = kb - 4 * qt
                        c0 = max(jd, 0) * 128
                        par = it % 2
                        it += 1
                        for c in range(2):
                            Bs = self.B(f"bank{2 * par + c}")
                            self.MM(self.pbank(2 * par + c)[:, c0:512], kT[c * 64:(c + 1) * 64, hl, kb * 128:(kb + 1) * 128],
                                    qT[c * 64:(c + 1) * 64, hl, qt * 512 + c0:(qt + 1) * 512], True, True,
                                    R=[self.B(f"a_kT{kb}")] + BqTs, W=[Bs])
                        for c in range(2):
                            Bs = self.B(f"bank{2 * par + c}")
                            Bp = self.B(f"a_P{par}{c}")
                            self.ACT(Pt[par][c][:, c0:512], self.pbank(2 * par + c)[:, c0:512], AF.Exp, R=[Bs], W=[Bp])
                            if jd >= 0:
                                self.TT("pool", Pt[par][c][:, c0:c0 + 128], Pt[par][c][:, c0:c0 + 128], self.cmask, ALU.mult, R=[Bp, cB], WA=[Bp])
                        for c in range(2):
                            Bp = self.B(f"a_P{par}{c}")
                            for qb in range(max(jd, 0), 4):
                                ap_, b = acc_ap(c, qb)
                                st = not started[b]
                                started[b] = True
                                self.MM(ap_, Pt[par][c][:, qb * 128:(qb + 1) * 128], V[:, kb, hl, :], st, kb == 4 * qt + qb,
                                        R=[Bp, self.B(f"a_V{kb}")], W=[Bacc[b]] if st else (), WA=() if st else [Bacc[b]])
                    for qb in range(4):
                        t = 4 * qt + qb
                        p = qb % 2
                        a1, b1 = acc_ap(0, qb)
                        a2, b2 = acc_ap(1, qb)
                        Bsm = self.B(f"a_sm{p}")
                        o_ = 8 + p * 8
                        self.P.op("dve", (lambda a1=a1, o_=o_: lambda e: e.reciprocal(out=sm[:, o_:o_ + 1], in_=a1[:, 128:129]))(), R=[Bacc[b1]], W=[Bsm] + self.xb(a1))
                        self.P.op("dve", (lambda a2=a2, o_=o_: lambda e: e.reciprocal(out=sm[:, o_ + 1:o_ + 2], in_=a2[:, 128:129]))(), R=[Bacc[b2]], W=self.xb(a2), WA=[Bsm])
                        self.TT("dve", sm[:, o_ + 1:o_ + 2], sm[:, o_ + 1:o_ + 2], neglam, ALU.mult, R=[Bsm, Bl], WA=[Bsm])
                        Bo = self.B(f"a_of{p}")
                        self.TS("dve", of[p], a1[:, 0:128], sm[:, o_:o_ + 1], None, ALU.mult, None, R=[Bacc[b1], Bsm], W=[Bo])
                        self.STT(of[p], a2[:, 0:128], sm[:, o_ + 1:o_ + 2], of[p], ALU.mult, ALU.add, R=[Bacc[b2], Bsm, Bo], WA=[Bo])
                        self.STT(junk2, of[p], 1.0, of[p], ALU.mult, ALU.mult, R=[Bo], WA=[Bsm], accum=sm[:, o_ + 2:o_ + 3])
                        self.emit_rstd(sm[:, o_ + 3:o_ + 4], sm[:, o_ + 2:o_ + 3], 1.0 / 128, R=[Bsm], W=[self.B(f"a_rs{p}")])
                        Bon = self.B(f"a_on{p}")
                        self.TS("dve", onb[p], of[p], sm[:, o_ + 3:o_ + 4], None, ALU.mult, None, R=[Bo, self.B(f"a_rs{p}")], W=[Bon])
                        pt2 = self.pbank(7, BF16)[:, 512 + p * 128:512 + (p + 1) * 128]
                        Bp2 = self.B(f"bank7b{p}")
                        self.TR(pt2, onb[p], R=[Bon], W=[Bp2])
                        self.TS("dve", onT[:, hl, t * 128:(t + 1) * 128], pt2, sgcol, None, ALU.mult, None, R=[Bp2, Bl], W=[self.B(f"a_onT{hl}_{t}")])
            for t in range(NT):
                p = t % 2
                tsl = slice(t * 128, (t + 1) * 128)
                for dh in range(2):
                    Bk = self.B(f"bank{2 * p + dh}")
                    for hl in range(2):
                        self.MM(self.pbank(2 * p + dh), onT[:, hl, tsl], WB[sl][:, hl, dh * 512:(dh + 1) * 512], hl == 0, hl == 1,
                                R=[self.B(f"a_onT{hl}_{t}"), Bwo], W=[Bk] if hl == 0 else (), WA=() if hl == 0 else [Bk])
                    BXt = self.B(f"X{t}")
                    xs = X[:, t, dh * 512:(dh + 1) * 512]
                    self.TT("dve", xs, xs, self.pbank(2 * p + dh), ALU.add, R=[Bk, BXt], WA=[BXt])

    def emit_ffn(self, li):
        cB = self.B("consts")
        X, HT = self.X, self.HT
        Win = [self.carve([8, 1024], BF16) for _ in range(2)]
        Wout = [self.carve([4, D], BF16) for _ in range(2)]
        gb = [self.carve([516], F32) for _ in range(2)]
        tb = [self.carve([512], F32) for _ in range(2)]
        sl_ = [self.carve([512], F32) for _ in range(2)]
        actT = [self.carve([4, 512], BF16) for _ in range(2)]
        carry = self.carve([4, 2], F32)
        self.emit_norm(self.nfg[:, li * 8:(li + 1) * 8])
        win = self.f_win[self.ffn_ls.index(li)]
        wout = self.f_wout[self.ffn_ls.index(li)]
        groups = [(0, 4), (512, 4), (1024, 4), (1536, 4), (2048, 4), (2560, 2)]
        cw = self.cw[:, li * 66:(li + 1) * 66].rearrange("p (f j) -> p f j", j=3)
        cb = self.cb[:, li * 22:(li + 1) * 22]

        def load_w(gi):
            f0, nch = groups[gi]
            nf = nch * 128
            s_ = gi % 2
            Bw = self.B(f"f_Win{s_}")
            self.DMA("pool", Win[s_][:, :, 0:nf], win[:, f0:f0 + nf].rearrange("(k p) c -> p k c", p=128), f"f_Win{s_}", W=[Bw])
            self.DMA("pool", Win[s_][:, :, 512:512 + nf], win[:, FFN + f0:FFN + f0 + nf].rearrange("(k p) c -> p k c", p=128), f"f_Win{s_}", WA=[Bw])
            self.DMA("pool", Wout[s_][:, 0:nch, :], wout[f0:f0 + nf, :].rearrange("(c p) d -> p c d", p=128), f"f_Wout{s_}", W=[self.B(f"f_Wout{s_}")])

        load_w(0)
        it = 0
        for gi, (f0, nch) in enumerate(groups):
            s_ = gi % 2
            if gi + 1 < len(groups):
                load_w(gi + 1)
            Bw = self.B(f"f_Win{s_}")
            Bwo = self.B(f"f_Wout{s_}")
            for T in range(4):
                ap_ = T % 2
                BHs = [self.B(f"HT{4 * T + i}") for i in range(4)]
                Bact = self.B(f"f_act{ap_}")
                for fc in range(nch):
                    par = it % 2
                    it += 1
                    fi = f0 // 128 + fc
                    psG = self.pbank(2 * par)
                    psU = self.pbank(2 * par + 1)
                    BG = self.B(f"bank{2 * par}")
                    BU = self.B(f"bank{2 * par + 1}")
                    for k in range(8):
                        self.MM(psG, Win[s_][:, k, fc * 128:(fc + 1) * 128], HT[:, k, T * 512:(T + 1) * 512], k == 0, k == 7,
                                R=BHs + [Bw], W=[BG] if k == 0 else (), WA=() if k == 0 else [BG])
                    for k in range(8):
                        self.MM(psU, Win[s_][:, k, 512 + fc * 128:512 + (fc + 1) * 128], HT[:, k, T * 512:(T + 1) * 512], k == 0, k == 7,
                                R=BHs + [Bw], W=[BU] if k == 0 else (), WA=() if k == 0 else [BU])
                    Bgb = self.B(f"f_gb{par}")
                    Bc = self.B(f"f_carry{fc}")
                    if T == 0:
                        self.MEMSET("pool", gb[par][:, 0:2], 0.0, W=[Bgb])
                    else:
                        self.CP("pool", gb[par][:, 0:2], carry[:, fc, :], R=[Bc], W=[Bgb])
                    self.CP("act", gb[par][:, 2:514], psG, R=[BG], WA=[Bgb])
                    self.CP("pool", carry[:, fc, :], gb[par][:, 512:514], R=[Bgb], W=[Bc])
                    Btb = self.B(f"f_tb{par}")
                    self.TS("pool", tb[par], gb[par][:, 2:514], cw[:, fi, 2:3], cb[:, fi:fi + 1], ALU.mult, ALU.add, R=[Bgb, cB], W=[Btb])
                    self.STT(tb[par], gb[par][:, 1:513], cw[:, fi, 1:2], tb[par], ALU.mult, ALU.add, R=[Bgb, cB, Btb], WA=[Btb])
                    self.STT(tb[par], gb[par][:, 0:512], cw[:, fi, 0:1], tb[par], ALU.mult, ALU.add, R=[Bgb, cB, Btb], WA=[Btb])
                    Bsl = self.B(f"f_sl{par}")
                    self.ACT(sl_[par], tb[par], AF.Silu, R=[Btb], W=[Bsl])
                    self.TT("dve", actT[ap_][:, fc, :], sl_[par], psU, ALU.mult, R=[Bsl, BU], W=[Bact] if fc == 0 else (), WA=() if fc == 0 else [Bact])
                for tb_ in range(4):
                    t = 4 * T + tb_
                    p2 = t % 2
                    BXt = self.B(f"X{t}")
                    for dh in range(2):
                        bk = 4 + 2 * p2 + dh
                        Bk = self.B(f"bank{bk}")
                        for fc in range(nch):
                            self.MM(self.pbank(bk), actT[ap_][:, fc, tb_ * 128:(tb_ + 1) * 128], Wout[s_][:, fc, dh * 512:(dh + 1) * 512],
                                    fc == 0, fc == nch - 1, R=[Bact, Bwo], W=[Bk] if fc == 0 else (), WA=() if fc == 0 else [Bk])
                        xs = X[:, t, dh * 512:(dh + 1) * 512]
                        self.TT("dve", xs, xs, self.pbank(bk), ALU.add, R=[Bk, BXt], WA=[BXt])

    def emit_ret(self, li):
        j = li // 2
        cB = self.B("consts")
        X, HT = self.X, self.HT
        WA = self.carve([8, 1536], BF16)
        WB = [self.carve([4, D], BF16) for _ in range(2)]
        St = self.carve([2, 512], F32)
        Sbf = [self.carve([2, 512], BF16) for _ in range(2)]
        rt = [self.carve([2, 128], F32) for _ in range(4)]
        qkr = [self.carve([2, 2, 128], BF16) for _ in range(2)]
        vbf = [self.carve([512], BF16) for _ in range(2)]
        sg = [self.carve([512], F32) for _ in range(2)]
        kd = [self.carve([256], BF16) for _ in range(2)]
        qkT = [self.carve([4, 128], BF16) for _ in range(2)]
        innT = [self.carve([128], BF16) for _ in range(2)]
        gated = [self.carve([512], BF16) for _ in range(2)]
        goT = [self.carve([4, 128], BF16) for _ in range(2)]
        junk = self.carve([512], BF16)
        sm = self.carve([16], F32)
        self.emit_norm(self.nmg[:, li * 8:(li + 1) * 8])
        wq = self.r_wqkvg[self.ret_js.index(j)]
        wo = self.r_wo[self.ret_js.index(j)]
        rcos = self.rcs[:, 0]
        rsin = self.rcs[:, 1]
        Brcs = self.B("rcs")
        gam = [1.0 - 2.0 ** (-5.0 - h) for h in range(4)]

        def load_w(h):
            blocks = [(h * 256, 256, 0), (1024 + h * 256, 256, 256), (2048 + h * 512, 512, 512), (4096 + h * 512, 512, 1024)]
            names = ["r_Wqk", "r_Wqk", "r_Wv", "r_Wg"]
            first = {"r_Wqk": True, "r_Wv": True, "r_Wg": True}
            for (c0, n, d0), nm in zip(blocks, names):
                src = wq[:, c0:c0 + n].rearrange("(k p) c -> p k c", p=128)
                Bw = self.B(nm)
                self.DMA("pool", WA[:, :, d0:d0 + n], src, nm, W=[Bw] if first[nm] else (), WA=() if first[nm] else [Bw])
                first[nm] = False
            s_ = h % 2
            self.DMA("pool", WB[s_], wo[h * 512:(h + 1) * 512, :].rearrange("(c p) d -> p c d", p=128), f"r_WB{s_}", W=[self.B(f"r_WB{s_}")])

        for h in range(4):
            load_w(h)
            s_ = h % 2
            Bwo = self.B(f"r_WB{s_}")
            cd = gam[h] ** 128
            qd = self.rcol[:, h:h + 1]
            qd2 = self.rcol[:, 4 + h:5 + h]
            kdc = self.rcol[:, 8 + h:9 + h]
            maskT = self.rmask[:, h, :]

            def stageA(n):
                p = n % 2
                tsl = slice(n * 128, (n + 1) * 128)
                BHt = self.B(f"HT{n}")
                names = ["r_Wqk", "r_Wv", "r_Wg"]
                for b in range(3):
                    Bk = self.B(f"bank{b}")
                    Bw = self.B(names[b])
                    for k in range(8):
                        self.MM(self.pbank(b), HT[:, k, tsl], WA[:, k, b * 512:(b + 1) * 512], k == 0, k == 7, R=[BHt, Bw],
                                W=[Bk] if k == 0 else (), WA=() if k == 0 else [Bk])
                B0, B1, B2 = self.B("bank0"), self.B("bank1"), self.B("bank2")
                v4 = self.pbank(0).rearrange("p (a i e) -> p a i e", i=128, e=2)
                E = v4[:, :, :, 0]
                O = v4[:, :, :, 1]
                cb_ = rcos[:, n, :].unsqueeze(1).to_broadcast([128, 2, 128])
                sb_ = rsin[:, n, :].unsqueeze(1).to_broadcast([128, 2, 128])
                Brt = self.B("r_rt")
                self.TT("dve", rt[0], E, cb_, ALU.mult, R=[B0, Brcs], W=[Brt])
                self.TT("dve", rt[1], O, sb_, ALU.mult, R=[B0, Brcs], WA=[Brt])
                self.TT("dve", rt[2], O, cb_, ALU.mult, R=[B0, Brcs], WA=[Brt])
                self.TT("dve", rt[3], E, sb_, ALU.mult, R=[B0, Brcs], WA=[Brt])
                Bqr = self.B(f"r_qkr{p}")
                self.TT("dve", qkr[p][:, :, 0, :], rt[0], rt[1], ALU.subtract, R=[Brt], W=[Bqr])
                self.TT("dve", qkr[p][:, :, 1, :], rt[2], rt[3], ALU.add, R=[Brt], WA=[Bqr])
                self.CP("act", vbf[p], self.pbank(1), R=[B1], W=[self.B(f"r_v{p}")])
                self.ACT(sg[p], self.pbank(2), AF.Silu, R=[B2], W=[self.B(f"r_sg{p}")])
                kflat = qkr[p][:, 1].rearrange("p a b -> p (a b)")
                self.ACT(kd[p], kflat, AF.Copy, R=[Bqr, cB], W=[self.B(f"r_kd{p}")], scale=kdc)
                ptb = self.pbank(3, BF16)[:, 0:512].rearrange("p (i t) -> p i t", t=128)
                Bpt = self.B("bank3a")
                qflat = qkr[p].rearrange("p a b c -> p (a b c)")
                for i in range(4):
                    self.TR(ptb[:, i, :], qflat[:, i * 128:(i + 1) * 128], R=[Bqr], W=[Bpt] if i == 0 else (), WA=() if i == 0 else [Bpt])
                self.CP("act", qkT[p], ptb, R=[Bpt], W=[self.B(f"r_qkT{p}")])

            def stageB(n):
                p = n % 2
                BqkT = self.B(f"r_qkT{p}")
                Bv = self.B(f"r_v{p}")
                pin = self.pbank(3)[:, 256:384]
                Bin = self.B("bank3b")
                for dc in range(2):
                    self.MM(pin, qkT[p][:, 2 + dc, :], qkT[p][:, dc, :], dc == 0, dc == 1, R=[BqkT], W=[Bin] if dc == 0 else (), WA=() if dc == 0 else [Bin])
                Bit = self.B(f"r_innT{p}")
                self.TT("dve", innT[p], pin, maskT, ALU.mult, R=[Bin, cB], W=[Bit])
                BO = self.B("bank4")
                pO = self.pbank(4)
                sp_ = (n - 1) % 2
                self.MM(pO, innT[p], vbf[p], True, n == 0, R=[Bit, Bv], W=[BO])
                if n > 0:
                    Bs = self.B(f"r_Sbf{sp_}")
                    for dc in range(2):
                        self.MM(pO, qkT[p][:, dc, :], Sbf[sp_][:, dc, :], False, dc == 1, R=[BqkT, Bs], WA=[BO])
                Bkd = self.B(f"r_kd{p}")
                BSt = self.B("r_St")
                if n < NT - 1:
                    for dc in range(2):
                        Bd = self.B(f"bank{5 + dc}")
                        self.MM(self.pbank(5 + dc), kd[p][:, dc * 128:(dc + 1) * 128], vbf[p], True, True, R=[Bkd, Bv], W=[Bd])
                        if n == 0:
                            self.CP("dve", St[:, dc, :], self.pbank(5 + dc), R=[Bd], W=[BSt] if dc == 0 else (), WA=() if dc == 0 else [BSt])
                        else:
                            self.STT(St[:, dc, :], St[:, dc, :], cd, self.pbank(5 + dc), ALU.mult, ALU.add, R=[Bd, BSt], WA=[BSt])
                    self.CP("pool", Sbf[p], St, R=[BSt], W=[self.B(f"r_Sbf{p}")])
                Bsm = self.B(f"r_sm{p}")
                o_ = p * 8
                self.ACT(junk, pO, AF.Square, R=[BO], W=[Bsm], WA=[self.B("r_junk")], accum=sm[:, o_:o_ + 1])
                self.TT("dve", sm[:, o_ + 1:o_ + 2], sm[:, o_:o_ + 1], qd2, ALU.mult, R=[Bsm, cB], WA=[Bsm])
                self.emit_rstd(sm[:, o_ + 2:o_ + 3], sm[:, o_ + 1:o_ + 2], 1.0, R=[Bsm], W=[self.B(f"r_rs{p}")])
                self.TT("dve", sm[:, o_ + 3:o_ + 4], sm[:, o_ + 2:o_ + 3], qd, ALU.mult, R=[self.B(f"r_rs{p}"), cB], W=[self.B(f"r_rq{p}")])
                Bg = self.B(f"r_gated{p}")
                self.STT(gated[p], pO, sm[:, o_ + 3:o_ + 4], sg[p], ALU.mult, ALU.mult, R=[BO, self.B(f"r_rq{p}"), self.B(f"r_sg{p}")], W=[Bg])
                ptb = self.pbank(7, BF16)[:, 0:512].rearrange("p (i t) -> p i t", t=128)
                Bpt = self.B("bank7")
                for i in range(4):
                    self.TR(ptb[:, i, :], gated[p][:, i * 128:(i + 1) * 128], R=[Bg], W=[Bpt] if i == 0 else (), WA=() if i == 0 else [Bpt])
                BgT = self.B(f"r_goT{p}")
                self.CP("act", goT[p], ptb, R=[Bpt], W=[BgT])
                BXt = self.B(f"X{n}")
                tslx = n
                for dh in range(2):
                    Bk = self.B("bank6") if dh == 0 else self.B("bank5")
                    bk = 6 if dh == 0 else 5
                    if dh == 1:
                        bk = 6
                        Bk = self.B("bank6")
                    for ec in range(4):
                        self.MM(self.pbank(bk), goT[p][:, ec, :], WB[s_][:, ec, dh * 512:(dh + 1) * 512], ec == 0, ec == 3, R=[BgT, Bwo],
                                W=[Bk] if ec == 0 else (), WA=() if ec == 0 else [Bk])
                    xs = X[:, tslx, dh * 512:(dh + 1) * 512]
                    self.TT("dve", xs, xs, self.pbank(bk), ALU.add, R=[Bk, BXt], WA=[BXt])

            stageA(0)
            for n in range(NT):
                if n + 1 < NT:
                    stageA(n + 1)
                stageB(n)

    def emit_out(self, s):
        junk = self.carve([D], BF16)
        ob = [self.carve([D], F32) for _ in range(2)]
        ss = self.carve([4], F32)
        gfull = self.carve([D], F32)
        Bg = self.B("o_g")
        self.DMA("sp", gfull, self.fng_d, "o_g", W=[Bg])
        for t in range(NT):
            p = t % 2
            BXt = self.B(f"X{t}")
            Bob = self.B(f"o_b{p}")
            if self.final_norm:
                Bss = self.B(f"o_ss{p}")
                self.ACT(junk, self.X[:, t, :], AF.Square, R=[BXt], W=[Bss], WA=[self.B("o_junk")], accum=ss[:, p:p + 1])
                self.emit_rstd(ss[:, 2 + p:3 + p], ss[:, p:p + 1], 1.0 / D, R=[Bss], W=[self.B(f"o_rs{p}")])
                self.STT(ob[p], self.X[:, t, :], ss[:, 2 + p:3 + p], gfull, ALU.mult, ALU.mult, R=[BXt, self.B(f"o_rs{p}"), Bg], W=[Bob])
            else:
                self.CP("dve", ob[p], self.X[:, t, :], R=[BXt], W=[Bob])
            self.DMA("sp", self.out_d[s, t * 128:(t + 1) * 128, :], ob[p], "out", R=[Bob], WA=[self.B("out")])


def _consts():
    ident = np.eye(128, dtype=np.float32)
    cm = (np.arange(128)[:, None] <= np.arange(128)[None, :]).astype(np.float32)
    cbf = np.concatenate([ident, cm], axis=1).astype(ml_dtypes.bfloat16)
    afreq = (500000.0 ** (-np.arange(0, 16, 2, dtype=np.float32) / np.float32(16))).astype(np.float32)
    rfreq = (1.0 / (10000.0 ** np.linspace(0.0, 1.0, 128, dtype=np.float32))).astype(np.float32)
    cf = np.zeros((128, 664), np.float32)
    cf[:, 0:8] = afreq[None, :]
    cf[:, 8:136] = rfreq[None, :]
    idx = np.arange(128, dtype=np.float64)
    for h in range(4):
        gam = 1.0 - 2.0 ** (-5.0 - h)
        m = np.where(idx[None, :] >= idx[:, None], (gam ** (-(idx[:, None] + 1.0))) / 16.0, 0.0)
        cf[:, 136 + h * 128:136 + (h + 1) * 128] = m
        qd = gam ** (idx + 1.0)
        cf[:, 648 + h] = qd
        cf[:, 652 + h] = qd * qd / 512.0
        cf[:, 656 + h] = gam ** (127.0 - idx) / 16.0
    return cbf, cf


_NC_CACHE = {}


def _get_nc(layers, nseq, final_norm):
    key = (tuple(layers), nseq, final_norm)
    if key not in _NC_CACHE:
        _NC_CACHE[key] = Builder(list(layers), nseq, final_norm).build()
    return _NC_CACHE[key]


PLAN = [[0, 1, 2, 3]]


def _norm_layers(layers):
    return tuple(e if isinstance(e, tuple) else (e, True, True) for e in layers)


def _run(inputs, layers=None, final_norm=True, plan=None):
    f = lambda a: np.ascontiguousarray(np.asarray(a, dtype=np.float32))
    x = f(inputs["x"])
    pos = np.ascontiguousarray(np.asarray(inputs["positions"], dtype=np.int32))
    cbf, cf = _consts()
    fm = lambda g: np.ascontiguousarray(f(g).reshape(DEPTH, 8, 128).transpose(2, 0, 1).reshape(128, DEPTH * 8))
    lam = np.concatenate([f(inputs["attn_lambda_q1"]), f(inputs["attn_lambda_k1"]),
                          f(inputs["attn_lambda_q2"]), f(inputs["attn_lambda_k2"])], axis=1)
    lam = np.ascontiguousarray(np.broadcast_to(lam.reshape(1, 512), (128, 512)))
    subln = np.ascontiguousarray(f(inputs["attn_subln_g"]).T)
    cw = f(inputs["ffn_conv_w"]).reshape(DEPTH, 3, NFC, 128).transpose(3, 0, 2, 1)
    cw = np.ascontiguousarray(cw.reshape(128, DEPTH * NFC * 3))
    cb = np.ascontiguousarray(f(inputs["ffn_conv_b"]).reshape(DEPTH, NFC, 128).transpose(2, 0, 1).reshape(128, DEPTH * NFC))
    small = dict(
        nmg=fm(inputs["norm_mix_g"]), nfg=fm(inputs["norm_ffn_g"]),
        fng=np.ascontiguousarray(np.broadcast_to(f(inputs["final_norm_g"]).reshape(1, D), (128, D))),
        lam_in=lam, subln=subln, f_cw=cw, f_cb=cb, c_bf=cbf, c_f32=cf,
    )
    if plan is None:
        plan = [list(layers)] if layers is not None else PLAN
    pcs = [np.ascontiguousarray(pos[c * NSEQ:(c + 1) * NSEQ].reshape(NSEQ, NT, 128).transpose(0, 2, 1)) for c in range(N_CORES)]
    cur = x
    for li_, lay in enumerate(plan):
        lay = _norm_layers(lay)
        fn = final_norm and (li_ == len(plan) - 1)
        key = (lay, NSEQ, fn)
        if key not in _NC_CACHE:
            b = Builder(list(lay), NSEQ, fn)
            _NC_CACHE[key] = (b.build(), b)
        nc, b = _NC_CACHE[key]
        shared = dict(small)
        if b.attn_js:
            shared["a_wqkv"] = f(inputs["attn_w_qkv"])[b.attn_js]
            shared["a_wo"] = f(inputs["attn_w_o"])[b.attn_js]
        if b.ret_js:
            shared["r_wqkvg"] = f(inputs["ret_w_qkvg"])[b.ret_js]
            shared["r_wo"] = f(inputs["ret_w_o"])[b.ret_js]
        if b.ffn_ls:
            shared["f_win"] = f(inputs["ffn_w_in"])[b.ffn_ls]
            shared["f_wout"] = f(inputs["ffn_w_out"])[b.ffn_ls]
        in_maps = []
        for c in range(N_CORES):
            m = dict(shared)
            m["x"] = np.ascontiguousarray(cur[c * NSEQ:(c + 1) * NSEQ])
            m["pos"] = pcs[c]
            in_maps.append(m)
        res = run_bass_kernel_spmd(nc, in_maps, core_ids=list(range(N_CORES)))
        cur = np.concatenate([r["out"] for r in res.results], axis=0)
    return cur


def kernel(**inputs):
    return _run(inputs)
```

```python
import math
import numpy as np
import ml_dtypes
import concourse.bass as bass
import concourse.mybir as mybir
from concourse.bass_utils import run_bass_kernel_spmd

dt = mybir.dt
F32, BF16, I32 = dt.float32, dt.bfloat16, dt.int32
AF = mybir.ActivationFunctionType
ALU = mybir.AluOpType
COMPUTE = ("pe", "act", "dve", "pool")

D = 1024
S = 2048
NT = 16
NSEQ = 2
DEPTH = 4
FFN = 2816
NFC = 22
EPS = 1e-6
N_CORES = 8
PI = math.pi
EPOCH = 1024


class Buf:
    __slots__ = ("name", "w", "r")

    def __init__(self, name):
        self.name = name
        self.w = {}
        self.r = {}


class Op:
    __slots__ = ("eng", "fn", "deps", "sig", "val", "key", "is_dma", "order", "epoch")

    def __init__(self, eng, fn, is_dma=False, key=None):
        self.eng = eng
        self.fn = fn
        self.deps = {}
        self.sig = False
        self.val = 0
        self.key = key if key is not None else eng
        self.is_dma = is_dma


class Prog:
    def __init__(self):
        self.streams = {e: [] for e in ("pe", "act", "dve", "pool", "sp")}
        self.dma_cnt = {}
        self.all_ops = []

    def op(self, eng, fn, R=(), W=(), WA=()):
        o = Op(eng, fn)
        self._track(o, R, W, WA)
        return o

    def dma(self, eng, fn, key, R=(), W=(), WA=()):
        o = Op(eng, fn, is_dma=True, key="dma:" + key)
        self.dma_cnt[key] = self.dma_cnt.get(key, 0) + 1
        o.val = 16 * self.dma_cnt[key]
        o.sig = True
        self._track(o, R, W, WA)
        return o

    def _track(self, o, R, W, WA):
        o.order = len(self.all_ops)
        self.all_ops.append(o)
        self.streams[o.eng].append(o)
        for b in R:
            for s in b.w.values():
                self._add(o, s, True)
        for b in list(W) + list(WA):
            for s in b.w.values():
                self._add(o, s, False)
            for s in b.r.values():
                self._add(o, s, False)
        for b in R:
            b.r[o.key] = o
        for b in W:
            b.w = {o.key: o}
            b.r = {}
        for b in WA:
            b.w[o.key] = o

    def _add(self, o, s, raw):
        if s is o:
            return
        if (not s.is_dma) and (not o.is_dma) and s.eng == o.eng:
            if not raw or s.eng == "pe":
                return
        cur = o.deps.get(s.key)
        if cur is None or cur.order < s.order:
            o.deps[s.key] = s

    def finalize(self):
        for o in self.all_ops:
            for s in o.deps.values():
                s.sig = True
        cnt = {e: 0 for e in COMPUTE}
        for o in self.all_ops:
            if not o.is_dma and o.sig:
                cnt[o.eng] += 1
                o.epoch = (cnt[o.eng] - 1) // EPOCH
                o.val = (cnt[o.eng] - 1) % EPOCH + 1
        self.n_epochs = {e: (cnt[e] + EPOCH - 1) // EPOCH for e in COMPUTE}

    def replay(self, name, eng, sems, dma_sems):
        waited = {}
        for o in self.streams[name]:
            for k, s in o.deps.items():
                if s.is_dma:
                    if waited.get(k, 0) < s.val:
                        eng.wait_ge(dma_sems[k[4:]], s.val)
                        waited[k] = s.val
                else:
                    tv = (s.epoch, s.val)
                    if waited.get(k, (-1, 0)) < tv:
                        eng.wait_ge(sems[(s.eng, s.epoch)], s.val)
                        waited[k] = tv
            if o.fn is None:
                continue
            ins = o.fn(eng)
            if o.is_dma:
                ins.then_inc(dma_sems[o.key[4:]], 16)
            elif o.sig:
                ins.then_inc(sems[(o.eng, o.epoch)], 1)


class Builder:
    def __init__(self, layers, nseq, final_norm):
        self.layers = layers
        self.nseq = nseq
        self.final_norm = final_norm
        self.P = Prog()
        self.nc = bass.Bass("TRN2", target_bir_lowering=False)
        self.bufs = {}

    def B(self, name):
        b = self.bufs.get(name)
        if b is None:
            b = self.bufs[name] = Buf(name)
        return b

    def Bs(self, names):
        return [self.B(n) for n in names]

    def xb(self, *aps):
        out = []
        for a in aps:
            try:
                if a.tensor.name != "psum":
                    continue
            except AttributeError:
                continue
            es = 4 if a.dtype in (F32, I32) else 2
            b = (a.offset * es) // 2048
            bb = self.B(f"xbank{b}")
            if bb not in out:
                out.append(bb)
        return out

    def dram_in(self, name, shape, d=F32):
        return self.nc.dram_tensor(name, list(shape), d, kind="ExternalInput").ap()

    def MM(self, out, lhsT, rhs, start, stop, R, W=(), WA=()):
        self.P.op("pe", lambda e: e.matmul(out, lhsT=lhsT, rhs=rhs, start=start, stop=stop,
                                           skip_group_check=True), R=R, W=list(W) + self.xb(out), WA=WA)

    def TR(self, out, in_, R, W=(), WA=()):
        ident = self.ident
        self.P.op("pe", lambda e: e.transpose(out=out, in_=in_, identity=ident), R=list(R) + [self.B("consts")], W=list(W) + self.xb(out), WA=WA)

    def ACT(self, out, in_, func, R, W=(), WA=(), scale=None, bias=None, accum=None):
        kw = {}
        if scale is not None:
            kw["scale"] = scale
        if bias is not None:
            kw["bias"] = bias
        if accum is not None:
            kw["accum_out"] = accum
        self.P.op("act", lambda e: e.activation(out=out, in_=in_, func=func, **kw), R=R, W=list(W) + self.xb(out, in_), WA=WA)

    def TT(self, eng, out, in0, in1, op, R, W=(), WA=()):
        self.P.op(eng, lambda e: e.tensor_tensor(out=out, in0=in0, in1=in1, op=op), R=R, W=list(W) + self.xb(out, in0, in1), WA=WA)

    def TS(self, eng, out, in0, s1, s2, op0, op1, R, W=(), WA=()):
        if op1 is None:
            self.P.op(eng, lambda e: e.tensor_scalar(out=out, in0=in0, scalar1=s1, scalar2=None, op0=op0), R=R, W=list(W) + self.xb(out, in0), WA=WA)
        else:
            self.P.op(eng, lambda e: e.tensor_scalar(out=out, in0=in0, scalar1=s1, scalar2=s2, op0=op0, op1=op1), R=R, W=list(W) + self.xb(out, in0), WA=WA)

    def STT(self, out, in0, scalar, in1, op0, op1, R, W=(), WA=(), accum=None):
        if accum is None:
            self.P.op("dve", lambda e: e.scalar_tensor_tensor(out=out, in0=in0, scalar=scalar, in1=in1, op0=op0, op1=op1), R=R, W=list(W) + self.xb(out, in0, in1), WA=WA)
        else:
            self.P.op("dve", lambda e: e.scalar_tensor_tensor(out=out, in0=in0, scalar=scalar, in1=in1, op0=op0, op1=op1, accum_out=accum), R=R, W=list(W) + self.xb(out, in0, in1), WA=WA)

    def CP(self, eng, out, in_, R, W=(), WA=()):
        if eng == "act":
            self.P.op("act", lambda e: e.copy(out=out, in_=in_), R=R, W=list(W) + self.xb(out, in_), WA=WA)
        else:
            self.P.op(eng, lambda e: e.tensor_copy(out=out, in_=in_), R=R, W=list(W) + self.xb(out, in_), WA=WA)

    def MEMSET(self, eng, ap, val, W=(), WA=()):
        self.P.op(eng, lambda e: e.memset(ap, val), W=W, WA=WA)

    def DMA(self, eng, out, in_, key, R=(), W=(), WA=()):
        self.P.dma(eng, lambda e: e.dma_start(out=out, in_=in_), key, R=R, W=W, WA=WA)

    def barrier(self):
        allb = list(self.bufs.values())
        P = self.P
        for e in ("pe", "act", "dve", "pool", "sp"):
            o = Op(e, None)
            o.order = len(P.all_ops)
            for b in allb:
                for src in list(b.w.values()) + list(b.r.values()):
                    if src.fn is None:
                        continue
                    if (not src.is_dma) and src.eng == e:
                        continue
                    cur = o.deps.get(src.key)
                    if cur is None or cur.order < src.order:
                        o.deps[src.key] = src
            P.all_ops.append(o)
            P.streams[e].append(o)

    def arena_reset(self):
        self.aoff = 0

    def carve(self, shape, d):
        n = 1
        for v in shape:
            n *= v
        nbytes = n * (4 if d in (F32, I32) else 2)
        nbytes = (nbytes + 31) // 32 * 32
        o2 = self.aoff // 2
        assert self.aoff + nbytes <= self.arena_bytes, (self.aoff, nbytes, self.arena_bytes)
        v = self.arena[:, o2:o2 + nbytes // 2]
        self.aoff += nbytes
        if d != BF16:
            v = v.bitcast(d)
        v = v[:, 0:n]
        if len(shape) == 2:
            v = v.rearrange("p (a b) -> p a b", b=shape[1])
        elif len(shape) == 3:
            v = v.rearrange("p (a b c) -> p a b c", b=shape[1], c=shape[2])
        return v

    def pbank(self, i, d=F32):
        v = self.psum[:, i * 512:(i + 1) * 512]
        if d == BF16:
            v = v.bitcast(BF16)
        return v

    def build(self):
        nc = self.nc
        ns = self.nseq
        self.x_d = self.dram_in("x", [ns, S, D])
        self.pos_d = self.dram_in("pos", [ns, 128, NT], I32)
        self.nmg_d = self.dram_in("nmg", [128, DEPTH * 8])
        self.nfg_d = self.dram_in("nfg", [128, DEPTH * 8])
        self.fng_d = self.dram_in("fng", [128, D])
        self.lam_d = self.dram_in("lam_in", [128, 2 * 256])
        self.subln_d = self.dram_in("subln", [128, 2])
        self.cw_d = self.dram_in("f_cw", [128, DEPTH * NFC * 3])
        self.cb_d = self.dram_in("f_cb", [128, DEPTH * NFC])
        ents = [e if isinstance(e, tuple) else (e, True, True) for e in self.layers]
        self.attn_js = sorted({li // 2 for li, m, f in ents if m and li % 2 == 0})
        self.ret_js = sorted({li // 2 for li, m, f in ents if m and li % 2 == 1})
        self.ffn_ls = sorted({li for li, m, f in ents if f})
        if self.attn_js:
            self.a_wqkv = self.dram_in("a_wqkv", [len(self.attn_js), D, 3 * D])
            self.a_wo = self.dram_in("a_wo", [len(self.attn_js), D, D])
        if self.ret_js:
            self.r_wqkvg = self.dram_in("r_wqkvg", [len(self.ret_js), D, 6144])
            self.r_wo = self.dram_in("r_wo", [len(self.ret_js), 2048, D])
        if self.ffn_ls:
            self.f_win = self.dram_in("f_win", [len(self.ffn_ls), D, 2 * FFN])
            self.f_wout = self.dram_in("f_wout", [len(self.ffn_ls), FFN, D])
        self.cbf_d = self.dram_in("c_bf", [128, 256], BF16)
        self.cf_d = self.dram_in("c_f32", [128, 8 + 128 + 512 + 16])
        self.out_d = nc.dram_tensor("out", [ns, S, D], F32, kind="ExternalOutput").ap()

        self.X = nc.alloc_sbuf_tensor("X", [128, NT, D], F32)[:]
        self.HT = nc.alloc_sbuf_tensor("HT", [128, 8, S], BF16)[:]
        self.cbf = nc.alloc_sbuf_tensor("cbf", [128, 256], BF16)[:]
        self.cf = nc.alloc_sbuf_tensor("cf", [128, 664], F32)[:]
        self.ident = self.cbf[:, 0:128]
        self.cmask = self.cbf[:, 128:256]
        self.afreq = self.cf[:, 0:8]
        self.rfreq = self.cf[:, 8:136]
        self.rmask = self.cf[:, 136:648].rearrange("p (h i) -> p h i", i=128)
        self.rcol = self.cf[:, 648:664]
        self.params = nc.alloc_sbuf_tensor("params", [128, 32 + 32 + 512 + 2 + 264 + 88], F32)[:]
        o = 0
        self.nmg = self.params[:, o:o + 32]; o += 32
        self.nfg = self.params[:, o:o + 32]; o += 32
        self.lamin = self.params[:, o:o + 512]; o += 512
        self.subln = self.params[:, o:o + 2]; o += 2
        self.cw = self.params[:, o:o + 264]; o += 264
        self.cb = self.params[:, o:o + 88]; o += 88
        self.rcs = nc.alloc_sbuf_tensor("rcs", [128, 2, NT, 128], F32)[:]
        self.acs = nc.alloc_sbuf_tensor("acs", [128, 2, NT, 8], F32)[:]
        self.posf = nc.alloc_sbuf_tensor("posf", [128, NT], F32)[:]
        self.posi = nc.alloc_sbuf_tensor("posi", [128, NT], I32)[:]
        self.small = nc.alloc_sbuf_tensor("small", [128, 64], F32)[:]
        self.arena_bytes = (nc.sbuf_bytes_remaining - 256) // 64 * 64
        self.arena = nc.alloc_sbuf_tensor("arena", [128, self.arena_bytes // 2], BF16)[:]
        self.psum = nc.alloc_psum_tensor("psum", [128, 4096], F32)[:]

        cB = self.B("consts")
        self.DMA("sp", self.cbf, self.cbf_d, "consts", W=[cB])
        self.DMA("sp", self.cf, self.cf_d, "consts", WA=[cB])
        self.DMA("sp", self.nmg, self.nmg_d, "consts", WA=[cB])
        self.DMA("sp", self.nfg, self.nfg_d, "consts", WA=[cB])
        self.DMA("sp", self.lamin, self.lam_d, "consts", WA=[cB])
        self.DMA("sp", self.subln, self.subln_d, "consts", WA=[cB])
        self.DMA("sp", self.cw, self.cw_d, "consts", WA=[cB])
        self.DMA("sp", self.cb, self.cb_d, "consts", WA=[cB])
        self.MEMSET("dve", self.small[:, 0:1], EPS, WA=[cB])

        for s in range(ns):
            self.emit_seq(s)
        self.P.op("sp", None, R=[self.B("out")])
        self.P.finalize()

        from contextlib import ExitStack
        with ExitStack() as es:
            sems = {(e, k): es.enter_context(nc.semaphore(f"s_{e}{k}")) for e in COMPUTE for k in range(self.P.n_epochs[e])}
            dsems = {k: es.enter_context(nc.semaphore("d_" + k)) for k in self.P.dma_cnt}
            P = self.P
            with nc.Block() as block:
                @block.tensor
                def _(e):
                    P.replay("pe", e, sems, dsems)

                @block.scalar
                def _(e):
                    P.replay("act", e, sems, dsems)

                @block.vector
                def _(e):
                    P.replay("dve", e, sems, dsems)

                @block.gpsimd
                def _(e):
                    P.replay("pool", e, sems, dsems)

                @block.sync
                def _(e):
                    P.replay("sp", e, sems, dsems)
        return nc

    def emit_seq(self, s):
        BX = [self.B(f"X{t}") for t in range(NT)]
        self.barrier()
        for t in range(NT):
            self.DMA("sp", self.X[:, t, :], self.x_d[s, t * 128:(t + 1) * 128, :], f"X{t}", W=[BX[t]])
        self.DMA("sp", self.posi, self.pos_d[s], "pos", W=[self.B("posi")])
        self.CP("dve", self.posf, self.posi, R=[self.B("posi")], W=[self.B("posf")])
        self.arena_reset()
        self.emit_sincos(self.afreq, 8, self.acs, "acs")
        self.arena_reset()
        self.emit_sincos(self.rfreq, 128, self.rcs, "rcs")
        for ent in self.layers:
            li, do_mix, do_ffn = ent if isinstance(ent, tuple) else (ent, True, True)
            if do_mix:
                self.barrier()
                self.arena_reset()
                if li % 2 == 0:
                    self.emit_attn(li)
                else:
                    self.emit_ret(li)
            if do_ffn:
                self.barrier()
                self.arena_reset()
                self.emit_ffn(li)
        self.barrier()
        self.arena_reset()
        self.emit_out(s)

    def emit_sincos(self, freq, F, dst, name):
        cB = self.B("consts")
        Bt = self.B("sc_tmp")
        Bd = self.B(name)
        ang = self.carve([NT, F], F32)
        ki = self.carve([NT, F], I32)
        kf = self.carve([NT, F], F32)
        y = self.carve([NT, F], F32)
        fb = freq.unsqueeze(1).to_broadcast([128, NT, F])
        pb = self.posf.unsqueeze(2).to_broadcast([128, NT, F])
        self.TT("dve", ang, fb, pb, ALU.mult, R=[cB, self.B("posf")], W=[Bt])
        self.TS("dve", ki, ang, 1.0 / (2 * PI), None, ALU.mult, None, R=[Bt], WA=[Bt])
        self.CP("dve", kf, ki, R=[Bt], WA=[Bt])
        self.STT(ang, kf, -2 * PI, ang, ALU.mult, ALU.add, R=[Bt], WA=[Bt])
        for idx, shift in ((1, 0.0), (0, PI / 2)):
            self.TS("dve", y, ang, shift, None, ALU.add, None, R=[Bt], WA=[Bt])
            self.TS("dve", kf, y, PI, -2 * PI, ALU.is_gt, ALU.mult, R=[Bt], WA=[Bt])
            self.TT("dve", y, y, kf, ALU.add, R=[Bt], WA=[Bt])
            self.TS("dve", kf, y, -PI, 2 * PI, ALU.is_lt, ALU.mult, R=[Bt], WA=[Bt])
            self.TT("dve", y, y, kf, ALU.add, R=[Bt], WA=[Bt])
            self.ACT(dst[:, idx], y, AF.Sin, R=[Bt], WA=[Bd])

    def emit_rstd(self, out, in_, scale, R, W):
        self.ACT(out, in_, AF.Ln, R=list(R) + [self.B("consts")], W=W, scale=scale, bias=self.small[:, 0:1])
        self.ACT(out, out, AF.Exp, R=W, WA=W, scale=-0.5)

    def emit_norm(self, gcol):
        cB = self.B("consts")
        junk = self.carve([D], BF16)
        xn = [self.carve([D], BF16) for _ in range(2)]
        ss = self.carve([4], F32)
        gb = gcol.unsqueeze(2).to_broadcast([128, 8, 128])
        for t in range(NT):
            p = t % 2
            BXt = self.B(f"X{t}")
            Bss = self.B(f"n_ss{p}")
            Bxn = self.B(f"n_xn{p}")
            Bpt = self.B(f"bank{4 + p}")
            self.ACT(junk, self.X[:, t, :], AF.Square, R=[BXt], W=[Bss], WA=[self.B("n_junk")], accum=ss[:, p:p + 1])
            self.emit_rstd(ss[:, 2 + p:3 + p], ss[:, p:p + 1], 1.0 / D, R=[Bss], W=[self.B(f"n_rs{p}")])
            self.ACT(xn[p], self.X[:, t, :], AF.Copy, R=[BXt, self.B(f"n_rs{p}")], W=[Bxn], scale=ss[:, 2 + p:3 + p])
            pt = self.pbank(4 + p, BF16).rearrange("p (c t) -> p c t", t=128)
            for c in range(8):
                self.TR(pt[:, c, :], xn[p][:, c * 128:(c + 1) * 128], R=[Bxn], W=[Bpt] if c == 0 else (), WA=() if c == 0 else [Bpt])
            self.TT("dve", self.HT[:, :, t * 128:(t + 1) * 128], pt, gb, ALU.mult, R=[Bpt, cB], W=[self.B(f"HT{t}")])

    def emit_attn(self, li):
        j = li // 2
        lambda_init = 0.8 - 0.6 * math.exp(-0.3 * li)
        cB = self.B("consts")
        X, HT = self.X, self.HT
        WA = [self.carve([8, 768], BF16) for _ in range(2)]
        WB = [self.carve([2, D], BF16) for _ in range(2)]
        qT = self.carve([2, S], BF16)
        kT = self.carve([2, S], BF16)
        V = self.carve([NT, 2, 129], BF16)
        onT = self.carve([2, S], BF16)
        Pt = [[self.carve([512], BF16) for _ in range(2)] for _ in range(2)]
        qkf = [self.carve([512], F32) for _ in range(2)]
        qkb = [self.carve([512], BF16) for _ in range(2)]
        rt = [self.carve([8, 8], F32) for _ in range(4)]
        of = [self.carve([128], F32) for _ in range(2)]
        onb = [self.carve([128], BF16) for _ in range(2)]
        junk2 = self.carve([128], F32)
        sm = self.carve([32], F32)
        lamj = self.carve([64], F32)
        Bl = self.B("a_lam")
        li0 = self.lamin[:, j * 256:(j + 1) * 256]
        self.STT(lamj, li0[:, 0:64], 1.0, li0[:, 64:128], ALU.mult, ALU.mult, R=[cB], W=[Bl], accum=sm[:, 0:1])
        self.STT(lamj, li0[:, 128:192], 1.0, li0[:, 192:256], ALU.mult, ALU.mult, R=[cB], WA=[Bl], accum=sm[:, 1:2])
        self.ACT(sm[:, 2:4], sm[:, 0:2], AF.Exp, R=[Bl], WA=[Bl])
        self.TT("dve", sm[:, 4:5], sm[:, 3:4], sm[:, 2:3], ALU.subtract, R=[Bl], WA=[Bl])
        self.TS("dve", sm[:, 5:6], sm[:, 4:5], -lambda_init, None, ALU.add, None, R=[Bl], WA=[Bl])
        neglam = sm[:, 5:6]
        self.TS("dve", sm[:, 6:7], self.subln[:, j:j + 1], 1.0 - lambda_init, None, ALU.mult, None, R=[cB], WA=[Bl])
        sgcol = sm[:, 6:7]
        self.MEMSET("pool", V[:, :, :, 128:129], 1.0, W=[self.B("a_Vones")])

        mark = self.aoff
        self.emit_norm(self.nmg[:, li * 8:(li + 1) * 8])
        self.aoff = mark
        accs = self.carve([3, 387], F32)

        wq = self.a_wqkv[self.attn_js.index(j)]
        wo = self.a_wo[self.attn_js.index(j)]

        def load_w(g):
            sl = g % 2
            Bw = self.B(f"a_WA{sl}")
            for blk in range(3):
                src = wq[:, blk * D + g * 256: blk * D + (g + 1) * 256].rearrange("(k p) c -> p k c", p=128)
                self.DMA("pool", WA[sl][:, :, blk * 256:(blk + 1) * 256], src, f"a_WA{sl}", W=[Bw] if blk == 0 else (), WA=() if blk == 0 else [Bw])
            src = wo[g * 256:(g + 1) * 256, :].rearrange("(k p) c -> p k c", p=128)
            self.DMA("pool", WB[sl], src, f"a_WB{sl}", W=[self.B(f"a_WB{sl}")])

        acos = self.acs[:, 0]
        asin = self.acs[:, 1]
        load_w(0)
        for g in range(4):
            sl = g % 2
            if g + 1 < 4:
                load_w(g + 1)
            Bw = self.B(f"a_WA{sl}")
            Bwo = self.B(f"a_WB{sl}")
            def p1_mm(t):
                p = t % 2
                tsl = slice(t * 128, (t + 1) * 128)
                BHt = self.B(f"HT{t}")
                ps_qk = self.pbank(2 * p)
                ps_v = self.pbank(2 * p + 1)[:, 0:256]
                Bqk = self.B(f"bank{2 * p}")
                Bv = self.B(f"bank{2 * p + 1}")
                for k in range(8):
                    self.MM(ps_qk, HT[:, k, tsl], WA[sl][:, k, 0:512], k == 0, k == 7, R=[BHt, Bw], W=[Bqk] if k == 0 else (), WA=() if k == 0 else [Bqk])
                for k in range(8):
                    self.MM(ps_v, HT[:, k, tsl], WA[sl][:, k, 512:768], k == 0, k == 7, R=[BHt, Bw], W=[Bv] if k == 0 else (), WA=() if k == 0 else [Bv])

            def p1_post(t):
                p = t % 2
                tsl = slice(t * 128, (t + 1) * 128)
                ps_qk = self.pbank(2 * p)
                ps_v = self.pbank(2 * p + 1)[:, 0:256]
                Bqk = self.B(f"bank{2 * p}")
                Bv = self.B(f"bank{2 * p + 1}")
                BVt = self.B(f"a_V{t}")
                self.CP("act", V[:, t, :, 0:128], ps_v.rearrange("p (h e) -> p h e", e=128), R=[Bv, self.B("a_Vones")], W=[BVt])
                Bf = self.B(f"a_qkf{p}")
                self.ACT(qkf[p][:, 0:256], ps_qk[:, 0:256], AF.Copy, R=[Bqk], W=[Bf], scale=0.125)
                self.CP("act", qkf[p][:, 256:512], ps_qk[:, 256:512], R=[Bqk], WA=[Bf])
                v3 = qkf[p].rearrange("p (g d) -> p g d", d=64)
                x1 = v3[:, :, 0:8]
                x2 = v3[:, :, 8:16]
                cb_ = acos[:, t, :].unsqueeze(1).to_broadcast([128, 8, 8])
                sb_ = asin[:, t, :].unsqueeze(1).to_broadcast([128, 8, 8])
                Brt = self.B("a_rt")
                Bacs = self.B("acs")
                self.TT("dve", rt[0], x1, cb_, ALU.mult, R=[Bf, Bacs], W=[Brt])
                self.TT("dve", rt[1], x2, sb_, ALU.mult, R=[Bf, Bacs], WA=[Brt])
                self.TT("dve", rt[2], x2, cb_, ALU.mult, R=[Bf, Bacs], WA=[Brt])
                self.TT("dve", rt[3], x1, sb_, ALU.mult, R=[Bf, Bacs], WA=[Brt])
                self.TT("dve", x1, rt[0], rt[1], ALU.subtract, R=[Brt], WA=[Bf])
                self.TT("dve", x2, rt[2], rt[3], ALU.add, R=[Brt], WA=[Bf])
                Bb = self.B(f"a_qkb{p}")
                self.CP("pool", qkb[p], qkf[p], R=[Bf], W=[Bb])
                ptb = self.pbank(7, BF16)[:, 0:512].rearrange("p (i t) -> p i t", t=128)
                Bpt = self.B("bank7")
                for i in range(4):
                    self.TR(ptb[:, i, :], qkb[p][:, i * 128:(i + 1) * 128], R=[Bb], W=[Bpt] if i == 0 else (), WA=() if i == 0 else [Bpt])
                self.CP("dve", qT[:, :, tsl], ptb[:, 0:2, :], R=[Bpt], W=[self.B(f"a_qT{t}")])
                self.CP("act", kT[:, :, tsl], ptb[:, 2:4, :], R=[Bpt], W=[self.B(f"a_kT{t}")])

            p1_mm(0)
            for t in range(1, NT):
                p1_mm(t)
                p1_post(t - 1)
            p1_post(NT - 1)
            it = 0
            for hl in range(2):
                for qt in range(4):
                    started = [False, False, False]
                    Bacc = [self.B(f"bank{4 + b}") for b in range(3)]
                    BqTs = [self.B(f"a_qT{4 * qt + i}") for i in range(4)]
                    nkb = 4 * qt + 4

                    def acc_ap(c, qb):
                        a = c * 4 + qb
                        return self.pbank(4 + a // 3)[:, (a % 3) * 129:(a % 3) * 129 + 129], a // 3

                    for kb in range(nkb):
                        jd = kb - 4 * qt
                        c0 = max(jd, 0) * 128
                        par = it % 2
                        it += 1
                        for c in range(2):
                            Bs = self.B(f"bank{2 * par + c}")
                            self.MM(self.pbank(2 * par + c)[:, c0:512], kT[c * 64:(c + 1) * 64, hl, kb * 128:(kb + 1) * 128],
                                    qT[c * 64:(c + 1) * 64, hl, qt * 512 + c0:(qt + 1) * 512], True, True,
                                    R=[self.B(f"a_kT{kb}")] + BqTs, W=[Bs])
                        for c in range(2):
                            Bs = self.B(f"bank{2 * par + c}")
                            Bp = self.B(f"a_P{par}{c}")
                            self.ACT(Pt[par][c][:, c0:512], self.pbank(2 * par + c)[:, c0:512], AF.Exp, R=[Bs], W=[Bp])
                            if jd >= 0:
                                self.TT("pool", Pt[par][c][:, c0:c0 + 128], Pt[par][c][:, c0:c0 + 128], self.cmask, ALU.mult, R=[Bp, cB], WA=[Bp])
                        for c in range(2):
                            Bp = self.B(f"a_P{par}{c}")
                            for qb in range(max(jd, 0), 4):
                                ap_, b = acc_ap(c, qb)
                                st = not started[b]
                                started[b] = True
                                self.MM(ap_, Pt[par][c][:, qb * 128:(qb + 1) * 128], V[:, kb, hl, :], st, kb == 4 * qt + qb,
                                        R=[Bp, self.B(f"a_V{kb}")], W=[Bacc[b]] if st else (), WA=() if st else [Bacc[b]])
                    Baccs = self.B("a_accs")
                    for b in range(3):
                        n_ = 387 if b < 2 else 258
                        self.CP("dve", accs[:, b, 0:n_], self.pbank(4 + b)[:, 0:n_], R=[Bacc[b]], W=[Baccs] if b == 0 else (), WA=() if b == 0 else [Baccs])

                    def sacc(c, qb):
                        a = c * 4 + qb
                        return accs[:, a // 3, (a % 3) * 129:(a % 3) * 129 + 129]

                    for qb in range(4):
                        t = 4 * qt + qb
                        p = qb % 2
                        a1, a2 = sacc(0, qb), sacc(1, qb)
                        b1 = b2 = 0
                        Bsm = self.B(f"a_sm{p}")
                        o_ = 8 + p * 8
                        self.P.op("dve", (lambda a1=a1, o_=o_: lambda e: e.reciprocal(out=sm[:, o_:o_ + 1], in_=a1[:, 128:129]))(), R=[Baccs], W=[Bsm])
                        self.P.op("dve", (lambda a2=a2, o_=o_: lambda e: e.reciprocal(out=sm[:, o_ + 1:o_ + 2], in_=a2[:, 128:129]))(), R=[Baccs], WA=[Bsm])
                        self.TT("dve", sm[:, o_ + 1:o_ + 2], sm[:, o_ + 1:o_ + 2], neglam, ALU.mult, R=[Bsm, Bl], WA=[Bsm])
                        Bo = self.B(f"a_of{p}")
                        self.TS("dve", of[p], a1[:, 0:128], sm[:, o_:o_ + 1], None, ALU.mult, None, R=[Baccs, Bsm], W=[Bo])
                        self.STT(of[p], a2[:, 0:128], sm[:, o_ + 1:o_ + 2], of[p], ALU.mult, ALU.add, R=[Baccs, Bsm, Bo], WA=[Bo])
                        self.STT(junk2, of[p], 1.0, of[p], ALU.mult, ALU.mult, R=[Bo], WA=[Bsm], accum=sm[:, o_ + 2:o_ + 3])
                        self.emit_rstd(sm[:, o_ + 3:o_ + 4], sm[:, o_ + 2:o_ + 3], 1.0 / 128, R=[Bsm], W=[self.B(f"a_rs{p}")])
                        Bon = self.B(f"a_on{p}")
                        self.TS("dve", onb[p], of[p], sm[:, o_ + 3:o_ + 4], None, ALU.mult, None, R=[Bo, self.B(f"a_rs{p}")], W=[Bon])
                        pt2 = self.pbank(7, BF16)[:, 512 + p * 128:512 + (p + 1) * 128]
                        Bp2 = self.B(f"bank7b{p}")
                        self.TR(pt2, onb[p], R=[Bon], W=[Bp2])
                        self.TS("dve", onT[:, hl, t * 128:(t + 1) * 128], pt2, sgcol, None, ALU.mult, None, R=[Bp2, Bl], W=[self.B(f"a_onT{hl}_{t}")])
            for t in range(NT):
                p = t % 2
                tsl = slice(t * 128, (t + 1) * 128)
                for dh in range(2):
                    Bk = self.B(f"bank{2 * p + dh}")
                    for hl in range(2):
                        self.MM(self.pbank(2 * p + dh), onT[:, hl, tsl], WB[sl][:, hl, dh * 512:(dh + 1) * 512], hl == 0, hl == 1,
                                R=[self.B(f"a_onT{hl}_{t}"), Bwo], W=[Bk] if hl == 0 else (), WA=() if hl == 0 else [Bk])
                    BXt = self.B(f"X{t}")
                    xs = X[:, t, dh * 512:(dh + 1) * 512]
                    self.TT("dve", xs, xs, self.pbank(2 * p + dh), ALU.add, R=[Bk, BXt], WA=[BXt])

    def emit_ffn(self, li):
        cB = self.B("consts")
        X, HT = self.X, self.HT
        Win = [self.carve([8, 1024], BF16) for _ in range(2)]
        Wout = [self.carve([4, D], BF16) for _ in range(2)]
        gb = [self.carve([516], F32) for _ in range(2)]
        tb = [self.carve([512], F32) for _ in range(2)]
        sl_ = [self.carve([512], F32) for _ in range(2)]
        actT = [self.carve([4, 512], BF16) for _ in range(2)]
        carry = self.carve([4, 2], F32)
        self.emit_norm(self.nfg[:, li * 8:(li + 1) * 8])
        win = self.f_win[self.ffn_ls.index(li)]
        wout = self.f_wout[self.ffn_ls.index(li)]
        groups = [(0, 4), (512, 4), (1024, 4), (1536, 4), (2048, 4), (2560, 2)]
        cw = self.cw[:, li * 66:(li + 1) * 66].rearrange("p (f j) -> p f j", j=3)
        cb = self.cb[:, li * 22:(li + 1) * 22]

        def load_w(gi):
            f0, nch = groups[gi]
            nf = nch * 128
            s_ = gi % 2
            Bw = self.B(f"f_Win{s_}")
            self.DMA("pool", Win[s_][:, :, 0:nf], win[:, f0:f0 + nf].rearrange("(k p) c -> p k c", p=128), f"f_Win{s_}", W=[Bw])
            self.DMA("pool", Win[s_][:, :, 512:512 + nf], win[:, FFN + f0:FFN + f0 + nf].rearrange("(k p) c -> p k c", p=128), f"f_Win{s_}", WA=[Bw])
            self.DMA("pool", Wout[s_][:, 0:nch, :], wout[f0:f0 + nf, :].rearrange("(c p) d -> p c d", p=128), f"f_Wout{s_}", W=[self.B(f"f_Wout{s_}")])

        load_w(0)
        it = 0
        for gi, (f0, nch) in enumerate(groups):
            s_ = gi % 2
            if gi + 1 < len(groups):
                load_w(gi + 1)
            Bw = self.B(f"f_Win{s_}")
            Bwo = self.B(f"f_Wout{s_}")
            for T in range(4):
                ap_ = T % 2
                BHs = [self.B(f"HT{4 * T + i}") for i in range(4)]
                Bact = self.B(f"f_act{ap_}")
                for fc in range(nch):
                    par = it % 2
                    it += 1
                    fi = f0 // 128 + fc
                    psG = self.pbank(2 * par)
                    psU = self.pbank(2 * par + 1)
                    BG = self.B(f"bank{2 * par}")
                    BU = self.B(f"bank{2 * par + 1}")
                    for k in range(8):
                        self.MM(psG, Win[s_][:, k, fc * 128:(fc + 1) * 128], HT[:, k, T * 512:(T + 1) * 512], k == 0, k == 7,
                                R=BHs + [Bw], W=[BG] if k == 0 else (), WA=() if k == 0 else [BG])
                    for k in range(8):
                        self.MM(psU, Win[s_][:, k, 512 + fc * 128:512 + (fc + 1) * 128], HT[:, k, T * 512:(T + 1) * 512], k == 0, k == 7,
                                R=BHs + [Bw], W=[BU] if k == 0 else (), WA=() if k == 0 else [BU])
                    Bgb = self.B(f"f_gb{par}")
                    Bc = self.B(f"f_carry{fc}")
                    if T == 0:
                        self.MEMSET("pool", gb[par][:, 0:2], 0.0, W=[Bgb])
                    else:
                        self.CP("pool", gb[par][:, 0:2], carry[:, fc, :], R=[Bc], W=[Bgb])
                    self.CP("act", gb[par][:, 2:514], psG, R=[BG], WA=[Bgb])
                    self.CP("pool", carry[:, fc, :], gb[par][:, 512:514], R=[Bgb], W=[Bc])
                    Btb = self.B(f"f_tb{par}")
                    self.TS("pool", tb[par], gb[par][:, 2:514], cw[:, fi, 2:3], cb[:, fi:fi + 1], ALU.mult, ALU.add, R=[Bgb, cB], W=[Btb])
                    self.STT(tb[par], gb[par][:, 1:513], cw[:, fi, 1:2], tb[par], ALU.mult, ALU.add, R=[Bgb, cB, Btb], WA=[Btb])
                    self.STT(tb[par], gb[par][:, 0:512], cw[:, fi, 0:1], tb[par], ALU.mult, ALU.add, R=[Bgb, cB, Btb], WA=[Btb])
                    Bsl = self.B(f"f_sl{par}")
                    self.ACT(sl_[par], tb[par], AF.Silu, R=[Btb], W=[Bsl])
                    self.TT("dve", actT[ap_][:, fc, :], sl_[par], psU, ALU.mult, R=[Bsl, BU], W=[Bact] if fc == 0 else (), WA=() if fc == 0 else [Bact])
                for tb_ in range(4):
                    t = 4 * T + tb_
                    p2 = t % 2
                    BXt = self.B(f"X{t}")
                    for dh in range(2):
                        bk = 4 + 2 * p2 + dh
                        Bk = self.B(f"bank{bk}")
                        for fc in range(nch):
                            self.MM(self.pbank(bk), actT[ap_][:, fc, tb_ * 128:(tb_ + 1) * 128], Wout[s_][:, fc, dh * 512:(dh + 1) * 512],
                                    fc == 0, fc == nch - 1, R=[Bact, Bwo], W=[Bk] if fc == 0 else (), WA=() if fc == 0 else [Bk])
                        xs = X[:, t, dh * 512:(dh + 1) * 512]
                        self.TT("dve", xs, xs, self.pbank(bk), ALU.add, R=[Bk, BXt], WA=[BXt])

    def emit_ret(self, li):
        j = li // 2
        cB = self.B("consts")
        X, HT = self.X, self.HT
        WA = self.carve([8, 1536], BF16)
        WB = [self.carve([4, D], BF16) for _ in range(2)]
        St = self.carve([2, 512], F32)
        Sbf = [self.carve([2, 512], BF16) for _ in range(2)]
        rt = [self.carve([2, 128], F32) for _ in range(4)]
        qkr = [self.carve([2, 2, 128], BF16) for _ in range(2)]
        vbf = [self.carve([512], BF16) for _ in range(2)]
        sg = [self.carve([512], F32) for _ in range(2)]
        kd = [self.carve([256], BF16) for _ in range(2)]
        qkT = [self.carve([4, 128], BF16) for _ in range(2)]
        innT = [self.carve([128], BF16) for _ in range(2)]
        gated = [self.carve([512], BF16) for _ in range(2)]
        goT = [self.carve([4, 128], BF16) for _ in range(2)]
        junk = self.carve([512], BF16)
        sm = self.carve([16], F32)
        self.emit_norm(self.nmg[:, li * 8:(li + 1) * 8])
        wq = self.r_wqkvg[self.ret_js.index(j)]
        wo = self.r_wo[self.ret_js.index(j)]
        rcos = self.rcs[:, 0]
        rsin = self.rcs[:, 1]
        Brcs = self.B("rcs")
        gam = [1.0 - 2.0 ** (-5.0 - h) for h in range(4)]

        def load_w(h):
            blocks = [(h * 256, 256, 0), (1024 + h * 256, 256, 256), (2048 + h * 512, 512, 512), (4096 + h * 512, 512, 1024)]
            names = ["r_Wqk", "r_Wqk", "r_Wv", "r_Wg"]
            first = {"r_Wqk": True, "r_Wv": True, "r_Wg": True}
            for (c0, n, d0), nm in zip(blocks, names):
                src = wq[:, c0:c0 + n].rearrange("(k p) c -> p k c", p=128)
                Bw = self.B(nm)
                self.DMA("pool", WA[:, :, d0:d0 + n], src, nm, W=[Bw] if first[nm] else (), WA=() if first[nm] else [Bw])
                first[nm] = False
            s_ = h % 2
            self.DMA("pool", WB[s_], wo[h * 512:(h + 1) * 512, :].rearrange("(c p) d -> p c d", p=128), f"r_WB{s_}", W=[self.B(f"r_WB{s_}")])

        load_w(0)
        for h in range(4):
            s_ = h % 2
            Bwo = self.B(f"r_WB{s_}")
            cd = gam[h] ** 128
            qd = self.rcol[:, h:h + 1]
            qd2 = self.rcol[:, 4 + h:5 + h]
            kdc = self.rcol[:, 8 + h:9 + h]
            maskT = self.rmask[:, h, :]

            def stageA(n):
                p = n % 2
                tsl = slice(n * 128, (n + 1) * 128)
                BHt = self.B(f"HT{n}")
                names = ["r_Wqk", "r_Wv", "r_Wg"]
                for b in range(3):
                    Bk = self.B(f"bank{b}")
                    Bw = self.B(names[b])
                    for k in range(8):
                        self.MM(self.pbank(b), HT[:, k, tsl], WA[:, k, b * 512:(b + 1) * 512], k == 0, k == 7, R=[BHt, Bw],
                                W=[Bk] if k == 0 else (), WA=() if k == 0 else [Bk])
                B0, B1, B2 = self.B("bank0"), self.B("bank1"), self.B("bank2")
                v4 = self.pbank(0).rearrange("p (a i e) -> p a i e", i=128, e=2)
                E = v4[:, :, :, 0]
                O = v4[:, :, :, 1]
                cb_ = rcos[:, n, :].unsqueeze(1).to_broadcast([128, 2, 128])
                sb_ = rsin[:, n, :].unsqueeze(1).to_broadcast([128, 2, 128])
                Brt = self.B("r_rt")
                self.TT("dve", rt[0], E, cb_, ALU.mult, R=[B0, Brcs], W=[Brt])
                self.TT("dve", rt[1], O, sb_, ALU.mult, R=[B0, Brcs], WA=[Brt])
                self.TT("dve", rt[2], O, cb_, ALU.mult, R=[B0, Brcs], WA=[Brt])
                self.TT("dve", rt[3], E, sb_, ALU.mult, R=[B0, Brcs], WA=[Brt])
                Bqr = self.B(f"r_qkr{p}")
                self.TT("dve", qkr[p][:, :, 0, :], rt[0], rt[1], ALU.subtract, R=[Brt], W=[Bqr])
                self.TT("dve", qkr[p][:, :, 1, :], rt[2], rt[3], ALU.add, R=[Brt], WA=[Bqr])
                self.CP("act", vbf[p], self.pbank(1), R=[B1], W=[self.B(f"r_v{p}")])
                self.ACT(sg[p], self.pbank(2), AF.Silu, R=[B2], W=[self.B(f"r_sg{p}")])
                kflat = qkr[p][:, 1].rearrange("p a b -> p (a b)")
                self.ACT(kd[p], kflat, AF.Copy, R=[Bqr, cB], W=[self.B(f"r_kd{p}")], scale=kdc)
                ptb = self.pbank(3, BF16)[:, 0:512].rearrange("p (i t) -> p i t", t=128)
                Bpt = self.B("bank3a")
                qflat = qkr[p].rearrange("p a b c -> p (a b c)")
                for i in range(4):
                    self.TR(ptb[:, i, :], qflat[:, i * 128:(i + 1) * 128], R=[Bqr], W=[Bpt] if i == 0 else (), WA=() if i == 0 else [Bpt])
                self.CP("act", qkT[p], ptb, R=[Bpt], W=[self.B(f"r_qkT{p}")])

            def stageB(n):
                p = n % 2
                BqkT = self.B(f"r_qkT{p}")
                Bv = self.B(f"r_v{p}")
                pin = self.pbank(3)[:, 256:384]
                Bin = self.B("bank3b")
                for dc in range(2):
                    self.MM(pin, qkT[p][:, 2 + dc, :], qkT[p][:, dc, :], dc == 0, dc == 1, R=[BqkT], W=[Bin] if dc == 0 else (), WA=() if dc == 0 else [Bin])
                Bit = self.B(f"r_innT{p}")
                self.TT("dve", innT[p], pin, maskT, ALU.mult, R=[Bin, cB], W=[Bit])
                BO = self.B("bank4")
                pO = self.pbank(4)
                sp_ = (n - 1) % 2
                self.MM(pO, innT[p], vbf[p], True, n == 0, R=[Bit, Bv], W=[BO])
                if n > 0:
                    Bs = self.B(f"r_Sbf{sp_}")
                    for dc in range(2):
                        self.MM(pO, qkT[p][:, dc, :], Sbf[sp_][:, dc, :], False, dc == 1, R=[BqkT, Bs], WA=[BO])
                Bkd = self.B(f"r_kd{p}")
                BSt = self.B("r_St")
                if n < NT - 1:
                    for dc in range(2):
                        Bd = self.B(f"bank{5 + dc}")
                        self.MM(self.pbank(5 + dc), kd[p][:, dc * 128:(dc + 1) * 128], vbf[p], True, True, R=[Bkd, Bv], W=[Bd])
                        if n == 0:
                            self.CP("dve", St[:, dc, :], self.pbank(5 + dc), R=[Bd], W=[BSt] if dc == 0 else (), WA=() if dc == 0 else [BSt])
                        else:
                            self.STT(St[:, dc, :], St[:, dc, :], cd, self.pbank(5 + dc), ALU.mult, ALU.add, R=[Bd, BSt], WA=[BSt])
                    self.CP("pool", Sbf[p], St, R=[BSt], W=[self.B(f"r_Sbf{p}")])
                Bsm = self.B(f"r_sm{p}")
                o_ = p * 8
                self.ACT(junk, pO, AF.Square, R=[BO], W=[Bsm], WA=[self.B("r_junk")], accum=sm[:, o_:o_ + 1])
                self.TT("dve", sm[:, o_ + 1:o_ + 2], sm[:, o_:o_ + 1], qd2, ALU.mult, R=[Bsm, cB], WA=[Bsm])
                self.emit_rstd(sm[:, o_ + 2:o_ + 3], sm[:, o_ + 1:o_ + 2], 1.0, R=[Bsm], W=[self.B(f"r_rs{p}")])
                self.TT("dve", sm[:, o_ + 3:o_ + 4], sm[:, o_ + 2:o_ + 3], qd, ALU.mult, R=[self.B(f"r_rs{p}"), cB], W=[self.B(f"r_rq{p}")])
                Bg = self.B(f"r_gated{p}")
                self.STT(gated[p], pO, sm[:, o_ + 3:o_ + 4], sg[p], ALU.mult, ALU.mult, R=[BO, self.B(f"r_rq{p}"), self.B(f"r_sg{p}")], W=[Bg])
                ptb = self.pbank(7, BF16)[:, 0:512].rearrange("p (i t) -> p i t", t=128)
                Bpt = self.B("bank7")
                for i in range(4):
                    self.TR(ptb[:, i, :], gated[p][:, i * 128:(i + 1) * 128], R=[Bg], W=[Bpt] if i == 0 else (), WA=() if i == 0 else [Bpt])
                BgT = self.B(f"r_goT{p}")
                self.CP("act", goT[p], ptb, R=[Bpt], W=[BgT])
                BXt = self.B(f"X{n}")
                tslx = n
                for dh in range(2):
                    Bk = self.B("bank6") if dh == 0 else self.B("bank5")
                    bk = 6 if dh == 0 else 5
                    if dh == 1:
                        bk = 6
                        Bk = self.B("bank6")
                    for ec in range(4):
                        self.MM(self.pbank(bk), goT[p][:, ec, :], WB[s_][:, ec, dh * 512:(dh + 1) * 512], ec == 0, ec == 3, R=[BgT, Bwo],
                                W=[Bk] if ec == 0 else (), WA=() if ec == 0 else [Bk])
                    xs = X[:, tslx, dh * 512:(dh + 1) * 512]
                    self.TT("dve", xs, xs, self.pbank(bk), ALU.add, R=[Bk, BXt], WA=[BXt])

            stageA(0)
            for n in range(NT):
                if n + 1 < NT:
                    stageA(n + 1)
                    if n + 1 == NT - 1 and h + 1 < 4:
                        load_w(h + 1)
                stageB(n)

    def emit_out(self, s):
        junk = self.carve([D], BF16)
        ob = [self.carve([D], F32) for _ in range(2)]
        ss = self.carve([4], F32)
        gfull = self.carve([D], F32)
        Bg = self.B("o_g")
        self.DMA("sp", gfull, self.fng_d, "o_g", W=[Bg])
        for t in range(NT):
            p = t % 2
            BXt = self.B(f"X{t}")
            Bob = self.B(f"o_b{p}")
            if self.final_norm:
                Bss = self.B(f"o_ss{p}")
                self.ACT(junk, self.X[:, t, :], AF.Square, R=[BXt], W=[Bss], WA=[self.B("o_junk")], accum=ss[:, p:p + 1])
                self.emit_rstd(ss[:, 2 + p:3 + p], ss[:, p:p + 1], 1.0 / D, R=[Bss], W=[self.B(f"o_rs{p}")])
                self.STT(ob[p], self.X[:, t, :], ss[:, 2 + p:3 + p], gfull, ALU.mult, ALU.mult, R=[BXt, self.B(f"o_rs{p}"), Bg], W=[Bob])
            else:
                self.CP("dve", ob[p], self.X[:, t, :], R=[BXt], W=[Bob])
            self.DMA("sp", self.out_d[s, t * 128:(t + 1) * 128, :], ob[p], "out", R=[Bob], WA=[self.B("out")])


def _consts():
    ident = np.eye(128, dtype=np.float32)
    cm = (np.arange(128)[:, None] <= np.arange(128)[None, :]).astype(np.float32)
    cbf = np.concatenate([ident, cm], axis=1).astype(ml_dtypes.bfloat16)
    afreq = (500000.0 ** (-np.arange(0, 16, 2, dtype=np.float32) / np.float32(16))).astype(np.float32)
    rfreq = (1.0 / (10000.0 ** np.linspace(0.0, 1.0, 128, dtype=np.float32))).astype(np.float32)
    cf = np.zeros((128, 664), np.float32)
    cf[:, 0:8] = afreq[None, :]
    cf[:, 8:136] = rfreq[None, :]
    idx = np.arange(128, dtype=np.float64)
    for h in range(4):
        gam = 1.0 - 2.0 ** (-5.0 - h)
        m = np.where(idx[None, :] >= idx[:, None], (gam ** (-(idx[:, None] + 1.0))) / 16.0, 0.0)
        cf[:, 136 + h * 128:136 + (h + 1) * 128] = m
        qd = gam ** (idx + 1.0)
        cf[:, 648 + h] = qd
        cf[:, 652 + h] = qd * qd / 512.0
        cf[:, 656 + h] = gam ** (127.0 - idx) / 16.0
    return cbf, cf


_NC_CACHE = {}


def _get_nc(layers, nseq, final_norm):
    key = (tuple(layers), nseq, final_norm)
    if key not in _NC_CACHE:
        _NC_CACHE[key] = Builder(list(layers), nseq, final_norm).build()
    return _NC_CACHE[key]


PLAN = [[0, 1, 2, 3]]


def _norm_layers(layers):
    return tuple(e if isinstance(e, tuple) else (e, True, True) for e in layers)


def _run(inputs, layers=None, final_norm=True, plan=None):
    f = lambda a: np.ascontiguousarray(np.asarray(a, dtype=np.float32))
    x = f(inputs["x"])
    pos = np.ascontiguousarray(np.asarray(inputs["positions"], dtype=np.int32))
    cbf, cf = _consts()
    fm = lambda g: np.ascontiguousarray(f(g).reshape(DEPTH, 8, 128).transpose(2, 0, 1).reshape(128, DEPTH * 8))
    lam = np.concatenate([f(inputs["attn_lambda_q1"]), f(inputs["attn_lambda_k1"]),
                          f(inputs["attn_lambda_q2"]), f(inputs["attn_lambda_k2"])], axis=1)
    lam = np.ascontiguousarray(np.broadcast_to(lam.reshape(1, 512), (128, 512)))
    subln = np.ascontiguousarray(f(inputs["attn_subln_g"]).T)
    cw = f(inputs["ffn_conv_w"]).reshape(DEPTH, 3, NFC, 128).transpose(3, 0, 2, 1)
    cw = np.ascontiguousarray(cw.reshape(128, DEPTH * NFC * 3))
    cb = np.ascontiguousarray(f(inputs["ffn_conv_b"]).reshape(DEPTH, NFC, 128).transpose(2, 0, 1).reshape(128, DEPTH * NFC))
    small = dict(
        nmg=fm(inputs["norm_mix_g"]), nfg=fm(inputs["norm_ffn_g"]),
        fng=np.ascontiguousarray(np.broadcast_to(f(inputs["final_norm_g"]).reshape(1, D), (128, D))),
        lam_in=lam, subln=subln, f_cw=cw, f_cb=cb, c_bf=cbf, c_f32=cf,
    )
    if plan is None:
        plan = [list(layers)] if layers is not None else PLAN
    pcs = [np.ascontiguousarray(pos[c * NSEQ:(c + 1) * NSEQ].reshape(NSEQ, NT, 128).transpose(0, 2, 1)) for c in range(N_CORES)]
    cur = x
    for li_, lay in enumerate(plan):
        lay = _norm_layers(lay)
        fn = final_norm and (li_ == len(plan) - 1)
        key = (lay, NSEQ, fn)
        if key not in _NC_CACHE:
            b = Builder(list(lay), NSEQ, fn)
            _NC_CACHE[key] = (b.build(), b)
        nc, b = _NC_CACHE[key]
        shared = dict(small)
        if b.attn_js:
            shared["a_wqkv"] = f(inputs["attn_w_qkv"])[b.attn_js]
            shared["a_wo"] = f(inputs["attn_w_o"])[b.attn_js]
        if b.ret_js:
            shared["r_wqkvg"] = f(inputs["ret_w_qkvg"])[b.ret_js]
            shared["r_wo"] = f(inputs["ret_w_o"])[b.ret_js]
        if b.ffn_ls:
            shared["f_win"] = f(inputs["ffn_w_in"])[b.ffn_ls]
            shared["f_wout"] = f(inputs["ffn_w_out"])[b.ffn_ls]
        in_maps = []
        for c in range(N_CORES):
            m = dict(shared)
            m["x"] = np.ascontiguousarray(cur[c * NSEQ:(c + 1) * NSEQ])
            m["pos"] = pcs[c]
            in_maps.append(m)
        res = run_bass_kernel_spmd(nc, in_maps, core_ids=list(range(N_CORES)))
        cur = np.concatenate([r["out"] for r in res.results], axis=0)
    return cur


def kernel(**inputs):
    return _run(inputs)
```

```python
import math
import numpy as np
import ml_dtypes
import concourse.bass as bass
import concourse.mybir as mybir
from concourse.bass_utils import run_bass_kernel_spmd

dt = mybir.dt
F32, BF16, I32 = dt.float32, dt.bfloat16, dt.int32
AF = mybir.ActivationFunctionType
ALU = mybir.AluOpType
COMPUTE = ("pe", "act", "dve", "pool")

D = 1024
S = 2048
NT = 16
NSEQ = 2
DEPTH = 4
FFN = 2816
NFC = 22
EPS = 1e-6
N_CORES = 8
PI = math.pi
EPOCH = 1024


class Buf:
    __slots__ = ("name", "w", "r")

    def __init__(self, name):
        self.name = name
        self.w = {}
        self.r = {}


class Op:
    __slots__ = ("eng", "fn", "deps", "sig", "val", "key", "is_dma", "order", "epoch")

    def __init__(self, eng, fn, is_dma=False, key=None):
        self.eng = eng
        self.fn = fn
        self.deps = {}
        self.sig = False
        self.val = 0
        self.key = key if key is not None else eng
        self.is_dma = is_dma


class Prog:
    def __init__(self):
        self.streams = {e: [] for e in ("pe", "act", "dve", "pool", "sp")}
        self.dma_cnt = {}
        self.all_ops = []

    def op(self, eng, fn, R=(), W=(), WA=()):
        o = Op(eng, fn)
        self._track(o, R, W, WA)
        return o

    def dma(self, eng, fn, key, R=(), W=(), WA=()):
        o = Op(eng, fn, is_dma=True, key="dma:" + key)
        self.dma_cnt[key] = self.dma_cnt.get(key, 0) + 1
        o.val = 16 * self.dma_cnt[key]
        o.sig = True
        self._track(o, R, W, WA)
        return o

    def _track(self, o, R, W, WA):
        o.order = len(self.all_ops)
        self.all_ops.append(o)
        self.streams[o.eng].append(o)
        for b in R:
            for s in b.w.values():
                self._add(o, s, True)
        for b in list(W) + list(WA):
            for s in b.w.values():
                self._add(o, s, False)
            for s in b.r.values():
                self._add(o, s, False)
        for b in R:
            b.r[o.key] = o
        for b in W:
            b.w = {o.key: o}
            b.r = {}
        for b in WA:
            b.w[o.key] = o

    def _add(self, o, s, raw):
        if s is o:
            return
        if (not s.is_dma) and (not o.is_dma) and s.eng == o.eng:
            if not raw or s.eng == "pe":
                return
        cur = o.deps.get(s.key)
        if cur is None or cur.order < s.order:
            o.deps[s.key] = s

    def finalize(self):
        for o in self.all_ops:
            for s in o.deps.values():
                s.sig = True
        cnt = {e: 0 for e in COMPUTE}
        for o in self.all_ops:
            if not o.is_dma and o.sig:
                cnt[o.eng] += 1
                o.epoch = (cnt[o.eng] - 1) // EPOCH
                o.val = (cnt[o.eng] - 1) % EPOCH + 1
        self.n_epochs = {e: (cnt[e] + EPOCH - 1) // EPOCH for e in COMPUTE}

    def replay(self, name, eng, sems, dma_sems):
        waited = {}
        for o in self.streams[name]:
            for k, s in o.deps.items():
                if s.is_dma:
                    if waited.get(k, 0) < s.val:
                        eng.wait_ge(dma_sems[k[4:]], s.val)
                        waited[k] = s.val
                else:
                    tv = (s.epoch, s.val)
                    if waited.get(k, (-1, 0)) < tv:
                        eng.wait_ge(sems[(s.eng, s.epoch)], s.val)
                        waited[k] = tv
            if o.fn is None:
                continue
            ins = o.fn(eng)
            if o.is_dma:
                ins.then_inc(dma_sems[o.key[4:]], 16)
            elif o.sig:
                ins.then_inc(sems[(o.eng, o.epoch)], 1)


class Builder:
    def __init__(self, layers, nseq, final_norm):
        self.layers = layers
        self.nseq = nseq
        self.final_norm = final_norm
        self.P = Prog()
        self.nc = bass.Bass("TRN2", target_bir_lowering=False)
        self.bufs = {}

    def B(self, name):
        b = self.bufs.get(name)
        if b is None:
            b = self.bufs[name] = Buf(name)
        return b

    def Bs(self, names):
        return [self.B(n) for n in names]

    def xb(self, *aps):
        out = []
        for a in aps:
            try:
                if a.tensor.name != "psum":
                    continue
            except AttributeError:
                continue
            es = 4 if a.dtype in (F32, I32) else 2
            b = (a.offset * es) // 2048
            bb = self.B(f"xbank{b}")
            if bb not in out:
                out.append(bb)
        return out

    def dram_in(self, name, shape, d=F32):
        return self.nc.dram_tensor(name, list(shape), d, kind="ExternalInput").ap()

    def MM(self, out, lhsT, rhs, start, stop, R, W=(), WA=()):
        self.P.op("pe", lambda e: e.matmul(out, lhsT=lhsT, rhs=rhs, start=start, stop=stop,
                                           skip_group_check=True), R=R, W=list(W) + self.xb(out), WA=WA)

    def TR(self, out, in_, R, W=(), WA=()):
        ident = self.ident
        self.P.op("pe", lambda e: e.transpose(out=out, in_=in_, identity=ident), R=list(R) + [self.B("consts")], W=list(W) + self.xb(out), WA=WA)

    def ACT(self, out, in_, func, R, W=(), WA=(), scale=None, bias=None, accum=None):
        kw = {}
        if scale is not None:
            kw["scale"] = scale
        if bias is not None:
            kw["bias"] = bias
        if accum is not None:
            kw["accum_out"] = accum
        self.P.op("act", lambda e: e.activation(out=out, in_=in_, func=func, **kw), R=R, W=list(W) + self.xb(out, in_), WA=WA)

    def TT(self, eng, out, in0, in1, op, R, W=(), WA=()):
        self.P.op(eng, lambda e: e.tensor_tensor(out=out, in0=in0, in1=in1, op=op), R=R, W=list(W) + self.xb(out, in0, in1), WA=WA)

    def TS(self, eng, out, in0, s1, s2, op0, op1, R, W=(), WA=()):
        if op1 is None:
            self.P.op(eng, lambda e: e.tensor_scalar(out=out, in0=in0, scalar1=s1, scalar2=None, op0=op0), R=R, W=list(W) + self.xb(out, in0), WA=WA)
        else:
            self.P.op(eng, lambda e: e.tensor_scalar(out=out, in0=in0, scalar1=s1, scalar2=s2, op0=op0, op1=op1), R=R, W=list(W) + self.xb(out, in0), WA=WA)

    def STT(self, out, in0, scalar, in1, op0, op1, R, W=(), WA=(), accum=None):
        if accum is None:
            self.P.op("dve", lambda e: e.scalar_tensor_tensor(out=out, in0=in0, scalar=scalar, in1=in1, op0=op0, op1=op1), R=R, W=list(W) + self.xb(out, in0, in1), WA=WA)
        else:
            self.P.op("dve", lambda e: e.scalar_tensor_tensor(out=out, in0=in0, scalar=scalar, in1=in1, op0=op0, op1=op1, accum_out=accum), R=R, W=list(W) + self.xb(out, in0, in1), WA=WA)

    def CP(self, eng, out, in_, R, W=(), WA=()):
        if eng == "act":
            self.P.op("act", lambda e: e.copy(out=out, in_=in_), R=R, W=list(W) + self.xb(out, in_), WA=WA)
        else:
            self.P.op(eng, lambda e: e.tensor_copy(out=out, in_=in_), R=R, W=list(W) + self.xb(out, in_), WA=WA)

    def MEMSET(self, eng, ap, val, W=(), WA=()):
        self.P.op(eng, lambda e: e.memset(ap, val), W=W, WA=WA)

    def DMA(self, eng, out, in_, key, R=(), W=(), WA=()):
        self.P.dma(eng, lambda e: e.dma_start(out=out, in_=in_), key, R=R, W=W, WA=WA)

    def barrier(self):
        allb = list(self.bufs.values())
        P = self.P
        for e in ("pe", "act", "dve", "pool", "sp"):
            o = Op(e, None)
            o.order = len(P.all_ops)
            for b in allb:
                for src in list(b.w.values()) + list(b.r.values()):
                    if src.fn is None:
                        continue
                    if (not src.is_dma) and src.eng == e:
                        continue
                    cur = o.deps.get(src.key)
                    if cur is None or cur.order < src.order:
                        o.deps[src.key] = src
            P.all_ops.append(o)
            P.streams[e].append(o)

    def arena_reset(self):
        self.aoff = 0

    def carve(self, shape, d):
        n = 1
        for v in shape:
            n *= v
        nbytes = n * (4 if d in (F32, I32) else 2)
        nbytes = (nbytes + 31) // 32 * 32
        o2 = self.aoff // 2
        assert self.aoff + nbytes <= self.arena_bytes, (self.aoff, nbytes, self.arena_bytes)
        v = self.arena[:, o2:o2 + nbytes // 2]
        self.aoff += nbytes
        if d != BF16:
            v = v.bitcast(d)
        v = v[:, 0:n]
        if len(shape) == 2:
            v = v.rearrange("p (a b) -> p a b", b=shape[1])
        elif len(shape) == 3:
            v = v.rearrange("p (a b c) -> p a b c", b=shape[1], c=shape[2])
        return v

    def pbank(self, i, d=F32):
        v = self.psum[:, i * 512:(i + 1) * 512]
        if d == BF16:
            v = v.bitcast(BF16)
        return v

    def build(self):
        nc = self.nc
        ns = self.nseq
        self.x_d = self.dram_in("x", [ns, S, D])
        self.pos_d = self.dram_in("pos", [ns, 128, NT], I32)
        self.nmg_d = self.dram_in("nmg", [128, DEPTH * 8])
        self.nfg_d = self.dram_in("nfg", [128, DEPTH * 8])
        self.fng_d = self.dram_in("fng", [128, D])
        self.lam_d = self.dram_in("lam_in", [128, 2 * 256])
        self.subln_d = self.dram_in("subln", [128, 2])
        self.cw_d = self.dram_in("f_cw", [128, DEPTH * NFC * 3])
        self.cb_d = self.dram_in("f_cb", [128, DEPTH * NFC])
        ents = [e if isinstance(e, tuple) else (e, True, True) for e in self.layers]
        self.attn_js = sorted({li // 2 for li, m, f in ents if m and li % 2 == 0})
        self.ret_js = sorted({li // 2 for li, m, f in ents if m and li % 2 == 1})
        self.ffn_ls = sorted({li for li, m, f in ents if f})
        if self.attn_js:
            self.a_wqkv = self.dram_in("a_wqkv", [len(self.attn_js), D, 3 * D])
            self.a_wo = self.dram_in("a_wo", [len(self.attn_js), D, D])
        if self.ret_js:
            self.r_wqkvg = self.dram_in("r_wqkvg", [len(self.ret_js), D, 6144])
            self.r_wo = self.dram_in("r_wo", [len(self.ret_js), 2048, D])
        if self.ffn_ls:
            self.f_win = self.dram_in("f_win", [len(self.ffn_ls), D, 2 * FFN])
            self.f_wout = self.dram_in("f_wout", [len(self.ffn_ls), FFN, D])
        self.cbf_d = self.dram_in("c_bf", [128, 256], BF16)
        self.cf_d = self.dram_in("c_f32", [128, 8 + 128 + 512 + 16])
        self.out_d = nc.dram_tensor("out", [ns, S, D], F32, kind="ExternalOutput").ap()

        self.X = nc.alloc_sbuf_tensor("X", [128, NT, D], F32)[:]
        self.HT = nc.alloc_sbuf_tensor("HT", [128, 8, S], BF16)[:]
        self.cbf = nc.alloc_sbuf_tensor("cbf", [128, 256], BF16)[:]
        self.cf = nc.alloc_sbuf_tensor("cf", [128, 664], F32)[:]
        self.ident = self.cbf[:, 0:128]
        self.cmask = self.cbf[:, 128:256]
        self.afreq = self.cf[:, 0:8]
        self.rfreq = self.cf[:, 8:136]
        self.rmask = self.cf[:, 136:648].rearrange("p (h i) -> p h i", i=128)
        self.rcol = self.cf[:, 648:664]
        self.params = nc.alloc_sbuf_tensor("params", [128, 32 + 32 + 512 + 2 + 264 + 88], F32)[:]
        o = 0
        self.nmg = self.params[:, o:o + 32]; o += 32
        self.nfg = self.params[:, o:o + 32]; o += 32
        self.lamin = self.params[:, o:o + 512]; o += 512
        self.subln = self.params[:, o:o + 2]; o += 2
        self.cw = self.params[:, o:o + 264]; o += 264
        self.cb = self.params[:, o:o + 88]; o += 88
        self.rcs = nc.alloc_sbuf_tensor("rcs", [128, 2, NT, 128], F32)[:]
        self.acs = nc.alloc_sbuf_tensor("acs", [128, 2, NT, 8], F32)[:]
        self.posf = nc.alloc_sbuf_tensor("posf", [128, NT], F32)[:]
        self.posi = nc.alloc_sbuf_tensor("posi", [128, NT], I32)[:]
        self.small = nc.alloc_sbuf_tensor("small", [128, 64], F32)[:]
        self.arena_bytes = (nc.sbuf_bytes_remaining - 256) // 64 * 64
        self.arena = nc.alloc_sbuf_tensor("arena", [128, self.arena_bytes // 2], BF16)[:]
        self.psum = nc.alloc_psum_tensor("psum", [128, 4096], F32)[:]

        cB = self.B("consts")
        self.DMA("sp", self.cbf, self.cbf_d, "consts", W=[cB])
        self.DMA("sp", self.cf, self.cf_d, "consts", WA=[cB])
        self.DMA("sp", self.nmg, self.nmg_d, "consts", WA=[cB])
        self.DMA("sp", self.nfg, self.nfg_d, "consts", WA=[cB])
        self.DMA("sp", self.lamin, self.lam_d, "consts", WA=[cB])
        self.DMA("sp", self.subln, self.subln_d, "consts", WA=[cB])
        self.DMA("sp", self.cw, self.cw_d, "consts", WA=[cB])
        self.DMA("sp", self.cb, self.cb_d, "consts", WA=[cB])
        self.MEMSET("dve", self.small[:, 0:1], EPS, WA=[cB])

        for s in range(ns):
            self.emit_seq(s)
        self.P.op("sp", None, R=[self.B("out")])
        self.P.finalize()

        from contextlib import ExitStack
        with ExitStack() as es:
            sems = {(e, k): es.enter_context(nc.semaphore(f"s_{e}{k}")) for e in COMPUTE for k in range(self.P.n_epochs[e])}
            dsems = {k: es.enter_context(nc.semaphore("d_" + k)) for k in self.P.dma_cnt}
            P = self.P
            with nc.Block() as block:
                @block.tensor
                def _(e):
                    P.replay("pe", e, sems, dsems)

                @block.scalar
                def _(e):
                    P.replay("act", e, sems, dsems)

                @block.vector
                def _(e):
                    P.replay("dve", e, sems, dsems)

                @block.gpsimd
                def _(e):
                    P.replay("pool", e, sems, dsems)

                @block.sync
                def _(e):
                    P.replay("sp", e, sems, dsems)
        return nc

    def emit_seq(self, s):
        BX = [self.B(f"X{t}") for t in range(NT)]
        self.barrier()
        for t in range(NT):
            self.DMA("sp", self.X[:, t, :], self.x_d[s, t * 128:(t + 1) * 128, :], f"X{t}", W=[BX[t]])
        self.DMA("sp", self.posi, self.pos_d[s], "pos", W=[self.B("posi")])
        self.CP("dve", self.posf, self.posi, R=[self.B("posi")], W=[self.B("posf")])
        self.arena_reset()
        self.emit_sincos(self.afreq, 8, self.acs, "acs")
        self.arena_reset()
        self.emit_sincos(self.rfreq, 128, self.rcs, "rcs")
        for ent in self.layers:
            li, do_mix, do_ffn = ent if isinstance(ent, tuple) else (ent, True, True)
            if do_mix:
                self.barrier()
                self.arena_reset()
                if li % 2 == 0:
                    self.emit_attn(li)
                else:
                    self.emit_ret(li)
            if do_ffn:
                self.barrier()
                self.arena_reset()
                self.emit_ffn(li)
        self.barrier()
        self.arena_reset()
        self.emit_out(s)

    def emit_sincos(self, freq, F, dst, name):
        cB = self.B("consts")
        Bt = self.B("sc_tmp")
        Bd = self.B(name)
        ang = self.carve([NT, F], F32)
        ki = self.carve([NT, F], I32)
        kf = self.carve([NT, F], F32)
        y = self.carve([NT, F], F32)
        fb = freq.unsqueeze(1).to_broadcast([128, NT, F])
        pb = self.posf.unsqueeze(2).to_broadcast([128, NT, F])
        self.TT("dve", ang, fb, pb, ALU.mult, R=[cB, self.B("posf")], W=[Bt])
        self.TS("dve", ki, ang, 1.0 / (2 * PI), None, ALU.mult, None, R=[Bt], WA=[Bt])
        self.CP("dve", kf, ki, R=[Bt], WA=[Bt])
        self.STT(ang, kf, -2 * PI, ang, ALU.mult, ALU.add, R=[Bt], WA=[Bt])
        for idx, shift in ((1, 0.0), (0, PI / 2)):
            self.TS("dve", y, ang, shift, None, ALU.add, None, R=[Bt], WA=[Bt])
            self.TS("dve", kf, y, PI, -2 * PI, ALU.is_gt, ALU.mult, R=[Bt], WA=[Bt])
            self.TT("dve", y, y, kf, ALU.add, R=[Bt], WA=[Bt])
            self.TS("dve", kf, y, -PI, 2 * PI, ALU.is_lt, ALU.mult, R=[Bt], WA=[Bt])
            self.TT("dve", y, y, kf, ALU.add, R=[Bt], WA=[Bt])
            self.ACT(dst[:, idx], y, AF.Sin, R=[Bt], WA=[Bd])

    def emit_rstd(self, out, in_, scale, R, W):
        self.ACT(out, in_, AF.Ln, R=list(R) + [self.B("consts")], W=W, scale=scale, bias=self.small[:, 0:1])
        self.ACT(out, out, AF.Exp, R=W, WA=W, scale=-0.5)

    def emit_norm(self, gcol):
        cB = self.B("consts")
        junk = self.carve([D], BF16)
        xn = [self.carve([D], BF16) for _ in range(2)]
        ss = self.carve([4], F32)
        gb = gcol.unsqueeze(2).to_broadcast([128, 8, 128])
        for t in range(NT):
            p = t % 2
            BXt = self.B(f"X{t}")
            Bss = self.B(f"n_ss{p}")
            Bxn = self.B(f"n_xn{p}")
            Bpt = self.B(f"bank{4 + p}")
            self.ACT(junk, self.X[:, t, :], AF.Square, R=[BXt], W=[Bss], WA=[self.B("n_junk")], accum=ss[:, p:p + 1])
            self.emit_rstd(ss[:, 2 + p:3 + p], ss[:, p:p + 1], 1.0 / D, R=[Bss], W=[self.B(f"n_rs{p}")])
            self.ACT(xn[p], self.X[:, t, :], AF.Copy, R=[BXt, self.B(f"n_rs{p}")], W=[Bxn], scale=ss[:, 2 + p:3 + p])
            pt = self.pbank(4 + p, BF16).rearrange("p (c t) -> p c t", t=128)
            for c in range(8):
                self.TR(pt[:, c, :], xn[p][:, c * 128:(c + 1) * 128], R=[Bxn], W=[Bpt] if c == 0 else (), WA=() if c == 0 else [Bpt])
            self.TT("dve", self.HT[:, :, t * 128:(t + 1) * 128], pt, gb, ALU.mult, R=[Bpt, cB], W=[self.B(f"HT{t}")])

    def emit_attn(self, li):
        j = li // 2
        lambda_init = 0.8 - 0.6 * math.exp(-0.3 * li)
        cB = self.B("consts")
        X, HT = self.X, self.HT
        WA = [self.carve([8, 768], BF16) for _ in range(2)]
        WB = [self.carve([2, D], BF16) for _ in range(2)]
        qT = self.carve([2, S], BF16)
        kT = self.carve([2, S], BF16)
        V = self.carve([NT, 2, 129], BF16)
        onT = self.carve([2, S], BF16)
        Pt = [[self.carve([512], BF16) for _ in range(2)] for _ in range(2)]
        qkf = [self.carve([512], F32) for _ in range(2)]
        qkb = [self.carve([512], BF16) for _ in range(2)]
        rt = [self.carve([8, 8], F32) for _ in range(4)]
        onb4 = [self.carve([4, 128], BF16) for _ in range(2)]
        sm = self.carve([32], F32)
        lamj = self.carve([64], F32)
        Bl = self.B("a_lam")
        li0 = self.lamin[:, j * 256:(j + 1) * 256]
        self.STT(lamj, li0[:, 0:64], 1.0, li0[:, 64:128], ALU.mult, ALU.mult, R=[cB], W=[Bl], accum=sm[:, 0:1])
        self.STT(lamj, li0[:, 128:192], 1.0, li0[:, 192:256], ALU.mult, ALU.mult, R=[cB], WA=[Bl], accum=sm[:, 1:2])
        self.ACT(sm[:, 2:4], sm[:, 0:2], AF.Exp, R=[Bl], WA=[Bl])
        self.TT("dve", sm[:, 4:5], sm[:, 3:4], sm[:, 2:3], ALU.subtract, R=[Bl], WA=[Bl])
        self.TS("dve", sm[:, 5:6], sm[:, 4:5], -lambda_init, None, ALU.add, None, R=[Bl], WA=[Bl])
        neglam = sm[:, 5:6]
        self.TS("dve", sm[:, 6:7], self.subln[:, j:j + 1], 1.0 - lambda_init, None, ALU.mult, None, R=[cB], WA=[Bl])
        sgcol = sm[:, 6:7]
        self.MEMSET("pool", V[:, :, :, 128:129], 1.0, W=[self.B("a_Vones")])

        mark = self.aoff
        self.emit_norm(self.nmg[:, li * 8:(li + 1) * 8])
        self.aoff = mark
        accs = self.carve([3, 387], F32)

        wq = self.a_wqkv[self.attn_js.index(j)]
        wo = self.a_wo[self.attn_js.index(j)]

        def load_w(g):
            sl = g % 2
            Bw = self.B(f"a_WA{sl}")
            for blk in range(3):
                src = wq[:, blk * D + g * 256: blk * D + (g + 1) * 256].rearrange("(k p) c -> p k c", p=128)
                self.DMA("pool", WA[sl][:, :, blk * 256:(blk + 1) * 256], src, f"a_WA{sl}", W=[Bw] if blk == 0 else (), WA=() if blk == 0 else [Bw])
            src = wo[g * 256:(g + 1) * 256, :].rearrange("(k p) c -> p k c", p=128)
            self.DMA("pool", WB[sl], src, f"a_WB{sl}", W=[self.B(f"a_WB{sl}")])

        acos = self.acs[:, 0]
        asin = self.acs[:, 1]
        load_w(0)
        for g in range(4):
            sl = g % 2
            if g + 1 < 4:
                load_w(g + 1)
            Bw = self.B(f"a_WA{sl}")
            Bwo = self.B(f"a_WB{sl}")
            def p1_mm(t):
                p = t % 2
                tsl = slice(t * 128, (t + 1) * 128)
                BHt = self.B(f"HT{t}")
                ps_qk = self.pbank(2 * p)
                ps_v = self.pbank(2 * p + 1)[:, 0:256]
                Bqk = self.B(f"bank{2 * p}")
                Bv = self.B(f"bank{2 * p + 1}")
                for k in range(8):
                    self.MM(ps_qk, HT[:, k, tsl], WA[sl][:, k, 0:512], k == 0, k == 7, R=[BHt, Bw], W=[Bqk] if k == 0 else (), WA=() if k == 0 else [Bqk])
                for k in range(8):
                    self.MM(ps_v, HT[:, k, tsl], WA[sl][:, k, 512:768], k == 0, k == 7, R=[BHt, Bw], W=[Bv] if k == 0 else (), WA=() if k == 0 else [Bv])

            def p1_post(t):
                p = t % 2
                tsl = slice(t * 128, (t + 1) * 128)
                ps_qk = self.pbank(2 * p)
                ps_v = self.pbank(2 * p + 1)[:, 0:256]
                Bqk = self.B(f"bank{2 * p}")
                Bv = self.B(f"bank{2 * p + 1}")
                BVt = self.B(f"a_V{t}")
                self.CP("act", V[:, t, :, 0:128], ps_v.rearrange("p (h e) -> p h e", e=128), R=[Bv, self.B("a_Vones")], W=[BVt])
                Bf = self.B(f"a_qkf{p}")
                self.ACT(qkf[p][:, 0:256], ps_qk[:, 0:256], AF.Copy, R=[Bqk], W=[Bf], scale=0.125)
                self.CP("act", qkf[p][:, 256:512], ps_qk[:, 256:512], R=[Bqk], WA=[Bf])
                v3 = qkf[p].rearrange("p (g d) -> p g d", d=64)
                x1 = v3[:, :, 0:8]
                x2 = v3[:, :, 8:16]
                cb_ = acos[:, t, :].unsqueeze(1).to_broadcast([128, 8, 8])
                sb_ = asin[:, t, :].unsqueeze(1).to_broadcast([128, 8, 8])
                Brt = self.B("a_rt")
                Bacs = self.B("acs")
                self.TT("dve", rt[0], x1, cb_, ALU.mult, R=[Bf, Bacs], W=[Brt])
                self.TT("dve", rt[1], x2, sb_, ALU.mult, R=[Bf, Bacs], WA=[Brt])
                self.TT("dve", rt[2], x2, cb_, ALU.mult, R=[Bf, Bacs], WA=[Brt])
                self.TT("dve", rt[3], x1, sb_, ALU.mult, R=[Bf, Bacs], WA=[Brt])
                self.TT("dve", x1, rt[0], rt[1], ALU.subtract, R=[Brt], WA=[Bf])
                self.TT("dve", x2, rt[2], rt[3], ALU.add, R=[Brt], WA=[Bf])
                Bb = self.B(f"a_qkb{p}")
                self.CP("pool", qkb[p], qkf[p], R=[Bf], W=[Bb])
                ptb = self.pbank(7, BF16)[:, 0:512].rearrange("p (i t) -> p i t", t=128)
                Bpt = self.B("bank7")
                for i in range(4):
                    self.TR(ptb[:, i, :], qkb[p][:, i * 128:(i + 1) * 128], R=[Bb], W=[Bpt] if i == 0 else (), WA=() if i == 0 else [Bpt])
                self.CP("dve", qT[:, :, tsl], ptb[:, 0:2, :], R=[Bpt], W=[self.B(f"a_qT{t}")])
                self.CP("act", kT[:, :, tsl], ptb[:, 2:4, :], R=[Bpt], W=[self.B(f"a_kT{t}")])

            p1_mm(0)
            for t in range(1, NT):
                p1_mm(t)
                p1_post(t - 1)
            p1_post(NT - 1)
            steps = [(hl, qt, kb) for hl in range(2) for qt in range(4) for kb in range(4 * qt + 4)]
            nst = len(steps)
            Bacc = [self.B(f"bank{4 + b}") for b in range(3)]
            Baccs = self.B("a_accs")
            accv = accs.rearrange("p b n -> p (b n)")[:, 0:1032].rearrange("p (a n) -> p a n", n=129)
            of4 = qkf[1].rearrange("p (q e) -> p q e", e=128)
            sq4 = qkf[0].rearrange("p (q e) -> p q e", e=128)
            Bof, Bsq = self.B("a_qkf1"), self.B("a_qkf0")
            state = {"started": [False] * 3}

            def acc_ap(c, qb):
                a = c * 4 + qb
                return self.pbank(4 + a // 3)[:, (a % 3) * 129:(a % 3) * 129 + 129], a // 3

            def st_S(i):
                hl, qt, kb = steps[i]
                par = i % 2
                c0 = max(kb - 4 * qt, 0) * 128
                BqTs = [self.B(f"a_qT{4 * qt + q}") for q in range(4)]
                for c in range(2):
                    self.MM(self.pbank(2 * par + c)[:, c0:512], kT[c * 64:(c + 1) * 64, hl, kb * 128:(kb + 1) * 128],
                            qT[c * 64:(c + 1) * 64, hl, qt * 512 + c0:(qt + 1) * 512], True, True,
                            R=[self.B(f"a_kT{kb}")] + BqTs, W=[self.B(f"bank{2 * par + c}")])

            def st_exp(i):
                hl, qt, kb = steps[i]
                par = i % 2
                jd = kb - 4 * qt
                c0 = max(jd, 0) * 128
                for c in range(2):
                    Bp = self.B(f"a_P{par}{c}")
                    self.ACT(Pt[par][c][:, c0:512], self.pbank(2 * par + c)[:, c0:512], AF.Exp, R=[self.B(f"bank{2 * par + c}")], W=[Bp])
                    if jd >= 0:
                        self.TT("pool", Pt[par][c][:, c0:c0 + 128], Pt[par][c][:, c0:c0 + 128], self.cmask, ALU.mult, R=[Bp, cB], WA=[Bp])

            def st_PV(i):
                hl, qt, kb = steps[i]
                par = i % 2
                jd = kb - 4 * qt
                if kb == 0:
                    state["started"] = [False] * 3
                for c in range(2):
                    Bp = self.B(f"a_P{par}{c}")
                    for qb in range(max(jd, 0), 4):
                        ap_, b = acc_ap(c, qb)
                        st = not state["started"][b]
                        state["started"][b] = True
                        self.MM(ap_, Pt[par][c][:, qb * 128:(qb + 1) * 128], V[:, kb, hl, :], st, kb == 4 * qt + qb,
                                R=[Bp, self.B(f"a_V{kb}")], W=[Bacc[b]] if st else (), WA=() if st else [Bacc[b]])

            def st_norm(hl, qt, dp):
                for b in range(3):
                    n_ = 387 if b < 2 else 258
                    self.CP("dve", accs[:, b, 0:n_], self.pbank(4 + b)[:, 0:n_], R=[Bacc[b]], W=[Baccs] if b == 0 else (), WA=() if b == 0 else [Baccs])
                Bsm = self.B("a_sm")
                rec = sm[:, 8:16]
                self.P.op("dve", lambda e: e.reciprocal(out=rec, in_=accv[:, :, 128]), R=[Baccs], W=[Bsm])
                self.TS("dve", sm[:, 12:16], sm[:, 12:16], neglam, None, ALU.mult, None, R=[Bsm, Bl], WA=[Bsm])
                r1 = sm[:, 8:12].unsqueeze(2).to_broadcast([128, 4, 128])
                r2 = sm[:, 12:16].unsqueeze(2).to_broadcast([128, 4, 128])
                self.TT("dve", of4, accv[:, 0:4, 0:128], r1, ALU.mult, R=[Baccs, Bsm], W=[Bof])
                self.TT("dve", sq4, accv[:, 4:8, 0:128], r2, ALU.mult, R=[Baccs, Bsm], W=[Bsq])
                self.TT("dve", of4, of4, sq4, ALU.add, R=[Bsq], WA=[Bof])
                self.TT("dve", sq4, of4, of4, ALU.mult, R=[Bof], W=[Bsq])
                self.P.op("dve", lambda e: e.tensor_reduce(out=sm[:, 16:20], in_=sq4, axis=mybir.AxisListType.X, op=ALU.add), R=[Bsq], WA=[Bsm])
                Brs = self.B("a_rs")
                self.emit_rstd(sm[:, 20:24], sm[:, 16:20], 1.0 / 128, R=[Bsm], W=[Brs])
                rs = sm[:, 20:24].unsqueeze(2).to_broadcast([128, 4, 128])
                self.TT("dve", onb4[dp], of4, rs, ALU.mult, R=[Bof, Brs], W=[self.B(f"a_on{dp}")])

            def st_norm_pe(hl, qt, dp):
                pt2 = self.pbank(7, BF16)[:, 512:1024].rearrange("p (q t) -> p q t", t=128)
                Bp2 = self.B("bank7b")
                for qb in range(4):
                    self.TR(pt2[:, qb, :], onb4[dp][:, qb, :], R=[self.B(f"a_on{dp}")], W=[Bp2] if qb == 0 else (), WA=() if qb == 0 else [Bp2])
                dst = onT[:, hl, qt * 512:(qt + 1) * 512].rearrange("p (q t) -> p q t", t=128)
                self.TS("dve", dst, pt2, sgcol, None, ALU.mult, None, R=[Bp2, Bl],
                        W=[self.B(f"a_onT{hl}_{4 * qt + q}") for q in range(4)])

            pending = []
            st_S(0)
            for i in range(nst):
                if i + 1 < nst:
                    st_S(i + 1)
                st_exp(i)
                st_PV(i)
                hl, qt, kb = steps[i]
                if kb == 4 * qt + 3:
                    dp = (hl * 4 + qt) % 2
                    st_norm(hl, qt, dp)
                    pending.append((i + 3, hl, qt, dp))
                while pending and pending[0][0] <= i:
                    _, h_, q_, d_ = pending.pop(0)
                    st_norm_pe(h_, q_, d_)
            for _, h_, q_, d_ in pending:
                st_norm_pe(h_, q_, d_)
            for t in range(NT):
                p = t % 2
                tsl = slice(t * 128, (t + 1) * 128)
                for dh in range(2):
                    Bk = self.B(f"bank{2 * p + dh}")
                    for hl in range(2):
                        self.MM(self.pbank(2 * p + dh), onT[:, hl, tsl], WB[sl][:, hl, dh * 512:(dh + 1) * 512], hl == 0, hl == 1,
                                R=[self.B(f"a_onT{hl}_{t}"), Bwo], W=[Bk] if hl == 0 else (), WA=() if hl == 0 else [Bk])
                    BXt = self.B(f"X{t}")
                    xs = X[:, t, dh * 512:(dh + 1) * 512]
                    self.TT("dve", xs, xs, self.pbank(2 * p + dh), ALU.add, R=[Bk, BXt], WA=[BXt])

    def emit_ffn(self, li):
        cB = self.B("consts")
        X, HT = self.X, self.HT
        Win = [self.carve([8, 1024], BF16) for _ in range(2)]
        Wout = [self.carve([4, D], BF16) for _ in range(2)]
        gb = [self.carve([516], F32) for _ in range(2)]
        tb = [self.carve([512], F32) for _ in range(2)]
        sl_ = [self.carve([512], F32) for _ in range(2)]
        actT = [self.carve([4, 512], BF16) for _ in range(2)]
        carry = self.carve([4, 2], F32)
        self.emit_norm(self.nfg[:, li * 8:(li + 1) * 8])
        win = self.f_win[self.ffn_ls.index(li)]
        wout = self.f_wout[self.ffn_ls.index(li)]
        groups = [(0, 4), (512, 4), (1024, 4), (1536, 4), (2048, 4), (2560, 2)]
        cw = self.cw[:, li * 66:(li + 1) * 66].rearrange("p (f j) -> p f j", j=3)
        cb = self.cb[:, li * 22:(li + 1) * 22]

        def load_w(gi):
            f0, nch = groups[gi]
            nf = nch * 128
            s_ = gi % 2
            Bw = self.B(f"f_Win{s_}")
            self.DMA("pool", Win[s_][:, :, 0:nf], win[:, f0:f0 + nf].rearrange("(k p) c -> p k c", p=128), f"f_Win{s_}", W=[Bw])
            self.DMA("pool", Win[s_][:, :, 512:512 + nf], win[:, FFN + f0:FFN + f0 + nf].rearrange("(k p) c -> p k c", p=128), f"f_Win{s_}", WA=[Bw])
            self.DMA("pool", Wout[s_][:, 0:nch, :], wout[f0:f0 + nf, :].rearrange("(c p) d -> p c d", p=128), f"f_Wout{s_}", W=[self.B(f"f_Wout{s_}")])

        load_w(0)
        it = 0
        for gi, (f0, nch) in enumerate(groups):
            s_ = gi % 2
            if gi + 1 < len(groups):
                load_w(gi + 1)
            Bw = self.B(f"f_Win{s_}")
            Bwo = self.B(f"f_Wout{s_}")
            for T in range(4):
                ap_ = T % 2
                BHs = [self.B(f"HT{4 * T + i}") for i in range(4)]
                Bact = self.B(f"f_act{ap_}")
                for fc in range(nch):
                    par = it % 2
                    it += 1
                    fi = f0 // 128 + fc
                    psG = self.pbank(2 * par)
                    psU = self.pbank(2 * par + 1)
                    BG = self.B(f"bank{2 * par}")
                    BU = self.B(f"bank{2 * par + 1}")
                    for k in range(8):
                        self.MM(psG, Win[s_][:, k, fc * 128:(fc + 1) * 128], HT[:, k, T * 512:(T + 1) * 512], k == 0, k == 7,
                                R=BHs + [Bw], W=[BG] if k == 0 else (), WA=() if k == 0 else [BG])
                    for k in range(8):
                        self.MM(psU, Win[s_][:, k, 512 + fc * 128:512 + (fc + 1) * 128], HT[:, k, T * 512:(T + 1) * 512], k == 0, k == 7,
                                R=BHs + [Bw], W=[BU] if k == 0 else (), WA=() if k == 0 else [BU])
                    Bgb = self.B(f"f_gb{par}")
                    Bc = self.B(f"f_carry{fc}")
                    if T == 0:
                        self.MEMSET("pool", gb[par][:, 0:2], 0.0, W=[Bgb])
                    else:
                        self.CP("pool", gb[par][:, 0:2], carry[:, fc, :], R=[Bc], W=[Bgb])
                    self.CP("act", gb[par][:, 2:514], psG, R=[BG], WA=[Bgb])
                    self.CP("pool", carry[:, fc, :], gb[par][:, 512:514], R=[Bgb], W=[Bc])
                    Btb = self.B(f"f_tb{par}")
                    self.TS("pool", tb[par], gb[par][:, 2:514], cw[:, fi, 2:3], cb[:, fi:fi + 1], ALU.mult, ALU.add, R=[Bgb, cB], W=[Btb])
                    self.STT(tb[par], gb[par][:, 1:513], cw[:, fi, 1:2], tb[par], ALU.mult, ALU.add, R=[Bgb, cB, Btb], WA=[Btb])
                    self.STT(tb[par], gb[par][:, 0:512], cw[:, fi, 0:1], tb[par], ALU.mult, ALU.add, R=[Bgb, cB, Btb], WA=[Btb])
                    Bsl = self.B(f"f_sl{par}")
                    self.ACT(sl_[par], tb[par], AF.Silu, R=[Btb], W=[Bsl])
                    self.TT("dve", actT[ap_][:, fc, :], sl_[par], psU, ALU.mult, R=[Bsl, BU], W=[Bact] if fc == 0 else (), WA=() if fc == 0 else [Bact])
                for tb_ in range(4):
                    t = 4 * T + tb_
                    p2 = t % 2
                    BXt = self.B(f"X{t}")
                    for dh in range(2):
                        bk = 4 + 2 * p2 + dh
                        Bk = self.B(f"bank{bk}")
                        for fc in range(nch):
                            self.MM(self.pbank(bk), actT[ap_][:, fc, tb_ * 128:(tb_ + 1) * 128], Wout[s_][:, fc, dh * 512:(dh + 1) * 512],
                                    fc == 0, fc == nch - 1, R=[Bact, Bwo], W=[Bk] if fc == 0 else (), WA=() if fc == 0 else [Bk])
                        xs = X[:, t, dh * 512:(dh + 1) * 512]
                        self.TT("dve", xs, xs, self.pbank(bk), ALU.add, R=[Bk, BXt], WA=[BXt])

    def emit_ret(self, li):
        j = li // 2
        cB = self.B("consts")
        X, HT = self.X, self.HT
        WA = self.carve([8, 1536], BF16)
        WB = [self.carve([4, D], BF16) for _ in range(2)]
        St = self.carve([2, 512], F32)
        Sbf = [self.carve([2, 512], BF16) for _ in range(2)]
        rt = [self.carve([2, 128], F32) for _ in range(4)]
        qkr = [self.carve([2, 2, 128], BF16) for _ in range(2)]
        vbf = [self.carve([512], BF16) for _ in range(2)]
        sg = [self.carve([512], F32) for _ in range(2)]
        kd = [self.carve([256], BF16) for _ in range(2)]
        qkT = [self.carve([4, 128], BF16) for _ in range(2)]
        innT = [self.carve([128], BF16) for _ in range(2)]
        gated = [self.carve([512], BF16) for _ in range(2)]
        goT = [self.carve([4, 128], BF16) for _ in range(2)]
        junk = self.carve([512], BF16)
        sm = self.carve([16], F32)
        self.emit_norm(self.nmg[:, li * 8:(li + 1) * 8])
        wq = self.r_wqkvg[self.ret_js.index(j)]
        wo = self.r_wo[self.ret_js.index(j)]
        rcos = self.rcs[:, 0]
        rsin = self.rcs[:, 1]
        Brcs = self.B("rcs")
        gam = [1.0 - 2.0 ** (-5.0 - h) for h in range(4)]

        def load_w(h):
            blocks = [(h * 256, 256, 0), (1024 + h * 256, 256, 256), (2048 + h * 512, 512, 512), (4096 + h * 512, 512, 1024)]
            names = ["r_Wqk", "r_Wqk", "r_Wv", "r_Wg"]
            first = {"r_Wqk": True, "r_Wv": True, "r_Wg": True}
            for (c0, n, d0), nm in zip(blocks, names):
                src = wq[:, c0:c0 + n].rearrange("(k p) c -> p k c", p=128)
                Bw = self.B(nm)
                self.DMA("pool", WA[:, :, d0:d0 + n], src, nm, W=[Bw] if first[nm] else (), WA=() if first[nm] else [Bw])
                first[nm] = False
            s_ = h % 2
            self.DMA("pool", WB[s_], wo[h * 512:(h + 1) * 512, :].rearrange("(c p) d -> p c d", p=128), f"r_WB{s_}", W=[self.B(f"r_WB{s_}")])

        load_w(0)
        for h in range(4):
            s_ = h % 2
            Bwo = self.B(f"r_WB{s_}")
            cd = gam[h] ** 128
            qd = self.rcol[:, h:h + 1]
            qd2 = self.rcol[:, 4 + h:5 + h]
            kdc = self.rcol[:, 8 + h:9 + h]
            maskT = self.rmask[:, h, :]

            def stageA(n):
                p = n % 2
                tsl = slice(n * 128, (n + 1) * 128)
                BHt = self.B(f"HT{n}")
                names = ["r_Wqk", "r_Wv", "r_Wg"]
                for b in range(3):
                    Bk = self.B(f"bank{b}")
                    Bw = self.B(names[b])
                    for k in range(8):
                        self.MM(self.pbank(b), HT[:, k, tsl], WA[:, k, b * 512:(b + 1) * 512], k == 0, k == 7, R=[BHt, Bw],
                                W=[Bk] if k == 0 else (), WA=() if k == 0 else [Bk])
                B0, B1, B2 = self.B("bank0"), self.B("bank1"), self.B("bank2")
                v4 = self.pbank(0).rearrange("p (a i e) -> p a i e", i=128, e=2)
                E = v4[:, :, :, 0]
                O = v4[:, :, :, 1]
                cb_ = rcos[:, n, :].unsqueeze(1).to_broadcast([128, 2, 128])
                sb_ = rsin[:, n, :].unsqueeze(1).to_broadcast([128, 2, 128])
                Brt = self.B("r_rt")
                self.TT("dve", rt[0], E, cb_, ALU.mult, R=[B0, Brcs], W=[Brt])
                self.TT("dve", rt[1], O, sb_, ALU.mult, R=[B0, Brcs], WA=[Brt])
                self.TT("dve", rt[2], O, cb_, ALU.mult, R=[B0, Brcs], WA=[Brt])
                self.TT("dve", rt[3], E, sb_, ALU.mult, R=[B0, Brcs], WA=[Brt])
                Bqr = self.B(f"r_qkr{p}")
                self.TT("dve", qkr[p][:, :, 0, :], rt[0], rt[1], ALU.subtract, R=[Brt], W=[Bqr])
                self.TT("dve", qkr[p][:, :, 1, :], rt[2], rt[3], ALU.add, R=[Brt], WA=[Bqr])
                self.CP("act", vbf[p], self.pbank(1), R=[B1], W=[self.B(f"r_v{p}")])
                self.ACT(sg[p], self.pbank(2), AF.Silu, R=[B2], W=[self.B(f"r_sg{p}")])
                kflat = qkr[p][:, 1].rearrange("p a b -> p (a b)")
                self.ACT(kd[p], kflat, AF.Copy, R=[Bqr, cB], W=[self.B(f"r_kd{p}")], scale=kdc)

            def stageA2(n):
                p = n % 2
                Bqr = self.B(f"r_qkr{p}")
                ptb = self.pbank(3, BF16)[:, 0:512].rearrange("p (i t) -> p i t", t=128)
                Bpt = self.B("bank3a")
                qflat = qkr[p].rearrange("p a b c -> p (a b c)")
                for i in range(4):
                    self.TR(ptb[:, i, :], qflat[:, i * 128:(i + 1) * 128], R=[Bqr], W=[Bpt] if i == 0 else (), WA=() if i == 0 else [Bpt])
                self.CP("act", qkT[p], ptb, R=[Bpt], W=[self.B(f"r_qkT{p}")])

            def stageB(n):
                p = n % 2
                BqkT = self.B(f"r_qkT{p}")
                Bv = self.B(f"r_v{p}")
                pin = self.pbank(3)[:, 256:384]
                Bin = self.B("bank3b")
                for dc in range(2):
                    self.MM(pin, qkT[p][:, 2 + dc, :], qkT[p][:, dc, :], dc == 0, dc == 1, R=[BqkT], W=[Bin] if dc == 0 else (), WA=() if dc == 0 else [Bin])
                Bit = self.B(f"r_innT{p}")
                self.TT("dve", innT[p], pin, maskT, ALU.mult, R=[Bin, cB], W=[Bit])
                BO = self.B("bank4")
                pO = self.pbank(4)
                sp_ = (n - 1) % 2
                self.MM(pO, innT[p], vbf[p], True, n == 0, R=[Bit, Bv], W=[BO])
                if n > 0:
                    Bs = self.B(f"r_Sbf{sp_}")
                    for dc in range(2):
                        self.MM(pO, qkT[p][:, dc, :], Sbf[sp_][:, dc, :], False, dc == 1, R=[BqkT, Bs], WA=[BO])
                Bkd = self.B(f"r_kd{p}")
                BSt = self.B("r_St")
                if n < NT - 1:
                    for dc in range(2):
                        Bd = self.B(f"bank{5 + dc}")
                        self.MM(self.pbank(5 + dc), kd[p][:, dc * 128:(dc + 1) * 128], vbf[p], True, True, R=[Bkd, Bv], W=[Bd])
                        if n == 0:
                            self.CP("dve", St[:, dc, :], self.pbank(5 + dc), R=[Bd], W=[BSt] if dc == 0 else (), WA=() if dc == 0 else [BSt])
                        else:
                            self.STT(St[:, dc, :], St[:, dc, :], cd, self.pbank(5 + dc), ALU.mult, ALU.add, R=[Bd, BSt], WA=[BSt])
                    self.CP("pool", Sbf[p], St, R=[BSt], W=[self.B(f"r_Sbf{p}")])
                Bsm = self.B(f"r_sm{p}")
                o_ = p * 8
                self.ACT(junk, pO, AF.Square, R=[BO], W=[Bsm], WA=[self.B("r_junk")], accum=sm[:, o_:o_ + 1])
                self.TT("dve", sm[:, o_ + 1:o_ + 2], sm[:, o_:o_ + 1], qd2, ALU.mult, R=[Bsm, cB], WA=[Bsm])
                self.emit_rstd(sm[:, o_ + 2:o_ + 3], sm[:, o_ + 1:o_ + 2], 1.0, R=[Bsm], W=[self.B(f"r_rs{p}")])
                self.TT("dve", sm[:, o_ + 3:o_ + 4], sm[:, o_ + 2:o_ + 3], qd, ALU.mult, R=[self.B(f"r_rs{p}"), cB], W=[self.B(f"r_rq{p}")])
                Bg = self.B(f"r_gated{p}")
                self.STT(gated[p], pO, sm[:, o_ + 3:o_ + 4], sg[p], ALU.mult, ALU.mult, R=[BO, self.B(f"r_rq{p}"), self.B(f"r_sg{p}")], W=[Bg])

            def stageB2(n):
                p = n % 2
                Bg = self.B(f"r_gated{p}")
                ptb = self.pbank(7, BF16)[:, 0:512].rearrange("p (i t) -> p i t", t=128)
                Bpt = self.B("bank7")
                for i in range(4):
                    self.TR(ptb[:, i, :], gated[p][:, i * 128:(i + 1) * 128], R=[Bg], W=[Bpt] if i == 0 else (), WA=() if i == 0 else [Bpt])
                BgT = self.B(f"r_goT{p}")
                self.CP("act", goT[p], ptb, R=[Bpt], W=[BgT])
                BXt = self.B(f"X{n}")
                tslx = n
                for dh in range(2):
                    Bk = self.B("bank6") if dh == 0 else self.B("bank5")
                    bk = 6 if dh == 0 else 5
                    for ec in range(4):
                        self.MM(self.pbank(bk), goT[p][:, ec, :], WB[s_][:, ec, dh * 512:(dh + 1) * 512], ec == 0, ec == 3, R=[BgT, Bwo],
                                W=[Bk] if ec == 0 else (), WA=() if ec == 0 else [Bk])
                    xs = X[:, tslx, dh * 512:(dh + 1) * 512]
                    self.TT("dve", xs, xs, self.pbank(bk), ALU.add, R=[Bk, BXt], WA=[BXt])

            stageA(0)
            stageA2(0)
            for n in range(NT):
                if n + 1 < NT:
                    stageA(n + 1)
                    if n + 1 == NT - 1 and h + 1 < 4:
                        load_w(h + 1)
                stageB(n)
                if n + 1 < NT:
                    stageA2(n + 1)
                if n >= 1:
                    stageB2(n - 1)
            stageB2(NT - 1)

    def emit_out(self, s):
        junk = self.carve([D], BF16)
        ob = [self.carve([D], F32) for _ in range(2)]
        ss = self.carve([4], F32)
        gfull = self.carve([D], F32)
        Bg = self.B("o_g")
        self.DMA("sp", gfull, self.fng_d, "o_g", W=[Bg])
        for t in range(NT):
            p = t % 2
            BXt = self.B(f"X{t}")
            Bob = self.B(f"o_b{p}")
            if self.final_norm:
                Bss = self.B(f"o_ss{p}")
                self.ACT(junk, self.X[:, t, :], AF.Square, R=[BXt], W=[Bss], WA=[self.B("o_junk")], accum=ss[:, p:p + 1])
                self.emit_rstd(ss[:, 2 + p:3 + p], ss[:, p:p + 1], 1.0 / D, R=[Bss], W=[self.B(f"o_rs{p}")])
                self.STT(ob[p], self.X[:, t, :], ss[:, 2 + p:3 + p], gfull, ALU.mult, ALU.mult, R=[BXt, self.B(f"o_rs{p}"), Bg], W=[Bob])
            else:
                self.CP("dve", ob[p], self.X[:, t, :], R=[BXt], W=[Bob])
            self.DMA("sp", self.out_d[s, t * 128:(t + 1) * 128, :], ob[p], "out", R=[Bob], WA=[self.B("out")])


def _consts():
    ident = np.eye(128, dtype=np.float32)
    cm = (np.arange(128)[:, None] <= np.arange(128)[None, :]).astype(np.float32)
    cbf = np.concatenate([ident, cm], axis=1).astype(ml_dtypes.bfloat16)
    afreq = (500000.0 ** (-np.arange(0, 16, 2, dtype=np.float32) / np.float32(16))).astype(np.float32)
    rfreq = (1.0 / (10000.0 ** np.linspace(0.0, 1.0, 128, dtype=np.float32))).astype(np.float32)
    cf = np.zeros((128, 664), np.float32)
    cf[:, 0:8] = afreq[None, :]
    cf[:, 8:136] = rfreq[None, :]
    idx = np.arange(128, dtype=np.float64)
    for h in range(4):
        gam = 1.0 - 2.0 ** (-5.0 - h)
        m = np.where(idx[None, :] >= idx[:, None], (gam ** (-(idx[:, None] + 1.0))) / 16.0, 0.0)
        cf[:, 136 + h * 128:136 + (h + 1) * 128] = m
        qd = gam ** (idx + 1.0)
        cf[:, 648 + h] = qd
        cf[:, 652 + h] = qd * qd / 512.0
        cf[:, 656 + h] = gam ** (127.0 - idx) / 16.0
    return cbf, cf


_NC_CACHE = {}


def _get_nc(layers, nseq, final_norm):
    key = (tuple(layers), nseq, final_norm)
    if key not in _NC_CACHE:
        _NC_CACHE[key] = Builder(list(layers), nseq, final_norm).build()
    return _NC_CACHE[key]


PLAN = [[0, 1, 2, 3]]


def _norm_layers(layers):
    return tuple(e if isinstance(e, tuple) else (e, True, True) for e in layers)


def _run(inputs, layers=None, final_norm=True, plan=None):
    f = lambda a: np.ascontiguousarray(np.asarray(a, dtype=np.float32))
    x = f(inputs["x"])
    pos = np.ascontiguousarray(np.asarray(inputs["positions"], dtype=np.int32))
    cbf, cf = _consts()
    fm = lambda g: np.ascontiguousarray(f(g).reshape(DEPTH, 8, 128).transpose(2, 0, 1).reshape(128, DEPTH * 8))
    lam = np.concatenate([f(inputs["attn_lambda_q1"]), f(inputs["attn_lambda_k1"]),
                          f(inputs["attn_lambda_q2"]), f(inputs["attn_lambda_k2"])], axis=1)
    lam = np.ascontiguousarray(np.broadcast_to(lam.reshape(1, 512), (128, 512)))
    subln = np.ascontiguousarray(f(inputs["attn_subln_g"]).T)
    cw = f(inputs["ffn_conv_w"]).reshape(DEPTH, 3, NFC, 128).transpose(3, 0, 2, 1)
    cw = np.ascontiguousarray(cw.reshape(128, DEPTH * NFC * 3))
    cb = np.ascontiguousarray(f(inputs["ffn_conv_b"]).reshape(DEPTH, NFC, 128).transpose(2, 0, 1).reshape(128, DEPTH * NFC))
    small = dict(
        nmg=fm(inputs["norm_mix_g"]), nfg=fm(inputs["norm_ffn_g"]),
        fng=np.ascontiguousarray(np.broadcast_to(f(inputs["final_norm_g"]).reshape(1, D), (128, D))),
        lam_in=lam, subln=subln, f_cw=cw, f_cb=cb, c_bf=cbf, c_f32=cf,
    )
    if plan is None:
        plan = [list(layers)] if layers is not None else PLAN
    pcs = [np.ascontiguousarray(pos[c * NSEQ:(c + 1) * NSEQ].reshape(NSEQ, NT, 128).transpose(0, 2, 1)) for c in range(N_CORES)]
    cur = x
    for li_, lay in enumerate(plan):
        lay = _norm_layers(lay)
        fn = final_norm and (li_ == len(plan) - 1)
        key = (lay, NSEQ, fn)
        if key not in _NC_CACHE:
            b = Builder(list(lay), NSEQ, fn)
            _NC_CACHE[key] = (b.build(), b)
        nc, b = _NC_CACHE[key]
        shared = dict(small)
        if b.attn_js:
            shared["a_wqkv"] = f(inputs["attn_w_qkv"])[b.attn_js]
            shared["a_wo"] = f(inputs["attn_w_o"])[b.attn_js]
        if b.ret_js:
            shared["r_wqkvg"] = f(inputs["ret_w_qkvg"])[b.ret_js]
            shared["r_wo"] = f(inputs["ret_w_o"])[b.ret_js]
        if b.ffn_ls:
            shared["f_win"] = f(inputs["ffn_w_in"])[b.ffn_ls]
            shared["f_wout"] = f(inputs["ffn_w_out"])[b.ffn_ls]
        in_maps = []
        for c in range(N_CORES):
            m = dict(shared)
            m["x"] = np.ascontiguousarray(cur[c * NSEQ:(c + 1) * NSEQ])
            m["pos"] = pcs[c]
            in_maps.append(m)
        res = run_bass_kernel_spmd(nc, in_maps, core_ids=list(range(N_CORES)))
        cur = np.concatenate([r["out"] for r in res.results], axis=0)
    return cur


def kernel(**inputs):
    return _run(inputs)
```

```python
import math
import numpy as np
import ml_dtypes
import concourse.bass as bass
import concourse.mybir as mybir
from concourse.bass_utils import run_bass_kernel_spmd

dt = mybir.dt
F32, BF16, I32 = dt.float32, dt.bfloat16, dt.int32
AF = mybir.ActivationFunctionType
ALU = mybir.AluOpType
COMPUTE = ("pe", "act", "dve", "pool")

D = 1024
S = 2048
NT = 16
NSEQ = 2
DEPTH = 4
FFN = 2816
NFC = 22
EPS = 1e-6
N_CORES = 8
PI = math.pi
EPOCH = 1024


class Buf:
    __slots__ = ("name", "w", "r")

    def __init__(self, name):
        self.name = name
        self.w = {}
        self.r = {}


class Op:
    __slots__ = ("eng", "fn", "deps", "sig", "val", "key", "is_dma", "order", "epoch")

    def __init__(self, eng, fn, is_dma=False, key=None):
        self.eng = eng
        self.fn = fn
        self.deps = {}
        self.sig = False
        self.val = 0
        self.key = key if key is not None else eng
        self.is_dma = is_dma


class Prog:
    def __init__(self):
        self.streams = {e: [] for e in ("pe", "act", "dve", "pool", "sp")}
        self.dma_cnt = {}
        self.all_ops = []

    def op(self, eng, fn, R=(), W=(), WA=()):
        o = Op(eng, fn)
        self._track(o, R, W, WA)
        return o

    def dma(self, eng, fn, key, R=(), W=(), WA=()):
        o = Op(eng, fn, is_dma=True, key="dma:" + key)
        self.dma_cnt[key] = self.dma_cnt.get(key, 0) + 1
        o.val = 16 * self.dma_cnt[key]
        o.sig = True
        self._track(o, R, W, WA)
        return o

    def _track(self, o, R, W, WA):
        o.order = len(self.all_ops)
        self.all_ops.append(o)
        self.streams[o.eng].append(o)
        for b in R:
            for s in b.w.values():
                self._add(o, s, True)
        for b in list(W) + list(WA):
            for s in b.w.values():
                self._add(o, s, False)
            for s in b.r.values():
                self._add(o, s, False)
        for b in R:
            b.r[o.key] = o
        for b in W:
            b.w = {o.key: o}
            b.r = {}
        for b in WA:
            b.w[o.key] = o

    def _add(self, o, s, raw):
        if s is o:
            return
        if (not s.is_dma) and (not o.is_dma) and s.eng == o.eng:
            if not raw or s.eng == "pe":
                return
        cur = o.deps.get(s.key)
        if cur is None or cur.order < s.order:
            o.deps[s.key] = s

    def finalize(self):
        for o in self.all_ops:
            for s in o.deps.values():
                s.sig = True
        cnt = {e: 0 for e in COMPUTE}
        for o in self.all_ops:
            if not o.is_dma and o.sig:
                cnt[o.eng] += 1
                o.epoch = (cnt[o.eng] - 1) // EPOCH
                o.val = (cnt[o.eng] - 1) % EPOCH + 1
        self.n_epochs = {e: (cnt[e] + EPOCH - 1) // EPOCH for e in COMPUTE}

    def replay(self, name, eng, sems, dma_sems):
        waited = {}
        for o in self.streams[name]:
            for k, s in o.deps.items():
                if s.is_dma:
                    if waited.get(k, 0) < s.val:
                        eng.wait_ge(dma_sems[k[4:]], s.val)
                        waited[k] = s.val
                else:
                    tv = (s.epoch, s.val)
                    if waited.get(k, (-1, 0)) < tv:
                        eng.wait_ge(sems[(s.eng, s.epoch)], s.val)
                        waited[k] = tv
            if o.fn is None:
                continue
            ins = o.fn(eng)
            if o.is_dma:
                ins.then_inc(dma_sems[o.key[4:]], 16)
            elif o.sig:
                ins.then_inc(sems[(o.eng, o.epoch)], 1)


class Builder:
    def __init__(self, layers, nseq, final_norm):
        self.layers = layers
        self.nseq = nseq
        self.final_norm = final_norm
        self.P = Prog()
        self.nc = bass.Bass("TRN2", target_bir_lowering=False)
        self.bufs = {}

    def B(self, name):
        b = self.bufs.get(name)
        if b is None:
            b = self.bufs[name] = Buf(name)
        return b

    def Bs(self, names):
        return [self.B(n) for n in names]

    def xb(self, *aps):
        out = []
        for a in aps:
            try:
                if a.tensor.name != "psum":
                    continue
            except AttributeError:
                continue
            es = 4 if a.dtype in (F32, I32) else 2
            b = (a.offset * es) // 2048
            bb = self.B(f"xbank{b}")
            if bb not in out:
                out.append(bb)
        return out

    def dram_in(self, name, shape, d=F32):
        return self.nc.dram_tensor(name, list(shape), d, kind="ExternalInput").ap()

    def MM(self, out, lhsT, rhs, start, stop, R, W=(), WA=()):
        self.P.op("pe", lambda e: e.matmul(out, lhsT=lhsT, rhs=rhs, start=start, stop=stop,
                                           skip_group_check=True), R=R, W=list(W) + self.xb(out), WA=WA)

    def TR(self, out, in_, R, W=(), WA=()):
        ident = self.ident
        self.P.op("pe", lambda e: e.transpose(out=out, in_=in_, identity=ident), R=list(R) + [self.B("consts")], W=list(W) + self.xb(out), WA=WA)

    def ACT(self, out, in_, func, R, W=(), WA=(), scale=None, bias=None, accum=None):
        kw = {}
        if scale is not None:
            kw["scale"] = scale
        if bias is not None:
            kw["bias"] = bias
        if accum is not None:
            kw["accum_out"] = accum
        self.P.op("act", lambda e: e.activation(out=out, in_=in_, func=func, **kw), R=R, W=list(W) + self.xb(out, in_), WA=WA)

    def TT(self, eng, out, in0, in1, op, R, W=(), WA=()):
        self.P.op(eng, lambda e: e.tensor_tensor(out=out, in0=in0, in1=in1, op=op), R=R, W=list(W) + self.xb(out, in0, in1), WA=WA)

    def TS(self, eng, out, in0, s1, s2, op0, op1, R, W=(), WA=()):
        if op1 is None:
            self.P.op(eng, lambda e: e.tensor_scalar(out=out, in0=in0, scalar1=s1, scalar2=None, op0=op0), R=R, W=list(W) + self.xb(out, in0), WA=WA)
        else:
            self.P.op(eng, lambda e: e.tensor_scalar(out=out, in0=in0, scalar1=s1, scalar2=s2, op0=op0, op1=op1), R=R, W=list(W) + self.xb(out, in0), WA=WA)

    def STT(self, out, in0, scalar, in1, op0, op1, R, W=(), WA=(), accum=None):
        if accum is None:
            self.P.op("dve", lambda e: e.scalar_tensor_tensor(out=out, in0=in0, scalar=scalar, in1=in1, op0=op0, op1=op1), R=R, W=list(W) + self.xb(out, in0, in1), WA=WA)
        else:
            self.P.op("dve", lambda e: e.scalar_tensor_tensor(out=out, in0=in0, scalar=scalar, in1=in1, op0=op0, op1=op1, accum_out=accum), R=R, W=list(W) + self.xb(out, in0, in1), WA=WA)

    def CP(self, eng, out, in_, R, W=(), WA=()):
        if eng == "act":
            self.P.op("act", lambda e: e.copy(out=out, in_=in_), R=R, W=list(W) + self.xb(out, in_), WA=WA)
        else:
            self.P.op(eng, lambda e: e.tensor_copy(out=out, in_=in_), R=R, W=list(W) + self.xb(out, in_), WA=WA)

    def MEMSET(self, eng, ap, val, W=(), WA=()):
        self.P.op(eng, lambda e: e.memset(ap, val), W=W, WA=WA)

    def DMA(self, eng, out, in_, key, R=(), W=(), WA=()):
        self.P.dma(eng, lambda e: e.dma_start(out=out, in_=in_), key, R=R, W=W, WA=WA)

    def barrier(self):
        allb = list(self.bufs.values())
        P = self.P
        for e in ("pe", "act", "dve", "pool", "sp"):
            o = Op(e, None)
            o.order = len(P.all_ops)
            for b in allb:
                for src in list(b.w.values()) + list(b.r.values()):
                    if src.fn is None:
                        continue
                    if (not src.is_dma) and src.eng == e:
                        continue
                    cur = o.deps.get(src.key)
                    if cur is None or cur.order < src.order:
                        o.deps[src.key] = src
            P.all_ops.append(o)
            P.streams[e].append(o)

    def arena_reset(self):
        self.aoff = 0

    def carve(self, shape, d):
        n = 1
        for v in shape:
            n *= v
        nbytes = n * (4 if d in (F32, I32) else 2)
        nbytes = (nbytes + 31) // 32 * 32
        o2 = self.aoff // 2
        assert self.aoff + nbytes <= self.arena_bytes, (self.aoff, nbytes, self.arena_bytes)
        v = self.arena[:, o2:o2 + nbytes // 2]
        self.aoff += nbytes
        if d != BF16:
            v = v.bitcast(d)
        v = v[:, 0:n]
        if len(shape) == 2:
            v = v.rearrange("p (a b) -> p a b", b=shape[1])
        elif len(shape) == 3:
            v = v.rearrange("p (a b c) -> p a b c", b=shape[1], c=shape[2])
        return v

    def pbank(self, i, d=F32):
        v = self.psum[:, i * 512:(i + 1) * 512]
        if d == BF16:
            v = v.bitcast(BF16)
        return v

    def build(self):
        nc = self.nc
        ns = self.nseq
        self.x_d = self.dram_in("x", [ns, S, D])
        self.pos_d = self.dram_in("pos", [ns, 128, NT], I32)
        self.nmg_d = self.dram_in("nmg", [128, DEPTH * 8])
        self.nfg_d = self.dram_in("nfg", [128, DEPTH * 8])
        self.fng_d = self.dram_in("fng", [128, D])
        self.lam_d = self.dram_in("lam_in", [128, 2 * 256])
        self.subln_d = self.dram_in("subln", [128, 2])
        self.cw_d = self.dram_in("f_cw", [128, DEPTH * NFC * 3])
        self.cb_d = self.dram_in("f_cb", [128, DEPTH * NFC])
        ents = [e if isinstance(e, tuple) else (e, True, True) for e in self.layers]
        self.attn_js = sorted({li // 2 for li, m, f in ents if m and li % 2 == 0})
        self.ret_js = sorted({li // 2 for li, m, f in ents if m and li % 2 == 1})
        self.ffn_ls = sorted({li for li, m, f in ents if f})
        if self.attn_js:
            self.a_wqkv = self.dram_in("a_wqkv", [len(self.attn_js), D, 3 * D])
            self.a_wo = self.dram_in("a_wo", [len(self.attn_js), D, D])
        if self.ret_js:
            self.r_wqkvg = self.dram_in("r_wqkvg", [len(self.ret_js), D, 6144])
            self.r_wo = self.dram_in("r_wo", [len(self.ret_js), 2048, D])
        if self.ffn_ls:
            self.f_win = self.dram_in("f_win", [len(self.ffn_ls), D, 2 * FFN])
            self.f_wout = self.dram_in("f_wout", [len(self.ffn_ls), FFN, D])
        self.cbf_d = self.dram_in("c_bf", [128, 256], BF16)
        self.cf_d = self.dram_in("c_f32", [128, 8 + 128 + 512 + 16])
        self.out_d = nc.dram_tensor("out", [ns, S, D], F32, kind="ExternalOutput").ap()

        self.X = nc.alloc_sbuf_tensor("X", [128, NT, D], F32)[:]
        self.HT = nc.alloc_sbuf_tensor("HT", [128, 8, S], BF16)[:]
        self.cbf = nc.alloc_sbuf_tensor("cbf", [128, 256], BF16)[:]
        self.cf = nc.alloc_sbuf_tensor("cf", [128, 664], F32)[:]
        self.ident = self.cbf[:, 0:128]
        self.cmask = self.cbf[:, 128:256]
        self.afreq = self.cf[:, 0:8]
        self.rfreq = self.cf[:, 8:136]
        self.rmask = self.cf[:, 136:648].rearrange("p (h i) -> p h i", i=128)
        self.rcol = self.cf[:, 648:664]
        self.params = nc.alloc_sbuf_tensor("params", [128, 32 + 32 + 512 + 2 + 264 + 88], F32)[:]
        o = 0
        self.nmg = self.params[:, o:o + 32]; o += 32
        self.nfg = self.params[:, o:o + 32]; o += 32
        self.lamin = self.params[:, o:o + 512]; o += 512
        self.subln = self.params[:, o:o + 2]; o += 2
        self.cw = self.params[:, o:o + 264]; o += 264
        self.cb = self.params[:, o:o + 88]; o += 88
        self.rcs = nc.alloc_sbuf_tensor("rcs", [128, 2, NT, 128], F32)[:]
        self.acs = nc.alloc_sbuf_tensor("acs", [128, 2, NT, 8], F32)[:]
        self.posf = nc.alloc_sbuf_tensor("posf", [128, NT], F32)[:]
        self.posi = nc.alloc_sbuf_tensor("posi", [128, NT], I32)[:]
        self.small = nc.alloc_sbuf_tensor("small", [128, 64], F32)[:]
        self.arena_bytes = (nc.sbuf_bytes_remaining - 256) // 64 * 64
        self.arena = nc.alloc_sbuf_tensor("arena", [128, self.arena_bytes // 2], BF16)[:]
        self.psum = nc.alloc_psum_tensor("psum", [128, 4096], F32)[:]

        cB = self.B("consts")
        self.DMA("sp", self.cbf, self.cbf_d, "consts", W=[cB])
        self.DMA("sp", self.cf, self.cf_d, "consts", WA=[cB])
        self.DMA("sp", self.nmg, self.nmg_d, "consts", WA=[cB])
        self.DMA("sp", self.nfg, self.nfg_d, "consts", WA=[cB])
        self.DMA("sp", self.lamin, self.lam_d, "consts", WA=[cB])
        self.DMA("sp", self.subln, self.subln_d, "consts", WA=[cB])
        self.DMA("sp", self.cw, self.cw_d, "consts", WA=[cB])
        self.DMA("sp", self.cb, self.cb_d, "consts", WA=[cB])
        self.MEMSET("dve", self.small[:, 0:1], EPS, WA=[cB])

        for s in range(ns):
            self.emit_seq(s)
        self.P.op("sp", None, R=[self.B("out")])
        self.P.finalize()

        from contextlib import ExitStack
        with ExitStack() as es:
            sems = {(e, k): es.enter_context(nc.semaphore(f"s_{e}{k}")) for e in COMPUTE for k in range(self.P.n_epochs[e])}
            dsems = {k: es.enter_context(nc.semaphore("d_" + k)) for k in self.P.dma_cnt}
            P = self.P
            with nc.Block() as block:
                @block.tensor
                def _(e):
                    P.replay("pe", e, sems, dsems)

                @block.scalar
                def _(e):
                    P.replay("act", e, sems, dsems)

                @block.vector
                def _(e):
                    P.replay("dve", e, sems, dsems)

                @block.gpsimd
                def _(e):
                    P.replay("pool", e, sems, dsems)

                @block.sync
                def _(e):
                    P.replay("sp", e, sems, dsems)
        return nc

    def emit_seq(self, s):
        BX = [self.B(f"X{t}") for t in range(NT)]
        self.barrier()
        for t in range(NT):
            self.DMA("sp", self.X[:, t, :], self.x_d[s, t * 128:(t + 1) * 128, :], f"X{t}", W=[BX[t]])
        self.DMA("sp", self.posi, self.pos_d[s], "pos", W=[self.B("posi")])
        self.CP("dve", self.posf, self.posi, R=[self.B("posi")], W=[self.B("posf")])
        self.arena_reset()
        self.emit_sincos(self.afreq, 8, self.acs, "acs")
        self.arena_reset()
        self.emit_sincos(self.rfreq, 128, self.rcs, "rcs")
        for ent in self.layers:
            li, do_mix, do_ffn = ent if isinstance(ent, tuple) else (ent, True, True)
            if do_mix:
                self.barrier()
                self.arena_reset()
                if li % 2 == 0:
                    self.emit_attn(li)
                else:
                    self.emit_ret(li)
            if do_ffn:
                self.barrier()
                self.arena_reset()
                self.emit_ffn(li)
        self.barrier()
        self.arena_reset()
        self.emit_out(s)

    def emit_sincos(self, freq, F, dst, name):
        cB = self.B("consts")
        Bt = self.B("sc_tmp")
        Bd = self.B(name)
        ang = self.carve([NT, F], F32)
        ki = self.carve([NT, F], I32)
        kf = self.carve([NT, F], F32)
        y = self.carve([NT, F], F32)
        fb = freq.unsqueeze(1).to_broadcast([128, NT, F])
        pb = self.posf.unsqueeze(2).to_broadcast([128, NT, F])
        self.TT("dve", ang, fb, pb, ALU.mult, R=[cB, self.B("posf")], W=[Bt])
        self.TS("dve", ki, ang, 1.0 / (2 * PI), None, ALU.mult, None, R=[Bt], WA=[Bt])
        self.CP("dve", kf, ki, R=[Bt], WA=[Bt])
        self.STT(ang, kf, -2 * PI, ang, ALU.mult, ALU.add, R=[Bt], WA=[Bt])
        for idx, shift in ((1, 0.0), (0, PI / 2)):
            self.TS("dve", y, ang, shift, None, ALU.add, None, R=[Bt], WA=[Bt])
            self.TS("dve", kf, y, PI, -2 * PI, ALU.is_gt, ALU.mult, R=[Bt], WA=[Bt])
            self.TT("dve", y, y, kf, ALU.add, R=[Bt], WA=[Bt])
            self.TS("dve", kf, y, -PI, 2 * PI, ALU.is_lt, ALU.mult, R=[Bt], WA=[Bt])
            self.TT("dve", y, y, kf, ALU.add, R=[Bt], WA=[Bt])
            self.ACT(dst[:, idx], y, AF.Sin, R=[Bt], WA=[Bd])

    def emit_rstd(self, out, in_, scale, R, W):
        self.ACT(out, in_, AF.Ln, R=list(R) + [self.B("consts")], W=W, scale=scale, bias=self.small[:, 0:1])
        self.ACT(out, out, AF.Exp, R=W, WA=W, scale=-0.5)

    def emit_norm(self, gcol):
        cB = self.B("consts")
        junk = self.carve([D], BF16)
        junk2 = self.carve([D], BF16)
        xn = [self.carve([D], BF16) for _ in range(2)]
        ss = self.carve([2 * NT], F32)
        gb = gcol.unsqueeze(2).to_broadcast([128, 8, 128])
        Bss = self.B("n_ss")
        for t in range(NT):
            BXt = self.B(f"X{t}")
            first = (t == 0)
            if t % 2 == 0:
                self.ACT(junk, self.X[:, t, :], AF.Square, R=[BXt], W=[Bss] if first else (), WA=[self.B("n_junk")] + ([] if first else [Bss]),
                         accum=ss[:, t:t + 1])
            else:
                self.STT(junk2, self.X[:, t, :], 1.0, self.X[:, t, :], ALU.mult, ALU.mult, R=[BXt], WA=[self.B("n_junk2"), Bss], accum=ss[:, t:t + 1])
        Brs = self.B("n_rs")
        self.emit_rstd(ss[:, NT:2 * NT], ss[:, 0:NT], 1.0 / D, R=[Bss], W=[Brs])
        for t in range(NT):
            p = t % 2
            BXt = self.B(f"X{t}")
            Bxn = self.B(f"n_xn{p}")
            Bpt = self.B(f"bank{4 + p}")
            self.ACT(xn[p], self.X[:, t, :], AF.Copy, R=[BXt, Brs], W=[Bxn], scale=ss[:, NT + t:NT + t + 1])
            pt = self.pbank(4 + p, BF16).rearrange("p (c t) -> p c t", t=128)
            for c in range(8):
                self.TR(pt[:, c, :], xn[p][:, c * 128:(c + 1) * 128], R=[Bxn], W=[Bpt] if c == 0 else (), WA=() if c == 0 else [Bpt])
            self.TT("dve", self.HT[:, :, t * 128:(t + 1) * 128], pt, gb, ALU.mult, R=[Bpt, cB], W=[self.B(f"HT{t}")])

    def emit_attn(self, li):
        j = li // 2
        lambda_init = 0.8 - 0.6 * math.exp(-0.3 * li)
        cB = self.B("consts")
        X, HT = self.X, self.HT
        WA = [self.carve([8, 768], BF16) for _ in range(2)]
        WB = [self.carve([2, D], BF16) for _ in range(2)]
        qT = self.carve([2, S], BF16)
        kT = self.carve([2, S], BF16)
        V = self.carve([NT, 2, 129], BF16)
        onT = self.carve([2, S], BF16)
        Pt = [[self.carve([512], BF16) for _ in range(2)] for _ in range(2)]
        qkf = [self.carve([512], F32) for _ in range(2)]
        qkb = [self.carve([512], BF16) for _ in range(2)]
        rt = [self.carve([8, 8], F32) for _ in range(4)]
        onb4 = [self.carve([4, 128], BF16) for _ in range(2)]
        sm = self.carve([32], F32)
        lamj = self.carve([64], F32)
        Bl = self.B("a_lam")
        li0 = self.lamin[:, j * 256:(j + 1) * 256]
        self.STT(lamj, li0[:, 0:64], 1.0, li0[:, 64:128], ALU.mult, ALU.mult, R=[cB], W=[Bl], accum=sm[:, 0:1])
        self.STT(lamj, li0[:, 128:192], 1.0, li0[:, 192:256], ALU.mult, ALU.mult, R=[cB], WA=[Bl], accum=sm[:, 1:2])
        self.ACT(sm[:, 2:4], sm[:, 0:2], AF.Exp, R=[Bl], WA=[Bl])
        self.TT("dve", sm[:, 4:5], sm[:, 3:4], sm[:, 2:3], ALU.subtract, R=[Bl], WA=[Bl])
        self.TS("dve", sm[:, 5:6], sm[:, 4:5], -lambda_init, None, ALU.add, None, R=[Bl], WA=[Bl])
        neglam = sm[:, 5:6]
        self.TS("dve", sm[:, 6:7], self.subln[:, j:j + 1], 1.0 - lambda_init, None, ALU.mult, None, R=[cB], WA=[Bl])
        sgcol = sm[:, 6:7]
        self.MEMSET("pool", V[:, :, :, 128:129], 1.0, W=[self.B("a_Vones")])

        mark = self.aoff
        self.emit_norm(self.nmg[:, li * 8:(li + 1) * 8])
        self.aoff = mark
        accs = self.carve([3, 387], F32)

        wq = self.a_wqkv[self.attn_js.index(j)]
        wo = self.a_wo[self.attn_js.index(j)]

        def load_w(g):
            sl = g % 2
            Bw = self.B(f"a_WA{sl}")
            for blk in range(3):
                src = wq[:, blk * D + g * 256: blk * D + (g + 1) * 256].rearrange("(k p) c -> p k c", p=128)
                self.DMA("pool", WA[sl][:, :, blk * 256:(blk + 1) * 256], src, f"a_WA{sl}", W=[Bw] if blk == 0 else (), WA=() if blk == 0 else [Bw])
            src = wo[g * 256:(g + 1) * 256, :].rearrange("(k p) c -> p k c", p=128)
            self.DMA("pool", WB[sl], src, f"a_WB{sl}", W=[self.B(f"a_WB{sl}")])

        acos = self.acs[:, 0]
        asin = self.acs[:, 1]
        load_w(0)
        for g in range(4):
            sl = g % 2
            if g + 1 < 4:
                load_w(g + 1)
            Bw = self.B(f"a_WA{sl}")
            Bwo = self.B(f"a_WB{sl}")
            def p1_mm(t):
                p = t % 2
                tsl = slice(t * 128, (t + 1) * 128)
                BHt = self.B(f"HT{t}")
                ps_qk = self.pbank(2 * p)
                ps_v = self.pbank(2 * p + 1)[:, 0:256]
                Bqk = self.B(f"bank{2 * p}")
                Bv = self.B(f"bank{2 * p + 1}")
                for k in range(8):
                    self.MM(ps_qk, HT[:, k, tsl], WA[sl][:, k, 0:512], k == 0, k == 7, R=[BHt, Bw], W=[Bqk] if k == 0 else (), WA=() if k == 0 else [Bqk])
                for k in range(8):
                    self.MM(ps_v, HT[:, k, tsl], WA[sl][:, k, 512:768], k == 0, k == 7, R=[BHt, Bw], W=[Bv] if k == 0 else (), WA=() if k == 0 else [Bv])

            def p1_post(t):
                p = t % 2
                tsl = slice(t * 128, (t + 1) * 128)
                ps_qk = self.pbank(2 * p)
                ps_v = self.pbank(2 * p + 1)[:, 0:256]
                Bqk = self.B(f"bank{2 * p}")
                Bv = self.B(f"bank{2 * p + 1}")
                BVt = self.B(f"a_V{t}")
                self.CP("act", V[:, t, :, 0:128], ps_v.rearrange("p (h e) -> p h e", e=128), R=[Bv, self.B("a_Vones")], W=[BVt])
                Bf = self.B(f"a_qkf{p}")
                self.ACT(qkf[p][:, 0:256], ps_qk[:, 0:256], AF.Copy, R=[Bqk], W=[Bf], scale=0.125)
                self.CP("act", qkf[p][:, 256:512], ps_qk[:, 256:512], R=[Bqk], WA=[Bf])
                v3 = qkf[p].rearrange("p (g d) -> p g d", d=64)
                x1 = v3[:, :, 0:8]
                x2 = v3[:, :, 8:16]
                cb_ = acos[:, t, :].unsqueeze(1).to_broadcast([128, 8, 8])
                sb_ = asin[:, t, :].unsqueeze(1).to_broadcast([128, 8, 8])
                Brt = self.B("a_rt")
                Bacs = self.B("acs")
                self.TT("dve", rt[0], x1, cb_, ALU.mult, R=[Bf, Bacs], W=[Brt])
                self.TT("dve", rt[1], x2, sb_, ALU.mult, R=[Bf, Bacs], WA=[Brt])
                self.TT("dve", rt[2], x2, cb_, ALU.mult, R=[Bf, Bacs], WA=[Brt])
                self.TT("dve", rt[3], x1, sb_, ALU.mult, R=[Bf, Bacs], WA=[Brt])
                self.TT("dve", x1, rt[0], rt[1], ALU.subtract, R=[Brt], WA=[Bf])
                self.TT("dve", x2, rt[2], rt[3], ALU.add, R=[Brt], WA=[Bf])
                Bb = self.B(f"a_qkb{p}")
                self.CP("pool", qkb[p], qkf[p], R=[Bf], W=[Bb])
                ptb = self.pbank(7, BF16)[:, 0:512].rearrange("p (i t) -> p i t", t=128)
                Bpt = self.B("bank7")
                for i in range(4):
                    self.TR(ptb[:, i, :], qkb[p][:, i * 128:(i + 1) * 128], R=[Bb], W=[Bpt] if i == 0 else (), WA=() if i == 0 else [Bpt])
                self.CP("dve", qT[:, :, tsl], ptb[:, 0:2, :], R=[Bpt], W=[self.B(f"a_qT{t}")])
                self.CP("act", kT[:, :, tsl], ptb[:, 2:4, :], R=[Bpt], W=[self.B(f"a_kT{t}")])

            p1_mm(0)
            for t in range(1, NT):
                p1_mm(t)
                p1_post(t - 1)
            p1_post(NT - 1)
            steps = [(hl, qt, kb) for hl in range(2) for qt in range(4) for kb in range(4 * qt + 4)]
            nst = len(steps)
            Bacc = [self.B(f"bank{4 + b}") for b in range(3)]
            Baccs = self.B("a_accs")
            accv = accs.rearrange("p b n -> p (b n)")[:, 0:1032].rearrange("p (a n) -> p a n", n=129)
            of4 = qkf[1].rearrange("p (q e) -> p q e", e=128)
            sq4 = qkf[0].rearrange("p (q e) -> p q e", e=128)
            Bof, Bsq = self.B("a_qkf1"), self.B("a_qkf0")
            state = {"started": [False] * 3}

            def acc_ap(c, qb):
                a = c * 4 + qb
                return self.pbank(4 + a // 3)[:, (a % 3) * 129:(a % 3) * 129 + 129], a // 3

            def st_S(i):
                hl, qt, kb = steps[i]
                par = i % 2
                c0 = max(kb - 4 * qt, 0) * 128
                BqTs = [self.B(f"a_qT{4 * qt + q}") for q in range(4)]
                for c in range(2):
                    self.MM(self.pbank(2 * par + c)[:, c0:512], kT[c * 64:(c + 1) * 64, hl, kb * 128:(kb + 1) * 128],
                            qT[c * 64:(c + 1) * 64, hl, qt * 512 + c0:(qt + 1) * 512], True, True,
                            R=[self.B(f"a_kT{kb}")] + BqTs, W=[self.B(f"bank{2 * par + c}")])

            def st_exp(i):
                hl, qt, kb = steps[i]
                par = i % 2
                jd = kb - 4 * qt
                c0 = max(jd, 0) * 128
                for c in range(2):
                    Bp = self.B(f"a_P{par}{c}")
                    self.ACT(Pt[par][c][:, c0:512], self.pbank(2 * par + c)[:, c0:512], AF.Exp, R=[self.B(f"bank{2 * par + c}")], W=[Bp])
                    if jd >= 0:
                        self.TT("pool", Pt[par][c][:, c0:c0 + 128], Pt[par][c][:, c0:c0 + 128], self.cmask, ALU.mult, R=[Bp, cB], WA=[Bp])

            def st_PV(i):
                hl, qt, kb = steps[i]
                par = i % 2
                jd = kb - 4 * qt
                if kb == 0:
                    state["started"] = [False] * 3
                for c in range(2):
                    Bp = self.B(f"a_P{par}{c}")
                    for qb in range(max(jd, 0), 4):
                        ap_, b = acc_ap(c, qb)
                        st = not state["started"][b]
                        state["started"][b] = True
                        self.MM(ap_, Pt[par][c][:, qb * 128:(qb + 1) * 128], V[:, kb, hl, :], st, kb == 4 * qt + qb,
                                R=[Bp, self.B(f"a_V{kb}")], W=[Bacc[b]] if st else (), WA=() if st else [Bacc[b]])

            def st_norm(hl, qt, dp):
                for b in range(3):
                    n_ = 387 if b < 2 else 258
                    self.CP("dve", accs[:, b, 0:n_], self.pbank(4 + b)[:, 0:n_], R=[Bacc[b]], W=[Baccs] if b == 0 else (), WA=() if b == 0 else [Baccs])
                Bsm = self.B("a_sm")
                rec = sm[:, 8:16]
                self.P.op("dve", lambda e: e.reciprocal(out=rec, in_=accv[:, :, 128]), R=[Baccs], W=[Bsm])
                self.TS("dve", sm[:, 12:16], sm[:, 12:16], neglam, None, ALU.mult, None, R=[Bsm, Bl], WA=[Bsm])
                r1 = sm[:, 8:12].unsqueeze(2).to_broadcast([128, 4, 128])
                r2 = sm[:, 12:16].unsqueeze(2).to_broadcast([128, 4, 128])
                self.TT("dve", of4, accv[:, 0:4, 0:128], r1, ALU.mult, R=[Baccs, Bsm], W=[Bof])
                self.TT("dve", sq4, accv[:, 4:8, 0:128], r2, ALU.mult, R=[Baccs, Bsm], W=[Bsq])
                self.TT("dve", of4, of4, sq4, ALU.add, R=[Bsq], WA=[Bof])
                self.TT("dve", sq4, of4, of4, ALU.mult, R=[Bof], W=[Bsq])
                self.P.op("dve", lambda e: e.tensor_reduce(out=sm[:, 16:20], in_=sq4, axis=mybir.AxisListType.X, op=ALU.add), R=[Bsq], WA=[Bsm])
                Brs = self.B("a_rs")
                self.emit_rstd(sm[:, 20:24], sm[:, 16:20], 1.0 / 128, R=[Bsm], W=[Brs])
                rs = sm[:, 20:24].unsqueeze(2).to_broadcast([128, 4, 128])
                self.TT("dve", onb4[dp], of4, rs, ALU.mult, R=[Bof, Brs], W=[self.B(f"a_on{dp}")])

            def st_norm_pe(hl, qt, dp):
                pt2 = self.pbank(7, BF16)[:, 512:1024].rearrange("p (q t) -> p q t", t=128)
                Bp2 = self.B("bank7b")
                for qb in range(4):
                    self.TR(pt2[:, qb, :], onb4[dp][:, qb, :], R=[self.B(f"a_on{dp}")], W=[Bp2] if qb == 0 else (), WA=() if qb == 0 else [Bp2])
                dst = onT[:, hl, qt * 512:(qt + 1) * 512].rearrange("p (q t) -> p q t", t=128)
                self.TS("dve", dst, pt2, sgcol, None, ALU.mult, None, R=[Bp2, Bl],
                        W=[self.B(f"a_onT{hl}_{4 * qt + q}") for q in range(4)])

            pending = []
            st_S(0)
            for i in range(nst):
                if i + 1 < nst:
                    st_S(i + 1)
                st_exp(i)
                st_PV(i)
                hl, qt, kb = steps[i]
                if kb == 4 * qt + 3:
                    dp = (hl * 4 + qt) % 2
                    st_norm(hl, qt, dp)
                    pending.append((i + 3, hl, qt, dp))
                while pending and pending[0][0] <= i:
                    _, h_, q_, d_ = pending.pop(0)
                    st_norm_pe(h_, q_, d_)
            for _, h_, q_, d_ in pending:
                st_norm_pe(h_, q_, d_)
            for t in range(NT):
                p = t % 2
                tsl = slice(t * 128, (t + 1) * 128)
                for dh in range(2):
                    Bk = self.B(f"bank{2 * p + dh}")
                    for hl in range(2):
                        self.MM(self.pbank(2 * p + dh), onT[:, hl, tsl], WB[sl][:, hl, dh * 512:(dh + 1) * 512], hl == 0, hl == 1,
                                R=[self.B(f"a_onT{hl}_{t}"), Bwo], W=[Bk] if hl == 0 else (), WA=() if hl == 0 else [Bk])
                    BXt = self.B(f"X{t}")
                    xs = X[:, t, dh * 512:(dh + 1) * 512]
                    self.TT("dve", xs, xs, self.pbank(2 * p + dh), ALU.add, R=[Bk, BXt], WA=[BXt])

    def emit_ffn(self, li):
        cB = self.B("consts")
        X, HT = self.X, self.HT
        Win = [self.carve([8, 1024], BF16) for _ in range(2)]
        Wout = [self.carve([4, D], BF16) for _ in range(2)]
        gb = [self.carve([516], F32) for _ in range(2)]
        tb = [self.carve([512], F32) for _ in range(2)]
        sl_ = [self.carve([512], F32) for _ in range(2)]
        actT = [self.carve([4, 512], BF16) for _ in range(2)]
        carry = self.carve([4, 2], F32)
        self.emit_norm(self.nfg[:, li * 8:(li + 1) * 8])
        win = self.f_win[self.ffn_ls.index(li)]
        wout = self.f_wout[self.ffn_ls.index(li)]
        groups = [(0, 4), (512, 4), (1024, 4), (1536, 4), (2048, 4), (2560, 2)]
        cw = self.cw[:, li * 66:(li + 1) * 66].rearrange("p (f j) -> p f j", j=3)
        cb = self.cb[:, li * 22:(li + 1) * 22]

        def load_w(gi):
            f0, nch = groups[gi]
            nf = nch * 128
            s_ = gi % 2
            Bw = self.B(f"f_Win{s_}")
            self.DMA("pool", Win[s_][:, :, 0:nf], win[:, f0:f0 + nf].rearrange("(k p) c -> p k c", p=128), f"f_Win{s_}", W=[Bw])
            self.DMA("pool", Win[s_][:, :, 512:512 + nf], win[:, FFN + f0:FFN + f0 + nf].rearrange("(k p) c -> p k c", p=128), f"f_Win{s_}", WA=[Bw])
            self.DMA("pool", Wout[s_][:, 0:nch, :], wout[f0:f0 + nf, :].rearrange("(c p) d -> p c d", p=128), f"f_Wout{s_}", W=[self.B(f"f_Wout{s_}")])

        def emit_wout(gi, T):
            f0, nch = groups[gi]
            s_ = gi % 2
            ap_ = T % 2
            Bact = self.B(f"f_act{ap_}")
            Bwo = self.B(f"f_Wout{s_}")
            for tb_ in range(4):
                t = 4 * T + tb_
                p2 = t % 2
                BXt = self.B(f"X{t}")
                for dh in range(2):
                    bk = 4 + 2 * p2 + dh
                    Bk = self.B(f"bank{bk}")
                    for fc in range(nch):
                        self.MM(self.pbank(bk), actT[ap_][:, fc, tb_ * 128:(tb_ + 1) * 128], Wout[s_][:, fc, dh * 512:(dh + 1) * 512],
                                fc == 0, fc == nch - 1, R=[Bact, Bwo], W=[Bk] if fc == 0 else (), WA=() if fc == 0 else [Bk])
                    xs = X[:, t, dh * 512:(dh + 1) * 512]
                    self.TT("dve", xs, xs, self.pbank(bk), ALU.add, R=[Bk, BXt], WA=[BXt])

        load_w(0)
        it = 0
        pending = None
        for gi, (f0, nch) in enumerate(groups):
            s_ = gi % 2
            Bw = self.B(f"f_Win{s_}")
            Bwo = self.B(f"f_Wout{s_}")
            for T in range(4):
                ap_ = T % 2
                BHs = [self.B(f"HT{4 * T + i}") for i in range(4)]
                Bact = self.B(f"f_act{ap_}")
                for fc in range(nch):
                    if fc == 1:
                        if pending is not None:
                            emit_wout(*pending)
                            pending = None
                        if T == 0 and gi + 1 < len(groups):
                            load_w(gi + 1)
                    par = it % 2
                    it += 1
                    fi = f0 // 128 + fc
                    psG = self.pbank(2 * par)
                    psU = self.pbank(2 * par + 1)
                    BG = self.B(f"bank{2 * par}")
                    BU = self.B(f"bank{2 * par + 1}")
                    for k in range(8):
                        self.MM(psG, Win[s_][:, k, fc * 128:(fc + 1) * 128], HT[:, k, T * 512:(T + 1) * 512], k == 0, k == 7,
                                R=BHs + [Bw], W=[BG] if k == 0 else (), WA=() if k == 0 else [BG])
                    for k in range(8):
                        self.MM(psU, Win[s_][:, k, 512 + fc * 128:512 + (fc + 1) * 128], HT[:, k, T * 512:(T + 1) * 512], k == 0, k == 7,
                                R=BHs + [Bw], W=[BU] if k == 0 else (), WA=() if k == 0 else [BU])
                    Bgb = self.B(f"f_gb{par}")
                    Bc = self.B(f"f_carry{fc}")
                    if T == 0:
                        self.MEMSET("pool", gb[par][:, 0:2], 0.0, W=[Bgb])
                    else:
                        self.CP("pool", gb[par][:, 0:2], carry[:, fc, :], R=[Bc], W=[Bgb])
                    self.CP("act", gb[par][:, 2:514], psG, R=[BG], WA=[Bgb])
                    self.CP("pool", carry[:, fc, :], gb[par][:, 512:514], R=[Bgb], W=[Bc])
                    Btb = self.B(f"f_tb{par}")
                    self.TS("pool", tb[par], gb[par][:, 2:514], cw[:, fi, 2:3], cb[:, fi:fi + 1], ALU.mult, ALU.add, R=[Bgb, cB], W=[Btb])
                    self.STT(tb[par], gb[par][:, 1:513], cw[:, fi, 1:2], tb[par], ALU.mult, ALU.add, R=[Bgb, cB, Btb], WA=[Btb])
                    self.STT(tb[par], gb[par][:, 0:512], cw[:, fi, 0:1], tb[par], ALU.mult, ALU.add, R=[Bgb, cB, Btb], WA=[Btb])
                    Bsl = self.B(f"f_sl{par}")
                    self.ACT(sl_[par], tb[par], AF.Silu, R=[Btb], W=[Bsl])
                    self.TT("dve", actT[ap_][:, fc, :], sl_[par], psU, ALU.mult, R=[Bsl, BU], W=[Bact] if fc == 0 else (), WA=() if fc == 0 else [Bact])
                pending = (gi, T)
        emit_wout(*pending)

    def emit_ret(self, li):
        j = li // 2
        cB = self.B("consts")
        X, HT = self.X, self.HT
        WA = self.carve([8, 1536], BF16)
        WB = [self.carve([4, D], BF16) for _ in range(2)]
        St = self.carve([2, 512], F32)
        Sbf = [self.carve([2, 512], BF16) for _ in range(2)]
        rt = [self.carve([2, 128], F32) for _ in range(4)]
        qkr = [self.carve([2, 2, 128], BF16) for _ in range(2)]
        vbf = [self.carve([512], BF16) for _ in range(2)]
        sg = [self.carve([512], F32) for _ in range(2)]
        kd = [self.carve([256], BF16) for _ in range(2)]
        qkT = [self.carve([4, 128], BF16) for _ in range(2)]
        innT = [self.carve([128], BF16) for _ in range(2)]
        gated = [self.carve([512], BF16) for _ in range(2)]
        goT = [self.carve([4, 128], BF16) for _ in range(2)]
        junk = self.carve([512], BF16)
        sm = self.carve([16], F32)
        self.emit_norm(self.nmg[:, li * 8:(li + 1) * 8])
        wq = self.r_wqkvg[self.ret_js.index(j)]
        wo = self.r_wo[self.ret_js.index(j)]
        rcos = self.rcs[:, 0]
        rsin = self.rcs[:, 1]
        Brcs = self.B("rcs")
        gam = [1.0 - 2.0 ** (-5.0 - h) for h in range(4)]

        def load_w(h):
            blocks = [(h * 256, 256, 0), (1024 + h * 256, 256, 256), (2048 + h * 512, 512, 512), (4096 + h * 512, 512, 1024)]
            names = ["r_Wqk", "r_Wqk", "r_Wv", "r_Wg"]
            first = {"r_Wqk": True, "r_Wv": True, "r_Wg": True}
            for (c0, n, d0), nm in zip(blocks, names):
                src = wq[:, c0:c0 + n].rearrange("(k p) c -> p k c", p=128)
                Bw = self.B(nm)
                self.DMA("pool", WA[:, :, d0:d0 + n], src, nm, W=[Bw] if first[nm] else (), WA=() if first[nm] else [Bw])
                first[nm] = False
            s_ = h % 2
            self.DMA("pool", WB[s_], wo[h * 512:(h + 1) * 512, :].rearrange("(c p) d -> p c d", p=128), f"r_WB{s_}", W=[self.B(f"r_WB{s_}")])

        load_w(0)
        for h in range(4):
            s_ = h % 2
            Bwo = self.B(f"r_WB{s_}")
            cd = gam[h] ** 128
            qd = self.rcol[:, h:h + 1]
            qd2 = self.rcol[:, 4 + h:5 + h]
            kdc = self.rcol[:, 8 + h:9 + h]
            maskT = self.rmask[:, h, :]

            def stageA(n):
                p = n % 2
                tsl = slice(n * 128, (n + 1) * 128)
                BHt = self.B(f"HT{n}")
                names = ["r_Wqk", "r_Wv", "r_Wg"]
                for b in range(3):
                    Bk = self.B(f"bank{b}")
                    Bw = self.B(names[b])
                    for k in range(8):
                        self.MM(self.pbank(b), HT[:, k, tsl], WA[:, k, b * 512:(b + 1) * 512], k == 0, k == 7, R=[BHt, Bw],
                                W=[Bk] if k == 0 else (), WA=() if k == 0 else [Bk])
                B0, B1, B2 = self.B("bank0"), self.B("bank1"), self.B("bank2")
                v4 = self.pbank(0).rearrange("p (a i e) -> p a i e", i=128, e=2)
                E = v4[:, :, :, 0]
                O = v4[:, :, :, 1]
                cb_ = rcos[:, n, :].unsqueeze(1).to_broadcast([128, 2, 128])
                sb_ = rsin[:, n, :].unsqueeze(1).to_broadcast([128, 2, 128])
                Brt = self.B("r_rt")
                self.TT("dve", rt[0], E, cb_, ALU.mult, R=[B0, Brcs], W=[Brt])
                self.TT("dve", rt[1], O, sb_, ALU.mult, R=[B0, Brcs], WA=[Brt])
                self.TT("dve", rt[2], O, cb_, ALU.mult, R=[B0, Brcs], WA=[Brt])
                self.TT("dve", rt[3], E, sb_, ALU.mult, R=[B0, Brcs], WA=[Brt])
                Bqr = self.B(f"r_qkr{p}")
                self.TT("dve", qkr[p][:, :, 0, :], rt[0], rt[1], ALU.subtract, R=[Brt], W=[Bqr])
                self.TT("dve", qkr[p][:, :, 1, :], rt[2], rt[3], ALU.add, R=[Brt], WA=[Bqr])
                self.CP("act", vbf[p], self.pbank(1), R=[B1], W=[self.B(f"r_v{p}")])
                self.ACT(sg[p], self.pbank(2), AF.Silu, R=[B2], W=[self.B(f"r_sg{p}")])
                kflat = qkr[p][:, 1].rearrange("p a b -> p (a b)")
                self.ACT(kd[p], kflat, AF.Copy, R=[Bqr, cB], W=[self.B(f"r_kd{p}")], scale=kdc)

            def stageA2(n):
                p = n % 2
                Bqr = self.B(f"r_qkr{p}")
                ptb = self.pbank(3, BF16)[:, 0:512].rearrange("p (i t) -> p i t", t=128)
                Bpt = self.B("bank3a")
                qflat = qkr[p].rearrange("p a b c -> p (a b c)")
                for i in range(4):
                    self.TR(ptb[:, i, :], qflat[:, i * 128:(i + 1) * 128], R=[Bqr], W=[Bpt] if i == 0 else (), WA=() if i == 0 else [Bpt])
                self.CP("act", qkT[p], ptb, R=[Bpt], W=[self.B(f"r_qkT{p}")])

            def stageB(n):
                p = n % 2
                BqkT = self.B(f"r_qkT{p}")
                Bv = self.B(f"r_v{p}")
                pin = self.pbank(3)[:, 256:384]
                Bin = self.B("bank3b")
                for dc in range(2):
                    self.MM(pin, qkT[p][:, 2 + dc, :], qkT[p][:, dc, :], dc == 0, dc == 1, R=[BqkT], W=[Bin] if dc == 0 else (), WA=() if dc == 0 else [Bin])
                Bit = self.B(f"r_innT{p}")
                self.TT("dve", innT[p], pin, maskT, ALU.mult, R=[Bin, cB], W=[Bit])
                BO = self.B("bank4")
                pO = self.pbank(4)
                sp_ = (n - 1) % 2
                self.MM(pO, innT[p], vbf[p], True, n == 0, R=[Bit, Bv], W=[BO])
                if n > 0:
                    Bs = self.B(f"r_Sbf{sp_}")
                    for dc in range(2):
                        self.MM(pO, qkT[p][:, dc, :], Sbf[sp_][:, dc, :], False, dc == 1, R=[BqkT, Bs], WA=[BO])
                Bkd = self.B(f"r_kd{p}")
                BSt = self.B("r_St")
                if n < NT - 1:
                    for dc in range(2):
                        Bd = self.B(f"bank{5 + dc}")
                        self.MM(self.pbank(5 + dc), kd[p][:, dc * 128:(dc + 1) * 128], vbf[p], True, True, R=[Bkd, Bv], W=[Bd])
                        if n == 0:
                            self.CP("dve", St[:, dc, :], self.pbank(5 + dc), R=[Bd], W=[BSt] if dc == 0 else (), WA=() if dc == 0 else [BSt])
                        else:
                            self.STT(St[:, dc, :], St[:, dc, :], cd, self.pbank(5 + dc), ALU.mult, ALU.add, R=[Bd, BSt], WA=[BSt])
                    self.CP("pool", Sbf[p], St, R=[BSt], W=[self.B(f"r_Sbf{p}")])
                Bsm = self.B(f"r_sm{p}")
                o_ = p * 8
                self.ACT(junk, pO, AF.Square, R=[BO], W=[Bsm], WA=[self.B("r_junk")], accum=sm[:, o_:o_ + 1])
                self.TT("dve", sm[:, o_ + 1:o_ + 2], sm[:, o_:o_ + 1], qd2, ALU.mult, R=[Bsm, cB], WA=[Bsm])
                self.emit_rstd(sm[:, o_ + 2:o_ + 3], sm[:, o_ + 1:o_ + 2], 1.0, R=[Bsm], W=[self.B(f"r_rs{p}")])
                self.TT("dve", sm[:, o_ + 3:o_ + 4], sm[:, o_ + 2:o_ + 3], qd, ALU.mult, R=[self.B(f"r_rs{p}"), cB], W=[self.B(f"r_rq{p}")])
                Bg = self.B(f"r_gated{p}")
                self.STT(gated[p], pO, sm[:, o_ + 3:o_ + 4], sg[p], ALU.mult, ALU.mult, R=[BO, self.B(f"r_rq{p}"), self.B(f"r_sg{p}")], W=[Bg])

            def stageB2(n):
                p = n % 2
                Bg = self.B(f"r_gated{p}")
                ptb = self.pbank(7, BF16)[:, 0:512].rearrange("p (i t) -> p i t", t=128)
                Bpt = self.B("bank7")
                for i in range(4):
                    self.TR(ptb[:, i, :], gated[p][:, i * 128:(i + 1) * 128], R=[Bg], W=[Bpt] if i == 0 else (), WA=() if i == 0 else [Bpt])
                BgT = self.B(f"r_goT{p}")
                self.CP("act", goT[p], ptb, R=[Bpt], W=[BgT])
                BXt = self.B(f"X{n}")
                tslx = n
                for dh in range(2):
                    Bk = self.B("bank6") if dh == 0 else self.B("bank5")
                    bk = 6 if dh == 0 else 5
                    for ec in range(4):
                        self.MM(self.pbank(bk), goT[p][:, ec, :], WB[s_][:, ec, dh * 512:(dh + 1) * 512], ec == 0, ec == 3, R=[BgT, Bwo],
                                W=[Bk] if ec == 0 else (), WA=() if ec == 0 else [Bk])
                    xs = X[:, tslx, dh * 512:(dh + 1) * 512]
                    self.TT("dve", xs, xs, self.pbank(bk), ALU.add, R=[Bk, BXt], WA=[BXt])

            stageA(0)
            stageA2(0)
            for n in range(NT):
                if n + 1 < NT:
                    stageA(n + 1)
                    if n + 1 == NT - 1 and h + 1 < 4:
                        load_w(h + 1)
                stageB(n)
                if n + 1 < NT:
                    stageA2(n + 1)
                if n >= 1:
                    stageB2(n - 1)
            stageB2(NT - 1)

    def emit_out(self, s):
        junk = self.carve([D], BF16)
        ob = [self.carve([D], F32) for _ in range(2)]
        ss = self.carve([4], F32)
        gfull = self.carve([D], F32)
        Bg = self.B("o_g")
        self.DMA("sp", gfull, self.fng_d, "o_g", W=[Bg])
        for t in range(NT):
            p = t % 2
            BXt = self.B(f"X{t}")
            Bob = self.B(f"o_b{p}")
            if self.final_norm:
                Bss = self.B(f"o_ss{p}")
                self.ACT(junk, self.X[:, t, :], AF.Square, R=[BXt], W=[Bss], WA=[self.B("o_junk")], accum=ss[:, p:p + 1])
                self.emit_rstd(ss[:, 2 + p:3 + p], ss[:, p:p + 1], 1.0 / D, R=[Bss], W=[self.B(f"o_rs{p}")])
                self.STT(ob[p], self.X[:, t, :], ss[:, 2 + p:3 + p], gfull, ALU.mult, ALU.mult, R=[BXt, self.B(f"o_rs{p}"), Bg], W=[Bob])
            else:
                self.CP("dve", ob[p], self.X[:, t, :], R=[BXt], W=[Bob])
            self.DMA("sp", self.out_d[s, t * 128:(t + 1) * 128, :], ob[p], "out", R=[Bob], WA=[self.B("out")])


def _consts():
    ident = np.eye(128, dtype=np.float32)
    cm = (np.arange(128)[:, None] <= np.arange(128)[None, :]).astype(np.float32)
    cbf = np.concatenate([ident, cm], axis=1).astype(ml_dtypes.bfloat16)
    afreq = (500000.0 ** (-np.arange(0, 16, 2, dtype=np.float32) / np.float32(16))).astype(np.float32)
    rfreq = (1.0 / (10000.0 ** np.linspace(0.0, 1.0, 128, dtype=np.float32))).astype(np.float32)
    cf = np.zeros((128, 664), np.float32)
    cf[:, 0:8] = afreq[None, :]
    cf[:, 8:136] = rfreq[None, :]
    idx = np.arange(128, dtype=np.float64)
    for h in range(4):
        gam = 1.0 - 2.0 ** (-5.0 - h)
        m = np.where(idx[None, :] >= idx[:, None], (gam ** (-(idx[:, None] + 1.0))) / 16.0, 0.0)
        cf[:, 136 + h * 128:136 + (h + 1) * 128] = m
        qd = gam ** (idx + 1.0)
        cf[:, 648 + h] = qd
        cf[:, 652 + h] = qd * qd / 512.0
        cf[:, 656 + h] = gam ** (127.0 - idx) / 16.0
    return cbf, cf


_NC_CACHE = {}


def _get_nc(layers, nseq, final_norm):
    key = (tuple(layers), nseq, final_norm)
    if key not in _NC_CACHE:
        _NC_CACHE[key] = Builder(list(layers), nseq, final_norm).build()
    return _NC_CACHE[key]


PLAN = [[0, 1, 2, 3]]


def _norm_layers(layers):
    return tuple(e if isinstance(e, tuple) else (e, True, True) for e in layers)


def _run(inputs, layers=None, final_norm=True, plan=None):
    f = lambda a: np.ascontiguousarray(np.asarray(a, dtype=np.float32))
    x = f(inputs["x"])
    pos = np.ascontiguousarray(np.asarray(inputs["positions"], dtype=np.int32))
    cbf, cf = _consts()
    fm = lambda g: np.ascontiguousarray(f(g).reshape(DEPTH, 8, 128).transpose(2, 0, 1).reshape(128, DEPTH * 8))
    lam = np.concatenate([f(inputs["attn_lambda_q1"]), f(inputs["attn_lambda_k1"]),
                          f(inputs["attn_lambda_q2"]), f(inputs["attn_lambda_k2"])], axis=1)
    lam = np.ascontiguousarray(np.broadcast_to(lam.reshape(1, 512), (128, 512)))
    subln = np.ascontiguousarray(f(inputs["attn_subln_g"]).T)
    cw = f(inputs["ffn_conv_w"]).reshape(DEPTH, 3, NFC, 128).transpose(3, 0, 2, 1)
    cw = np.ascontiguousarray(cw.reshape(128, DEPTH * NFC * 3))
    cb = np.ascontiguousarray(f(inputs["ffn_conv_b"]).reshape(DEPTH, NFC, 128).transpose(2, 0, 1).reshape(128, DEPTH * NFC))
    small = dict(
        nmg=fm(inputs["norm_mix_g"]), nfg=fm(inputs["norm_ffn_g"]),
        fng=np.ascontiguousarray(np.broadcast_to(f(inputs["final_norm_g"]).reshape(1, D), (128, D))),
        lam_in=lam, subln=subln, f_cw=cw, f_cb=cb, c_bf=cbf, c_f32=cf,
    )
    if plan is None:
        plan = [list(layers)] if layers is not None else PLAN
    pcs = [np.ascontiguousarray(pos[c * NSEQ:(c + 1) * NSEQ].reshape(NSEQ, NT, 128).transpose(0, 2, 1)) for c in range(N_CORES)]
    cur = x
    for li_, lay in enumerate(plan):
        lay = _norm_layers(lay)
        fn = final_norm and (li_ == len(plan) - 1)
        key = (lay, NSEQ, fn)
        if key not in _NC_CACHE:
            b = Builder(list(lay), NSEQ, fn)
            _NC_CACHE[key] = (b.build(), b)
        nc, b = _NC_CACHE[key]
        shared = dict(small)
        if b.attn_js:
            shared["a_wqkv"] = f(inputs["attn_w_qkv"])[b.attn_js]
            shared["a_wo"] = f(inputs["attn_w_o"])[b.attn_js]
        if b.ret_js:
            shared["r_wqkvg"] = f(inputs["ret_w_qkvg"])[b.ret_js]
            shared["r_wo"] = f(inputs["ret_w_o"])[b.ret_js]
        if b.ffn_ls:
            shared["f_win"] = f(inputs["ffn_w_in"])[b.ffn_ls]
            shared["f_wout"] = f(inputs["ffn_w_out"])[b.ffn_ls]
        in_maps = []
        for c in range(N_CORES):
            m = dict(shared)
            m["x"] = np.ascontiguousarray(cur[c * NSEQ:(c + 1) * NSEQ])
            m["pos"] = pcs[c]
            in_maps.append(m)
        res = run_bass_kernel_spmd(nc, in_maps, core_ids=list(range(N_CORES)))
        cur = np.concatenate([r["out"] for r in res.results], axis=0)
    return cur


def kernel(**inputs):
    return _run(inputs)
```

```python
import math
import numpy as np
import ml_dtypes
import concourse.bass as bass
import concourse.mybir as mybir
from concourse.bass_utils import run_bass_kernel_spmd

dt = mybir.dt
F32, BF16, I32 = dt.float32, dt.bfloat16, dt.int32
AF = mybir.ActivationFunctionType
ALU = mybir.AluOpType
COMPUTE = ("pe", "act", "dve", "pool")

D = 1024
S = 2048
NT = 16
NSEQ = 2
DEPTH = 4
FFN = 2816
NFC = 22
EPS = 1e-6
N_CORES = 8
PI = math.pi
EPOCH = 1024


class Buf:
    __slots__ = ("name", "w", "r")

    def __init__(self, name):
        self.name = name
        self.w = {}
        self.r = {}


class Op:
    __slots__ = ("eng", "fn", "deps", "sig", "val", "key", "is_dma", "order", "epoch")

    def __init__(self, eng, fn, is_dma=False, key=None):
        self.eng = eng
        self.fn = fn
        self.deps = {}
        self.sig = False
        self.val = 0
        self.key = key if key is not None else eng
        self.is_dma = is_dma


class Prog:
    def __init__(self):
        self.streams = {e: [] for e in ("pe", "act", "dve", "pool", "sp")}
        self.dma_cnt = {}
        self.all_ops = []

    def op(self, eng, fn, R=(), W=(), WA=()):
        o = Op(eng, fn)
        self._track(o, R, W, WA)
        return o

    def dma(self, eng, fn, key, R=(), W=(), WA=()):
        o = Op(eng, fn, is_dma=True, key="dma:" + key)
        self.dma_cnt[key] = self.dma_cnt.get(key, 0) + 1
        o.val = 16 * self.dma_cnt[key]
        o.sig = True
        self._track(o, R, W, WA)
        return o

    def _track(self, o, R, W, WA):
        o.order = len(self.all_ops)
        self.all_ops.append(o)
        self.streams[o.eng].append(o)
        for b in R:
            for s in b.w.values():
                self._add(o, s, True)
        for b in list(W) + list(WA):
            for s in b.w.values():
                self._add(o, s, False)
            for s in b.r.values():
                self._add(o, s, False)
        for b in R:
            b.r[o.key] = o
        for b in W:
            b.w = {o.key: o}
            b.r = {}
        for b in WA:
            b.w[o.key] = o

    def _add(self, o, s, raw):
        if s is o:
            return
        if (not s.is_dma) and (not o.is_dma) and s.eng == o.eng:
            if not raw or s.eng == "pe":
                return
        cur = o.deps.get(s.key)
        if cur is None or cur.order < s.order:
            o.deps[s.key] = s

    def finalize(self):
        for o in self.all_ops:
            for s in o.deps.values():
                s.sig = True
        cnt = {e: 0 for e in COMPUTE}
        for o in self.all_ops:
            if not o.is_dma and o.sig:
                cnt[o.eng] += 1
                o.epoch = (cnt[o.eng] - 1) // EPOCH
                o.val = (cnt[o.eng] - 1) % EPOCH + 1
        self.n_epochs = {e: (cnt[e] + EPOCH - 1) // EPOCH for e in COMPUTE}

    def replay(self, name, eng, sems, dma_sems):
        waited = {}
        for o in self.streams[name]:
            for k, s in o.deps.items():
                if s.is_dma:
                    if waited.get(k, 0) < s.val:
                        eng.wait_ge(dma_sems[k[4:]], s.val)
                        waited[k] = s.val
                else:
                    tv = (s.epoch, s.val)
                    if waited.get(k, (-1, 0)) < tv:
                        eng.wait_ge(sems[(s.eng, s.epoch)], s.val)
                        waited[k] = tv
            if o.fn is None:
                continue
            ins = o.fn(eng)
            if o.is_dma:
                ins.then_inc(dma_sems[o.key[4:]], 16)
            elif o.sig:
                ins.then_inc(sems[(o.eng, o.epoch)], 1)


class Builder:
    def __init__(self, layers, nseq, final_norm):
        self.layers = layers
        self.nseq = nseq
        self.final_norm = final_norm
        self.P = Prog()
        self.nc = bass.Bass("TRN2", target_bir_lowering=False)
        self.bufs = {}

    def B(self, name):
        b = self.bufs.get(name)
        if b is None:
            b = self.bufs[name] = Buf(name)
        return b

    def Bs(self, names):
        return [self.B(n) for n in names]

    def xb(self, *aps):
        out = []
        for a in aps:
            try:
                if a.tensor.name != "psum":
                    continue
            except AttributeError:
                continue
            es = 4 if a.dtype in (F32, I32) else 2
            b = (a.offset * es) // 2048
            bb = self.B(f"xbank{b}")
            if bb not in out:
                out.append(bb)
        return out

    def dram_in(self, name, shape, d=F32):
        return self.nc.dram_tensor(name, list(shape), d, kind="ExternalInput").ap()

    def MM(self, out, lhsT, rhs, start, stop, R, W=(), WA=()):
        self.P.op("pe", lambda e: e.matmul(out, lhsT=lhsT, rhs=rhs, start=start, stop=stop,
                                           skip_group_check=True), R=R, W=list(W) + self.xb(out), WA=WA)

    def TR(self, out, in_, R, W=(), WA=()):
        ident = self.ident
        self.P.op("pe", lambda e: e.transpose(out=out, in_=in_, identity=ident), R=list(R) + [self.B("consts")], W=list(W) + self.xb(out), WA=WA)

    def ACT(self, out, in_, func, R, W=(), WA=(), scale=None, bias=None, accum=None):
        kw = {}
        if scale is not None:
            kw["scale"] = scale
        if bias is not None:
            kw["bias"] = bias
        if accum is not None:
            kw["accum_out"] = accum
        self.P.op("act", lambda e: e.activation(out=out, in_=in_, func=func, **kw), R=R, W=list(W) + self.xb(out, in_), WA=WA)

    def TT(self, eng, out, in0, in1, op, R, W=(), WA=()):
        self.P.op(eng, lambda e: e.tensor_tensor(out=out, in0=in0, in1=in1, op=op), R=R, W=list(W) + self.xb(out, in0, in1), WA=WA)

    def TS(self, eng, out, in0, s1, s2, op0, op1, R, W=(), WA=()):
        if op1 is None:
            self.P.op(eng, lambda e: e.tensor_scalar(out=out, in0=in0, scalar1=s1, scalar2=None, op0=op0), R=R, W=list(W) + self.xb(out, in0), WA=WA)
        else:
            self.P.op(eng, lambda e: e.tensor_scalar(out=out, in0=in0, scalar1=s1, scalar2=s2, op0=op0, op1=op1), R=R, W=list(W) + self.xb(out, in0), WA=WA)

    def STT(self, out, in0, scalar, in1, op0, op1, R, W=(), WA=(), accum=None):
        if accum is None:
            self.P.op("dve", lambda e: e.scalar_tensor_tensor(out=out, in0=in0, scalar=scalar, in1=in1, op0=op0, op1=op1), R=R, W=list(W) + self.xb(out, in0, in1), WA=WA)
        else:
            self.P.op("dve", lambda e: e.scalar_tensor_tensor(out=out, in0=in0, scalar=scalar, in1=in1, op0=op0, op1=op1, accum_out=accum), R=R, W=list(W) + self.xb(out, in0, in1), WA=WA)

    def CP(self, eng, out, in_, R, W=(), WA=()):
        if eng == "act":
            self.P.op("act", lambda e: e.copy(out=out, in_=in_), R=R, W=list(W) + self.xb(out, in_), WA=WA)
        else:
            self.P.op(eng, lambda e: e.tensor_copy(out=out, in_=in_), R=R, W=list(W) + self.xb(out, in_), WA=WA)

    def MEMSET(self, eng, ap, val, W=(), WA=()):
        self.P.op(eng, lambda e: e.memset(ap, val), W=W, WA=WA)

    def DMA(self, eng, out, in_, key, R=(), W=(), WA=()):
        self.P.dma(eng, lambda e: e.dma_start(out=out, in_=in_), key, R=R, W=W, WA=WA)

    def barrier(self):
        allb = list(self.bufs.values())
        P = self.P
        for e in ("pe", "act", "dve", "pool", "sp"):
            o = Op(e, None)
            o.order = len(P.all_ops)
            for b in allb:
                for src in list(b.w.values()) + list(b.r.values()):
                    if src.fn is None:
                        continue
                    if (not src.is_dma) and src.eng == e:
                        continue
                    cur = o.deps.get(src.key)
                    if cur is None or cur.order < src.order:
                        o.deps[src.key] = src
            P.all_ops.append(o)
            P.streams[e].append(o)

    def arena_reset(self):
        self.aoff = 0

    def carve(self, shape, d):
        n = 1
        for v in shape:
            n *= v
        nbytes = n * (4 if d in (F32, I32) else 2)
        nbytes = (nbytes + 31) // 32 * 32
        o2 = self.aoff // 2
        assert self.aoff + nbytes <= self.arena_bytes, (self.aoff, nbytes, self.arena_bytes)
        v = self.arena[:, o2:o2 + nbytes // 2]
        self.aoff += nbytes
        if d != BF16:
            v = v.bitcast(d)
        v = v[:, 0:n]
        if len(shape) == 2:
            v = v.rearrange("p (a b) -> p a b", b=shape[1])
        elif len(shape) == 3:
            v = v.rearrange("p (a b c) -> p a b c", b=shape[1], c=shape[2])
        return v

    def pbank(self, i, d=F32):
        v = self.psum[:, i * 512:(i + 1) * 512]
        if d == BF16:
            v = v.bitcast(BF16)
        return v

    def build(self):
        nc = self.nc
        ns = self.nseq
        self.x_d = self.dram_in("x", [ns, S, D])
        self.pos_d = self.dram_in("pos", [ns, 128, NT], I32)
        self.nmg_d = self.dram_in("nmg", [128, DEPTH * 8])
        self.nfg_d = self.dram_in("nfg", [128, DEPTH * 8])
        self.fng_d = self.dram_in("fng", [128, D])
        self.lam_d = self.dram_in("lam_in", [128, 2 * 256])
        self.subln_d = self.dram_in("subln", [128, 2])
        self.cw_d = self.dram_in("f_cw", [128, DEPTH * NFC * 3])
        self.cb_d = self.dram_in("f_cb", [128, DEPTH * NFC])
        ents = [e if isinstance(e, tuple) else (e, True, True) for e in self.layers]
        self.attn_js = sorted({li // 2 for li, m, f in ents if m and li % 2 == 0})
        self.ret_js = sorted({li // 2 for li, m, f in ents if m and li % 2 == 1})
        self.ffn_ls = sorted({li for li, m, f in ents if f})
        if self.attn_js:
            self.a_wqkv = self.dram_in("a_wqkv", [len(self.attn_js), D, 3 * D])
            self.a_wo = self.dram_in("a_wo", [len(self.attn_js), D, D])
        if self.ret_js:
            self.r_wqkvg = self.dram_in("r_wqkvg", [len(self.ret_js), D, 6144])
            self.r_wo = self.dram_in("r_wo", [len(self.ret_js), 2048, D])
        if self.ffn_ls:
            self.f_win = self.dram_in("f_win", [len(self.ffn_ls), D, 2 * FFN])
            self.f_wout = self.dram_in("f_wout", [len(self.ffn_ls), FFN, D])
        self.cbf_d = self.dram_in("c_bf", [128, 256], BF16)
        self.cf_d = self.dram_in("c_f32", [128, 8 + 128 + 512 + 16])
        self.out_d = nc.dram_tensor("out", [ns, S, D], F32, kind="ExternalOutput").ap()

        self.X = nc.alloc_sbuf_tensor("X", [128, NT, D], F32)[:]
        self.HT = nc.alloc_sbuf_tensor("HT", [128, 8, S], BF16)[:]
        self.cbf = nc.alloc_sbuf_tensor("cbf", [128, 256], BF16)[:]
        self.cf = nc.alloc_sbuf_tensor("cf", [128, 664], F32)[:]
        self.ident = self.cbf[:, 0:128]
        self.cmask = self.cbf[:, 128:256]
        self.afreq = self.cf[:, 0:8]
        self.rfreq = self.cf[:, 8:136]
        self.rmask = self.cf[:, 136:648].rearrange("p (h i) -> p h i", i=128)
        self.rcol = self.cf[:, 648:664]
        self.params = nc.alloc_sbuf_tensor("params", [128, 32 + 32 + 512 + 2 + 264 + 88], F32)[:]
        o = 0
        self.nmg = self.params[:, o:o + 32]; o += 32
        self.nfg = self.params[:, o:o + 32]; o += 32
        self.lamin = self.params[:, o:o + 512]; o += 512
        self.subln = self.params[:, o:o + 2]; o += 2
        self.cw = self.params[:, o:o + 264]; o += 264
        self.cb = self.params[:, o:o + 88]; o += 88
        self.rcs = nc.alloc_sbuf_tensor("rcs", [128, 2, NT, 128], F32)[:]
        self.acs = nc.alloc_sbuf_tensor("acs", [128, 2, NT, 8], F32)[:]
        self.posf = nc.alloc_sbuf_tensor("posf", [128, NT], F32)[:]
        self.posi = nc.alloc_sbuf_tensor("posi", [128, NT], I32)[:]
        self.small = nc.alloc_sbuf_tensor("small", [128, 64], F32)[:]
        self.arena_bytes = (nc.sbuf_bytes_remaining - 256) // 64 * 64
        self.arena = nc.alloc_sbuf_tensor("arena", [128, self.arena_bytes // 2], BF16)[:]
        self.psum = nc.alloc_psum_tensor("psum", [128, 4096], F32)[:]

        cB = self.B("consts")
        self.DMA("sp", self.cbf, self.cbf_d, "consts", W=[cB])
        self.DMA("sp", self.cf, self.cf_d, "consts", WA=[cB])
        self.DMA("sp", self.nmg, self.nmg_d, "consts", WA=[cB])
        self.DMA("sp", self.nfg, self.nfg_d, "consts", WA=[cB])
        self.DMA("sp", self.lamin, self.lam_d, "consts", WA=[cB])
        self.DMA("sp", self.subln, self.subln_d, "consts", WA=[cB])
        self.DMA("sp", self.cw, self.cw_d, "consts", WA=[cB])
        self.DMA("sp", self.cb, self.cb_d, "consts", WA=[cB])
        self.MEMSET("dve", self.small[:, 0:1], EPS, WA=[cB])

        for s in range(ns):
            self.emit_seq(s)
        self.P.op("sp", None, R=[self.B("out")])
        self.P.finalize()

        from contextlib import ExitStack
        with ExitStack() as es:
            sems = {(e, k): es.enter_context(nc.semaphore(f"s_{e}{k}")) for e in COMPUTE for k in range(self.P.n_epochs[e])}
            dsems = {k: es.enter_context(nc.semaphore("d_" + k)) for k in self.P.dma_cnt}
            P = self.P
            with nc.Block() as block:
                @block.tensor
                def _(e):
                    P.replay("pe", e, sems, dsems)

                @block.scalar
                def _(e):
                    P.replay("act", e, sems, dsems)

                @block.vector
                def _(e):
                    P.replay("dve", e, sems, dsems)

                @block.gpsimd
                def _(e):
                    P.replay("pool", e, sems, dsems)

                @block.sync
                def _(e):
                    P.replay("sp", e, sems, dsems)
        return nc

    def emit_seq(self, s):
        BX = [self.B(f"X{t}") for t in range(NT)]
        self.barrier()
        for t in range(NT):
            self.DMA("sp", self.X[:, t, :], self.x_d[s, t * 128:(t + 1) * 128, :], f"X{t}", W=[BX[t]])
        self.DMA("sp", self.posi, self.pos_d[s], "pos", W=[self.B("posi")])
        self.CP("dve", self.posf, self.posi, R=[self.B("posi")], W=[self.B("posf")])
        self.arena_reset()
        self.emit_sincos(self.afreq, 8, self.acs, "acs")
        self.arena_reset()
        self.emit_sincos(self.rfreq, 128, self.rcs, "rcs")
        for ent in self.layers:
            li, do_mix, do_ffn = ent if isinstance(ent, tuple) else (ent, True, True)
            if do_mix:
                self.barrier()
                self.arena_reset()
                if li % 2 == 0:
                    self.emit_attn(li)
                else:
                    self.emit_ret(li)
            if do_ffn:
                self.barrier()
                self.arena_reset()
                self.emit_ffn(li)
        self.barrier()
        self.arena_reset()
        self.emit_out(s)

    def emit_sincos(self, freq, F, dst, name):
        cB = self.B("consts")
        Bt = self.B("sc_tmp")
        Bd = self.B(name)
        ang = self.carve([NT, F], F32)
        ki = self.carve([NT, F], I32)
        kf = self.carve([NT, F], F32)
        y = self.carve([NT, F], F32)
        fb = freq.unsqueeze(1).to_broadcast([128, NT, F])
        pb = self.posf.unsqueeze(2).to_broadcast([128, NT, F])
        self.TT("dve", ang, fb, pb, ALU.mult, R=[cB, self.B("posf")], W=[Bt])
        self.TS("dve", ki, ang, 1.0 / (2 * PI), None, ALU.mult, None, R=[Bt], WA=[Bt])
        self.CP("dve", kf, ki, R=[Bt], WA=[Bt])
        self.STT(ang, kf, -2 * PI, ang, ALU.mult, ALU.add, R=[Bt], WA=[Bt])
        for idx, shift in ((1, 0.0), (0, PI / 2)):
            self.TS("dve", y, ang, shift, None, ALU.add, None, R=[Bt], WA=[Bt])
            self.TS("dve", kf, y, PI, -2 * PI, ALU.is_gt, ALU.mult, R=[Bt], WA=[Bt])
            self.TT("dve", y, y, kf, ALU.add, R=[Bt], WA=[Bt])
            self.TS("dve", kf, y, -PI, 2 * PI, ALU.is_lt, ALU.mult, R=[Bt], WA=[Bt])
            self.TT("dve", y, y, kf, ALU.add, R=[Bt], WA=[Bt])
            self.ACT(dst[:, idx], y, AF.Sin, R=[Bt], WA=[Bd])

    def emit_rstd(self, out, in_, scale, R, W):
        self.ACT(out, in_, AF.Ln, R=list(R) + [self.B("consts")], W=W, scale=scale, bias=self.small[:, 0:1])
        self.ACT(out, out, AF.Exp, R=W, WA=W, scale=-0.5)

    def emit_norm(self, gcol):
        cB = self.B("consts")
        junk = self.carve([D], BF16)
        junk2 = self.carve([D], BF16)
        xn = [self.carve([D], BF16) for _ in range(2)]
        ss = self.carve([2 * NT], F32)
        gb = gcol.unsqueeze(2).to_broadcast([128, 8, 128])
        Bss = self.B("n_ss")
        for t in range(NT):
            BXt = self.B(f"X{t}")
            first = (t == 0)
            if t % 2 == 0:
                self.ACT(junk, self.X[:, t, :], AF.Square, R=[BXt], W=[Bss] if first else (), WA=[self.B("n_junk")] + ([] if first else [Bss]),
                         accum=ss[:, t:t + 1])
            else:
                self.STT(junk2, self.X[:, t, :], 1.0, self.X[:, t, :], ALU.mult, ALU.mult, R=[BXt], WA=[self.B("n_junk2"), Bss], accum=ss[:, t:t + 1])
        Brs = self.B("n_rs")
        self.emit_rstd(ss[:, NT:2 * NT], ss[:, 0:NT], 1.0 / D, R=[Bss], W=[Brs])
        for t in range(NT):
            p = t % 2
            BXt = self.B(f"X{t}")
            Bxn = self.B(f"n_xn{p}")
            Bpt = self.B(f"bank{4 + p}")
            self.ACT(xn[p], self.X[:, t, :], AF.Copy, R=[BXt, Brs], W=[Bxn], scale=ss[:, NT + t:NT + t + 1])
            pt = self.pbank(4 + p, BF16).rearrange("p (c t) -> p c t", t=128)
            for c in range(8):
                self.TR(pt[:, c, :], xn[p][:, c * 128:(c + 1) * 128], R=[Bxn], W=[Bpt] if c == 0 else (), WA=() if c == 0 else [Bpt])
            self.TT("dve", self.HT[:, :, t * 128:(t + 1) * 128], pt, gb, ALU.mult, R=[Bpt, cB], W=[self.B(f"HT{t}")])

    def emit_attn(self, li):
        j = li // 2
        lambda_init = 0.8 - 0.6 * math.exp(-0.3 * li)
        cB = self.B("consts")
        X, HT = self.X, self.HT
        WA = [self.carve([8, 768], BF16) for _ in range(2)]
        WB = [self.carve([2, D], BF16) for _ in range(2)]
        qT = self.carve([2, S], BF16)
        kT = self.carve([2, S], BF16)
        V = self.carve([NT, 2, 129], BF16)
        onT = self.carve([2, S], BF16)
        Pt = [self.carve([2, 512], BF16) for _ in range(2)]
        qkf = [self.carve([512], F32) for _ in range(2)]
        qkb = [self.carve([512], BF16) for _ in range(2)]
        rt = [self.carve([8, 8], F32) for _ in range(4)]
        onb4 = [self.carve([4, 128], BF16) for _ in range(2)]
        qkf.append(Pt[0].rearrange("p c n -> p (c n)").bitcast(F32))
        qkb.append(onb4[0].rearrange("p q e -> p (q e)"))
        qkf_names = ["a_qkf0", "a_qkf1", "a_P0"]
        qkb_names = ["a_qkb0", "a_qkb1", "a_on0"]
        sm = self.carve([32], F32)
        lamj = self.carve([64], F32)
        Bl = self.B("a_lam")
        li0 = self.lamin[:, j * 256:(j + 1) * 256]
        self.STT(lamj, li0[:, 0:64], 1.0, li0[:, 64:128], ALU.mult, ALU.mult, R=[cB], W=[Bl], accum=sm[:, 0:1])
        self.STT(lamj, li0[:, 128:192], 1.0, li0[:, 192:256], ALU.mult, ALU.mult, R=[cB], WA=[Bl], accum=sm[:, 1:2])
        self.ACT(sm[:, 2:4], sm[:, 0:2], AF.Exp, R=[Bl], WA=[Bl])
        self.TT("dve", sm[:, 4:5], sm[:, 3:4], sm[:, 2:3], ALU.subtract, R=[Bl], WA=[Bl])
        self.TS("dve", sm[:, 5:6], sm[:, 4:5], -lambda_init, None, ALU.add, None, R=[Bl], WA=[Bl])
        neglam = sm[:, 5:6]
        self.TS("dve", sm[:, 6:7], self.subln[:, j:j + 1], 1.0 - lambda_init, None, ALU.mult, None, R=[cB], WA=[Bl])
        sgcol = sm[:, 6:7]
        self.MEMSET("pool", V[:, :, :, 128:129], 1.0, W=[self.B("a_Vones")])

        mark = self.aoff
        self.emit_norm(self.nmg[:, li * 8:(li + 1) * 8])
        self.aoff = mark
        accs = self.carve([3, 387], F32)

        wq = self.a_wqkv[self.attn_js.index(j)]
        wo = self.a_wo[self.attn_js.index(j)]

        def load_w(g):
            sl = g % 2
            Bw = self.B(f"a_WA{sl}")
            for blk in range(3):
                src = wq[:, blk * D + g * 256: blk * D + (g + 1) * 256].rearrange("(k p) c -> p k c", p=128)
                self.DMA("pool", WA[sl][:, :, blk * 256:(blk + 1) * 256], src, f"a_WA{sl}", W=[Bw] if blk == 0 else (), WA=() if blk == 0 else [Bw])
            src = wo[g * 256:(g + 1) * 256, :].rearrange("(k p) c -> p k c", p=128)
            self.DMA("pool", WB[sl], src, f"a_WB{sl}", W=[self.B(f"a_WB{sl}")])

        acos = self.acs[:, 0]
        asin = self.acs[:, 1]
        load_w(0)
        for g in range(4):
            sl = g % 2
            if g + 1 < 4:
                load_w(g + 1)
            Bw = self.B(f"a_WA{sl}")
            Bwo = self.B(f"a_WB{sl}")
            def p1_mm(t):
                p = t % 3
                tsl = slice(t * 128, (t + 1) * 128)
                BHt = self.B(f"HT{t}")
                ps_qk = self.pbank(2 * p)
                ps_v = self.pbank(2 * p + 1)[:, 0:256]
                Bqk = self.B(f"bank{2 * p}")
                Bv = self.B(f"bank{2 * p + 1}")
                for k in range(8):
                    self.MM(ps_qk, HT[:, k, tsl], WA[sl][:, k, 0:512], k == 0, k == 7, R=[BHt, Bw], W=[Bqk] if k == 0 else (), WA=() if k == 0 else [Bqk])
                for k in range(8):
                    self.MM(ps_v, HT[:, k, tsl], WA[sl][:, k, 512:768], k == 0, k == 7, R=[BHt, Bw], W=[Bv] if k == 0 else (), WA=() if k == 0 else [Bv])

            def p1_post(t):
                p = t % 3
                tsl = slice(t * 128, (t + 1) * 128)
                ps_qk = self.pbank(2 * p)
                ps_v = self.pbank(2 * p + 1)[:, 0:256]
                Bqk = self.B(f"bank{2 * p}")
                Bv = self.B(f"bank{2 * p + 1}")
                BVt = self.B(f"a_V{t}")
                self.CP("act", V[:, t, :, 0:128], ps_v.rearrange("p (h e) -> p h e", e=128), R=[Bv, self.B("a_Vones")], W=[BVt])
                Bf = self.B(qkf_names[p])
                self.ACT(qkf[p][:, 0:256], ps_qk[:, 0:256], AF.Copy, R=[Bqk], W=[Bf], scale=0.125)
                self.CP("act", qkf[p][:, 256:512], ps_qk[:, 256:512], R=[Bqk], WA=[Bf])
                v3 = qkf[p].rearrange("p (g d) -> p g d", d=64)
                x1 = v3[:, :, 0:8]
                x2 = v3[:, :, 8:16]
                cb_ = acos[:, t, :].unsqueeze(1).to_broadcast([128, 8, 8])
                sb_ = asin[:, t, :].unsqueeze(1).to_broadcast([128, 8, 8])
                Brt = self.B("a_rt")
                Bacs = self.B("acs")
                self.TT("dve", rt[0], x1, cb_, ALU.mult, R=[Bf, Bacs], W=[Brt])
                self.TT("dve", rt[1], x2, sb_, ALU.mult, R=[Bf, Bacs], WA=[Brt])
                self.TT("dve", rt[2], x2, cb_, ALU.mult, R=[Bf, Bacs], WA=[Brt])
                self.TT("dve", rt[3], x1, sb_, ALU.mult, R=[Bf, Bacs], WA=[Brt])
                self.TT("dve", x1, rt[0], rt[1], ALU.subtract, R=[Brt], WA=[Bf])
                self.TT("dve", x2, rt[2], rt[3], ALU.add, R=[Brt], WA=[Bf])
                Bb = self.B(qkb_names[p])
                self.CP("pool", qkb[p], qkf[p], R=[Bf], W=[Bb])

            def p1_post_b(t):
                p = t % 3
                tsl = slice(t * 128, (t + 1) * 128)
                Bb = self.B(qkb_names[p])
                ptb = self.pbank(7, BF16)[:, 0:512].rearrange("p (i t) -> p i t", t=128)
                Bpt = self.B("bank7")
                for i in range(4):
                    self.TR(ptb[:, i, :], qkb[p][:, i * 128:(i + 1) * 128], R=[Bb], W=[Bpt] if i == 0 else (), WA=() if i == 0 else [Bpt])
                self.CP("dve", qT[:, :, tsl], ptb[:, 0:2, :], R=[Bpt], W=[self.B(f"a_qT{t}")])
                self.CP("act", kT[:, :, tsl], ptb[:, 2:4, :], R=[Bpt], W=[self.B(f"a_kT{t}")])

            p1_mm(0)
            p1_mm(1)
            p1_post(0)
            for t in range(2, NT):
                p1_mm(t)
                p1_post(t - 1)
                p1_post_b(t - 2)
            p1_post(NT - 1)
            p1_post_b(NT - 2)
            p1_post_b(NT - 1)
            steps = [(hl, qt, kb) for hl in range(2) for qt in range(4) for kb in range(4 * qt + 4)]
            nst = len(steps)
            Bacc = [self.B(f"bank{4 + b}") for b in range(3)]
            Baccs = self.B("a_accs")
            accv = accs.rearrange("p b n -> p (b n)")[:, 0:1032].rearrange("p (a n) -> p a n", n=129)
            of4 = qkf[1].rearrange("p (q e) -> p q e", e=128)
            sq4 = qkf[0].rearrange("p (q e) -> p q e", e=128)
            Bof, Bsq = self.B("a_qkf1"), self.B("a_qkf0")
            state = {"started": [False] * 3}

            def acc_ap(c, qb):
                a = c * 4 + qb
                return self.pbank(4 + a // 3)[:, (a % 3) * 129:(a % 3) * 129 + 129], a // 3

            def st_S(i):
                hl, qt, kb = steps[i]
                par = i % 2
                c0 = max(kb - 4 * qt, 0) * 128
                BqTs = [self.B(f"a_qT{4 * qt + q}") for q in range(4)]
                diag = kb - 4 * qt >= 0
                for c in range(2):
                    self.MM(self.pbank(2 * par + c)[:, c0:512], kT[c * 64:(c + 1) * 64, hl, kb * 128:(kb + 1) * 128],
                            qT[c * 64:(c + 1) * 64, hl, qt * 512 + c0:(qt + 1) * 512], True, not diag,
                            R=[self.B(f"a_kT{kb}")] + BqTs, W=[self.B(f"bank{2 * par + c}")])
                if diag:
                    for c in range(2):
                        self.MM(self.pbank(2 * par + c)[:, c0:c0 + 128], self.ident, self.cmask, False, True,
                                R=[cB], WA=[self.B(f"bank{2 * par + c}")])

            def st_exp(i):
                hl, qt, kb = steps[i]
                par = i % 2
                jd = kb - 4 * qt
                c0 = max(jd, 0) * 128
                src = self.psum[:, 2 * par * 512:(2 * par + 2) * 512].rearrange("p (c n) -> p c n", n=512)[:, :, c0:512]
                self.ACT(Pt[par][:, :, c0:512], src, AF.Exp, R=[self.B(f"bank{2 * par}"), self.B(f"bank{2 * par + 1}")],
                         W=[self.B(f"a_P{par}"), self.B(f"xbank{2 * par + 1}")])

            def st_PV(i):
                hl, qt, kb = steps[i]
                par = i % 2
                jd = kb - 4 * qt
                if kb == 0:
                    state["started"] = [False] * 3
                for c in range(2):
                    Bp = self.B(f"a_P{par}")
                    for qb in range(max(jd, 0), 4):
                        ap_, b = acc_ap(c, qb)
                        st = not state["started"][b]
                        state["started"][b] = True
                        self.MM(ap_, Pt[par][:, c, qb * 128:(qb + 1) * 128], V[:, kb, hl, :], st, kb == 4 * qt + qb,
                                R=[Bp, self.B(f"a_V{kb}")], W=[Bacc[b]] if st else (), WA=() if st else [Bacc[b]])

            def st_norm(hl, qt, dp):
                for b in range(3):
                    n_ = 387 if b < 2 else 258
                    self.CP("dve", accs[:, b, 0:n_], self.pbank(4 + b)[:, 0:n_], R=[Bacc[b]], W=[Baccs] if b == 0 else (), WA=() if b == 0 else [Baccs])
                Bsm = self.B("a_sm")
                rec = sm[:, 8:16]
                self.P.op("dve", lambda e: e.reciprocal(out=rec, in_=accv[:, :, 128]), R=[Baccs], W=[Bsm])
                self.TS("dve", sm[:, 12:16], sm[:, 12:16], neglam, None, ALU.mult, None, R=[Bsm, Bl], WA=[Bsm])
                r1 = sm[:, 8:12].unsqueeze(2).to_broadcast([128, 4, 128])
                r2 = sm[:, 12:16].unsqueeze(2).to_broadcast([128, 4, 128])
                self.TT("dve", of4, accv[:, 0:4, 0:128], r1, ALU.mult, R=[Baccs, Bsm], W=[Bof])
                self.TT("dve", sq4, accv[:, 4:8, 0:128], r2, ALU.mult, R=[Baccs, Bsm], W=[Bsq])
                self.TT("dve", of4, of4, sq4, ALU.add, R=[Bsq], WA=[Bof])
                self.TT("dve", sq4, of4, of4, ALU.mult, R=[Bof], W=[Bsq])
                self.P.op("dve", lambda e: e.tensor_reduce(out=sm[:, 16:20], in_=sq4, axis=mybir.AxisListType.X, op=ALU.add), R=[Bsq], WA=[Bsm])

            def st_norm_b(hl, qt, dp):
                Bsm = self.B("a_sm")
                Brs = self.B("a_rs")
                self.emit_rstd(sm[:, 20:24], sm[:, 16:20], 1.0 / 128, R=[Bsm], W=[Brs])
                rs = sm[:, 20:24].unsqueeze(2).to_broadcast([128, 4, 128])
                self.TT("dve", onb4[dp], of4, rs, ALU.mult, R=[Bof, Brs], W=[self.B(f"a_on{dp}")])

            def st_norm_pe(hl, qt, dp):
                pt2 = self.pbank(7, BF16)[:, 512:1024].rearrange("p (q t) -> p q t", t=128)
                Bp2 = self.B("bank7b")
                for qb in range(4):
                    self.TR(pt2[:, qb, :], onb4[dp][:, qb, :], R=[self.B(f"a_on{dp}")], W=[Bp2] if qb == 0 else (), WA=() if qb == 0 else [Bp2])
                dst = onT[:, hl, qt * 512:(qt + 1) * 512].rearrange("p (q t) -> p q t", t=128)
                self.TS("dve", dst, pt2, sgcol, None, ALU.mult, None, R=[Bp2, Bl],
                        W=[self.B(f"a_onT{hl}_{4 * qt + q}") for q in range(4)])

            pending = []
            st_S(0)
            for i in range(nst):
                if i + 1 < nst:
                    st_S(i + 1)
                st_exp(i)
                st_PV(i)
                hl, qt, kb = steps[i]
                if kb == 4 * qt + 3:
                    dp = (hl * 4 + qt) % 2
                    st_norm(hl, qt, dp)
                    pending.append((i + 2, st_norm_b, hl, qt, dp))
                    pending.append((i + 4, st_norm_pe, hl, qt, dp))
                    pending.sort(key=lambda x: x[0])
                while pending and pending[0][0] <= i:
                    _, f_, h_, q_, d_ = pending.pop(0)
                    f_(h_, q_, d_)
            for _, f_, h_, q_, d_ in pending:
                f_(h_, q_, d_)
            for t in range(NT):
                p = t % 2
                tsl = slice(t * 128, (t + 1) * 128)
                for dh in range(2):
                    Bk = self.B(f"bank{2 * p + dh}")
                    for hl in range(2):
                        self.MM(self.pbank(2 * p + dh), onT[:, hl, tsl], WB[sl][:, hl, dh * 512:(dh + 1) * 512], hl == 0, hl == 1,
                                R=[self.B(f"a_onT{hl}_{t}"), Bwo], W=[Bk] if hl == 0 else (), WA=() if hl == 0 else [Bk])
                    BXt = self.B(f"X{t}")
                    xs = X[:, t, dh * 512:(dh + 1) * 512]
                    self.TT("dve", xs, xs, self.pbank(2 * p + dh), ALU.add, R=[Bk, BXt], WA=[BXt])

    def emit_ffn(self, li):
        cB = self.B("consts")
        X, HT = self.X, self.HT
        Win = [self.carve([8, 1024], BF16) for _ in range(2)]
        Wout = [self.carve([4, D], BF16) for _ in range(2)]
        gb = [self.carve([516], F32) for _ in range(2)]
        tb = [self.carve([512], F32) for _ in range(2)]
        sl_ = [self.carve([512], F32) for _ in range(2)]
        actT = [self.carve([4, 512], BF16) for _ in range(2)]
        carry = self.carve([4, 2], F32)
        self.emit_norm(self.nfg[:, li * 8:(li + 1) * 8])
        win = self.f_win[self.ffn_ls.index(li)]
        wout = self.f_wout[self.ffn_ls.index(li)]
        groups = [(0, 4), (512, 4), (1024, 4), (1536, 4), (2048, 4), (2560, 2)]
        cw = self.cw[:, li * 66:(li + 1) * 66].rearrange("p (f j) -> p f j", j=3)
        cb = self.cb[:, li * 22:(li + 1) * 22]

        def load_w(gi):
            f0, nch = groups[gi]
            nf = nch * 128
            s_ = gi % 2
            Bw = self.B(f"f_Win{s_}")
            self.DMA("pool", Win[s_][:, :, 0:nf], win[:, f0:f0 + nf].rearrange("(k p) c -> p k c", p=128), f"f_Win{s_}", W=[Bw])
            self.DMA("pool", Win[s_][:, :, 512:512 + nf], win[:, FFN + f0:FFN + f0 + nf].rearrange("(k p) c -> p k c", p=128), f"f_Win{s_}", WA=[Bw])
            self.DMA("pool", Wout[s_][:, 0:nch, :], wout[f0:f0 + nf, :].rearrange("(c p) d -> p c d", p=128), f"f_Wout{s_}", W=[self.B(f"f_Wout{s_}")])

        def emit_wout(gi, T):
            f0, nch = groups[gi]
            s_ = gi % 2
            ap_ = T % 2
            Bact = self.B(f"f_act{ap_}")
            Bwo = self.B(f"f_Wout{s_}")
            for tb_ in range(4):
                t = 4 * T + tb_
                p2 = t % 2
                BXt = self.B(f"X{t}")
                for dh in range(2):
                    bk = 4 + 2 * p2 + dh
                    Bk = self.B(f"bank{bk}")
                    for fc in range(nch):
                        self.MM(self.pbank(bk), actT[ap_][:, fc, tb_ * 128:(tb_ + 1) * 128], Wout[s_][:, fc, dh * 512:(dh + 1) * 512],
                                fc == 0, fc == nch - 1, R=[Bact, Bwo], W=[Bk] if fc == 0 else (), WA=() if fc == 0 else [Bk])
                    xs = X[:, t, dh * 512:(dh + 1) * 512]
                    self.TT("dve", xs, xs, self.pbank(bk), ALU.add, R=[Bk, BXt], WA=[BXt])

        load_w(0)
        it = 0
        pending = None
        for gi, (f0, nch) in enumerate(groups):
            s_ = gi % 2
            Bw = self.B(f"f_Win{s_}")
            Bwo = self.B(f"f_Wout{s_}")
            for T in range(4):
                ap_ = T % 2
                BHs = [self.B(f"HT{4 * T + i}") for i in range(4)]
                Bact = self.B(f"f_act{ap_}")
                for fc in range(nch):
                    if fc == 1:
                        if pending is not None:
                            emit_wout(*pending)
                            pending = None
                        if T == 0 and gi + 1 < len(groups):
                            load_w(gi + 1)
                    par = it % 2
                    it += 1
                    fi = f0 // 128 + fc
                    psG = self.pbank(2 * par)
                    psU = self.pbank(2 * par + 1)
                    BG = self.B(f"bank{2 * par}")
                    BU = self.B(f"bank{2 * par + 1}")
                    for k in range(8):
                        self.MM(psG, Win[s_][:, k, fc * 128:(fc + 1) * 128], HT[:, k, T * 512:(T + 1) * 512], k == 0, k == 7,
                                R=BHs + [Bw], W=[BG] if k == 0 else (), WA=() if k == 0 else [BG])
                    for k in range(8):
                        self.MM(psU, Win[s_][:, k, 512 + fc * 128:512 + (fc + 1) * 128], HT[:, k, T * 512:(T + 1) * 512], k == 0, k == 7,
                                R=BHs + [Bw], W=[BU] if k == 0 else (), WA=() if k == 0 else [BU])
                    Bgb = self.B(f"f_gb{par}")
                    Bc = self.B(f"f_carry{fc}")
                    if T == 0:
                        self.MEMSET("pool", gb[par][:, 0:2], 0.0, W=[Bgb])
                    else:
                        self.CP("pool", gb[par][:, 0:2], carry[:, fc, :], R=[Bc], W=[Bgb])
                    self.CP("act", gb[par][:, 2:514], psG, R=[BG], WA=[Bgb])
                    self.CP("pool", carry[:, fc, :], gb[par][:, 512:514], R=[Bgb], W=[Bc])
                    Btb = self.B(f"f_tb{par}")
                    self.TS("pool", tb[par], gb[par][:, 2:514], cw[:, fi, 2:3], cb[:, fi:fi + 1], ALU.mult, ALU.add, R=[Bgb, cB], W=[Btb])
                    self.STT(tb[par], gb[par][:, 1:513], cw[:, fi, 1:2], tb[par], ALU.mult, ALU.add, R=[Bgb, cB, Btb], WA=[Btb])
                    self.STT(tb[par], gb[par][:, 0:512], cw[:, fi, 0:1], tb[par], ALU.mult, ALU.add, R=[Bgb, cB, Btb], WA=[Btb])
                    Bsl = self.B(f"f_sl{par}")
                    self.ACT(sl_[par], tb[par], AF.Silu, R=[Btb], W=[Bsl])
                    self.TT("dve", actT[ap_][:, fc, :], sl_[par], psU, ALU.mult, R=[Bsl, BU], W=[Bact] if fc == 0 else (), WA=() if fc == 0 else [Bact])
                pending = (gi, T)
        emit_wout(*pending)

    def emit_ret(self, li):
        j = li // 2
        cB = self.B("consts")
        X, HT = self.X, self.HT
        WA = self.carve([8, 1536], BF16)
        WB = [self.carve([4, D], BF16) for _ in range(2)]
        St = self.carve([2, 512], F32)
        Sbf = [self.carve([2, 512], BF16) for _ in range(2)]
        rt = [self.carve([2, 128], F32) for _ in range(4)]
        qkr = [self.carve([2, 2, 128], BF16) for _ in range(2)]
        vbf = [self.carve([512], BF16) for _ in range(2)]
        sg = [self.carve([512], F32) for _ in range(2)]
        kd = [self.carve([256], BF16) for _ in range(2)]
        qkT = [self.carve([4, 128], BF16) for _ in range(2)]
        innT = [self.carve([128], BF16) for _ in range(2)]
        gated = [self.carve([512], BF16) for _ in range(2)]
        goT = [self.carve([4, 128], BF16) for _ in range(2)]
        junk = self.carve([512], BF16)
        sm = self.carve([16], F32)
        self.emit_norm(self.nmg[:, li * 8:(li + 1) * 8])
        wq = self.r_wqkvg[self.ret_js.index(j)]
        wo = self.r_wo[self.ret_js.index(j)]
        rcos = self.rcs[:, 0]
        rsin = self.rcs[:, 1]
        Brcs = self.B("rcs")
        gam = [1.0 - 2.0 ** (-5.0 - h) for h in range(4)]

        def load_w(h):
            blocks = [(h * 256, 256, 0), (1024 + h * 256, 256, 256), (2048 + h * 512, 512, 512), (4096 + h * 512, 512, 1024)]
            names = ["r_Wqk", "r_Wqk", "r_Wv", "r_Wg"]
            first = {"r_Wqk": True, "r_Wv": True, "r_Wg": True}
            for (c0, n, d0), nm in zip(blocks, names):
                src = wq[:, c0:c0 + n].rearrange("(k p) c -> p k c", p=128)
                Bw = self.B(nm)
                self.DMA("pool", WA[:, :, d0:d0 + n], src, nm, W=[Bw] if first[nm] else (), WA=() if first[nm] else [Bw])
                first[nm] = False
            s_ = h % 2
            self.DMA("pool", WB[s_], wo[h * 512:(h + 1) * 512, :].rearrange("(c p) d -> p c d", p=128), f"r_WB{s_}", W=[self.B(f"r_WB{s_}")])

        load_w(0)
        for h in range(4):
            s_ = h % 2
            Bwo = self.B(f"r_WB{s_}")
            cd = gam[h] ** 128
            qd = self.rcol[:, h:h + 1]
            qd2 = self.rcol[:, 4 + h:5 + h]
            kdc = self.rcol[:, 8 + h:9 + h]
            maskT = self.rmask[:, h, :]

            def stageA(n):
                p = n % 2
                tsl = slice(n * 128, (n + 1) * 128)
                BHt = self.B(f"HT{n}")
                names = ["r_Wqk", "r_Wv", "r_Wg"]
                for b in range(3):
                    Bk = self.B(f"bank{b}")
                    Bw = self.B(names[b])
                    for k in range(8):
                        self.MM(self.pbank(b), HT[:, k, tsl], WA[:, k, b * 512:(b + 1) * 512], k == 0, k == 7, R=[BHt, Bw],
                                W=[Bk] if k == 0 else (), WA=() if k == 0 else [Bk])
                B0, B1, B2 = self.B("bank0"), self.B("bank1"), self.B("bank2")
                v4 = self.pbank(0).rearrange("p (a i e) -> p a i e", i=128, e=2)
                E = v4[:, :, :, 0]
                O = v4[:, :, :, 1]
                cb_ = rcos[:, n, :].unsqueeze(1).to_broadcast([128, 2, 128])
                sb_ = rsin[:, n, :].unsqueeze(1).to_broadcast([128, 2, 128])
                Brt = self.B("r_rt")
                self.TT("dve", rt[0], E, cb_, ALU.mult, R=[B0, Brcs], W=[Brt])
                self.TT("dve", rt[1], O, sb_, ALU.mult, R=[B0, Brcs], WA=[Brt])
                self.TT("dve", rt[2], O, cb_, ALU.mult, R=[B0, Brcs], WA=[Brt])
                self.TT("dve", rt[3], E, sb_, ALU.mult, R=[B0, Brcs], WA=[Brt])
                Bqr = self.B(f"r_qkr{p}")
                self.TT("dve", qkr[p][:, :, 0, :], rt[0], rt[1], ALU.subtract, R=[Brt], W=[Bqr])
                self.TT("dve", qkr[p][:, :, 1, :], rt[2], rt[3], ALU.add, R=[Brt], WA=[Bqr])
                self.CP("act", vbf[p], self.pbank(1), R=[B1], W=[self.B(f"r_v{p}")])
                self.ACT(sg[p], self.pbank(2), AF.Silu, R=[B2], W=[self.B(f"r_sg{p}")])
                kflat = qkr[p][:, 1].rearrange("p a b -> p (a b)")
                self.ACT(kd[p], kflat, AF.Copy, R=[Bqr, cB], W=[self.B(f"r_kd{p}")], scale=kdc)

            def stageA2(n):
                p = n % 2
                Bqr = self.B(f"r_qkr{p}")
                ptb = self.pbank(3, BF16)[:, 0:512].rearrange("p (i t) -> p i t", t=128)
                Bpt = self.B("bank3a")
                qflat = qkr[p].rearrange("p a b c -> p (a b c)")
                for i in range(4):
                    self.TR(ptb[:, i, :], qflat[:, i * 128:(i + 1) * 128], R=[Bqr], W=[Bpt] if i == 0 else (), WA=() if i == 0 else [Bpt])
                self.CP("act", qkT[p], ptb, R=[Bpt], W=[self.B(f"r_qkT{p}")])

            def stageB(n):
                p = n % 2
                BqkT = self.B(f"r_qkT{p}")
                Bv = self.B(f"r_v{p}")
                pin = self.pbank(3)[:, 256:384]
                Bin = self.B("bank3b")
                for dc in range(2):
                    self.MM(pin, qkT[p][:, 2 + dc, :], qkT[p][:, dc, :], dc == 0, dc == 1, R=[BqkT], W=[Bin] if dc == 0 else (), WA=() if dc == 0 else [Bin])
                Bit = self.B(f"r_innT{p}")
                self.TT("dve", innT[p], pin, maskT, ALU.mult, R=[Bin, cB], W=[Bit])
                BO = self.B("bank4")
                pO = self.pbank(4)
                sp_ = (n - 1) % 2
                self.MM(pO, innT[p], vbf[p], True, n == 0, R=[Bit, Bv], W=[BO])
                if n > 0:
                    Bs = self.B(f"r_Sbf{sp_}")
                    for dc in range(2):
                        self.MM(pO, qkT[p][:, dc, :], Sbf[sp_][:, dc, :], False, dc == 1, R=[BqkT, Bs], WA=[BO])
                Bkd = self.B(f"r_kd{p}")
                BSt = self.B("r_St")
                if n < NT - 1:
                    for dc in range(2):
                        Bd = self.B(f"bank{5 + dc}")
                        self.MM(self.pbank(5 + dc), kd[p][:, dc * 128:(dc + 1) * 128], vbf[p], True, True, R=[Bkd, Bv], W=[Bd])
                        if n == 0:
                            self.CP("dve", St[:, dc, :], self.pbank(5 + dc), R=[Bd], W=[BSt] if dc == 0 else (), WA=() if dc == 0 else [BSt])
                        else:
                            self.STT(St[:, dc, :], St[:, dc, :], cd, self.pbank(5 + dc), ALU.mult, ALU.add, R=[Bd, BSt], WA=[BSt])
                    self.CP("pool", Sbf[p], St, R=[BSt], W=[self.B(f"r_Sbf{p}")])
                Bsm = self.B(f"r_sm{p}")
                o_ = p * 8
                self.ACT(junk, pO, AF.Square, R=[BO], W=[Bsm], WA=[self.B("r_junk")], accum=sm[:, o_:o_ + 1])
                self.TT("dve", sm[:, o_ + 1:o_ + 2], sm[:, o_:o_ + 1], qd2, ALU.mult, R=[Bsm, cB], WA=[Bsm])
                self.emit_rstd(sm[:, o_ + 2:o_ + 3], sm[:, o_ + 1:o_ + 2], 1.0, R=[Bsm], W=[self.B(f"r_rs{p}")])
                self.TT("dve", sm[:, o_ + 3:o_ + 4], sm[:, o_ + 2:o_ + 3], qd, ALU.mult, R=[self.B(f"r_rs{p}"), cB], W=[self.B(f"r_rq{p}")])
                Bg = self.B(f"r_gated{p}")
                self.STT(gated[p], pO, sm[:, o_ + 3:o_ + 4], sg[p], ALU.mult, ALU.mult, R=[BO, self.B(f"r_rq{p}"), self.B(f"r_sg{p}")], W=[Bg])

            def stageB2(n):
                p = n % 2
                Bg = self.B(f"r_gated{p}")
                ptb = self.pbank(7, BF16)[:, 0:512].rearrange("p (i t) -> p i t", t=128)
                Bpt = self.B("bank7")
                for i in range(4):
                    self.TR(ptb[:, i, :], gated[p][:, i * 128:(i + 1) * 128], R=[Bg], W=[Bpt] if i == 0 else (), WA=() if i == 0 else [Bpt])
                BgT = self.B(f"r_goT{p}")
                self.CP("act", goT[p], ptb, R=[Bpt], W=[BgT])
                BXt = self.B(f"X{n}")
                tslx = n
                for dh in range(2):
                    Bk = self.B("bank6") if dh == 0 else self.B("bank5")
                    bk = 6 if dh == 0 else 5
                    for ec in range(4):
                        self.MM(self.pbank(bk), goT[p][:, ec, :], WB[s_][:, ec, dh * 512:(dh + 1) * 512], ec == 0, ec == 3, R=[BgT, Bwo],
                                W=[Bk] if ec == 0 else (), WA=() if ec == 0 else [Bk])
                    xs = X[:, tslx, dh * 512:(dh + 1) * 512]
                    self.TT("dve", xs, xs, self.pbank(bk), ALU.add, R=[Bk, BXt], WA=[BXt])

            stageA(0)
            stageA2(0)
            for n in range(NT):
                if n + 1 < NT:
                    stageA(n + 1)
                    if n + 1 == NT - 1 and h + 1 < 4:
                        load_w(h + 1)
                stageB(n)
                if n + 1 < NT:
                    stageA2(n + 1)
                if n >= 1:
                    stageB2(n - 1)
            stageB2(NT - 1)

    def emit_out(self, s):
        junk = self.carve([D], BF16)
        ob = [self.carve([D], F32) for _ in range(2)]
        ss = self.carve([4], F32)
        gfull = self.carve([D], F32)
        Bg = self.B("o_g")
        self.DMA("sp", gfull, self.fng_d, "o_g", W=[Bg])
        for t in range(NT):
            p = t % 2
            BXt = self.B(f"X{t}")
            Bob = self.B(f"o_b{p}")
            if self.final_norm:
                Bss = self.B(f"o_ss{p}")
                self.ACT(junk, self.X[:, t, :], AF.Square, R=[BXt], W=[Bss], WA=[self.B("o_junk")], accum=ss[:, p:p + 1])
                self.emit_rstd(ss[:, 2 + p:3 + p], ss[:, p:p + 1], 1.0 / D, R=[Bss], W=[self.B(f"o_rs{p}")])
                self.STT(ob[p], self.X[:, t, :], ss[:, 2 + p:3 + p], gfull, ALU.mult, ALU.mult, R=[BXt, self.B(f"o_rs{p}"), Bg], W=[Bob])
            else:
                self.CP("dve", ob[p], self.X[:, t, :], R=[BXt], W=[Bob])
            self.DMA("sp", self.out_d[s, t * 128:(t + 1) * 128, :], ob[p], "out", R=[Bob], WA=[self.B("out")])


def _consts():
    ident = np.eye(128, dtype=np.float32)
    cm = np.where(np.arange(128)[:, None] <= np.arange(128)[None, :], 0.0, -30000.0).astype(np.float32)
    cbf = np.concatenate([ident, cm], axis=1).astype(ml_dtypes.bfloat16)
    afreq = (500000.0 ** (-np.arange(0, 16, 2, dtype=np.float32) / np.float32(16))).astype(np.float32)
    rfreq = (1.0 / (10000.0 ** np.linspace(0.0, 1.0, 128, dtype=np.float32))).astype(np.float32)
    cf = np.zeros((128, 664), np.float32)
    cf[:, 0:8] = afreq[None, :]
    cf[:, 8:136] = rfreq[None, :]
    idx = np.arange(128, dtype=np.float64)
    for h in range(4):
        gam = 1.0 - 2.0 ** (-5.0 - h)
        m = np.where(idx[None, :] >= idx[:, None], (gam ** (-(idx[:, None] + 1.0))) / 16.0, 0.0)
        cf[:, 136 + h * 128:136 + (h + 1) * 128] = m
        qd = gam ** (idx + 1.0)
        cf[:, 648 + h] = qd
        cf[:, 652 + h] = qd * qd / 512.0
        cf[:, 656 + h] = gam ** (127.0 - idx) / 16.0
    return cbf, cf


_NC_CACHE = {}


def _get_nc(layers, nseq, final_norm):
    key = (tuple(layers), nseq, final_norm)
    if key not in _NC_CACHE:
        _NC_CACHE[key] = Builder(list(layers), nseq, final_norm).build()
    return _NC_CACHE[key]


PLAN = [[0, 1, 2, 3]]


def _norm_layers(layers):
    return tuple(e if isinstance(e, tuple) else (e, True, True) for e in layers)


def _run(inputs, layers=None, final_norm=True, plan=None):
    f = lambda a: np.ascontiguousarray(np.asarray(a, dtype=np.float32))
    x = f(inputs["x"])
    pos = np.ascontiguousarray(np.asarray(inputs["positions"], dtype=np.int32))
    cbf, cf = _consts()
    fm = lambda g: np.ascontiguousarray(f(g).reshape(DEPTH, 8, 128).transpose(2, 0, 1).reshape(128, DEPTH * 8))
    lam = np.concatenate([f(inputs["attn_lambda_q1"]), f(inputs["attn_lambda_k1"]),
                          f(inputs["attn_lambda_q2"]), f(inputs["attn_lambda_k2"])], axis=1)
    lam = np.ascontiguousarray(np.broadcast_to(lam.reshape(1, 512), (128, 512)))
    subln = np.ascontiguousarray(f(inputs["attn_subln_g"]).T)
    cw = f(inputs["ffn_conv_w"]).reshape(DEPTH, 3, NFC, 128).transpose(3, 0, 2, 1)
    cw = np.ascontiguousarray(cw.reshape(128, DEPTH * NFC * 3))
    cb = np.ascontiguousarray(f(inputs["ffn_conv_b"]).reshape(DEPTH, NFC, 128).transpose(2, 0, 1).reshape(128, DEPTH * NFC))
    small = dict(
        nmg=fm(inputs["norm_mix_g"]), nfg=fm(inputs["norm_ffn_g"]),
        fng=np.ascontiguousarray(np.broadcast_to(f(inputs["final_norm_g"]).reshape(1, D), (128, D))),
        lam_in=lam, subln=subln, f_cw=cw, f_cb=cb, c_bf=cbf, c_f32=cf,
    )
    if plan is None:
        plan = [list(layers)] if layers is not None else PLAN
    pcs = [np.ascontiguousarray(pos[c * NSEQ:(c + 1) * NSEQ].reshape(NSEQ, NT, 128).transpose(0, 2, 1)) for c in range(N_CORES)]
    cur = x
    for li_, lay in enumerate(plan):
        lay = _norm_layers(lay)
        fn = final_norm and (li_ == len(plan) - 1)
        key = (lay, NSEQ, fn)
        if key not in _NC_CACHE:
            b = Builder(list(lay), NSEQ, fn)
            _NC_CACHE[key] = (b.build(), b)
        nc, b = _NC_CACHE[key]
        shared = dict(small)
        if b.attn_js:
            shared["a_wqkv"] = f(inputs["attn_w_qkv"])[b.attn_js]
            shared["a_wo"] = f(inputs["attn_w_o"])[b.attn_js]
        if b.ret_js:
            shared["r_wqkvg"] = f(inputs["ret_w_qkvg"])[b.ret_js]
            shared["r_wo"] = f(inputs["ret_w_o"])[b.ret_js]
        if b.ffn_ls:
            shared["f_win"] = f(inputs["ffn_w_in"])[b.ffn_ls]
            shared["f_wout"] = f(inputs["ffn_w_out"])[b.ffn_ls]
        in_maps = []
        for c in range(N_CORES):
            m = dict(shared)
            m["x"] = np.ascontiguousarray(cur[c * NSEQ:(c + 1) * NSEQ])
            m["pos"] = pcs[c]
            in_maps.append(m)
        res = run_bass_kernel_spmd(nc, in_maps, core_ids=list(range(N_CORES)))
        cur = np.concatenate([r["out"] for r in res.results], axis=0)
    return cur


def kernel(**inputs):
    return _run(inputs)
```

```python
import math
import numpy as np
import ml_dtypes
import concourse.bass as bass
import concourse.mybir as mybir
from concourse.bass_utils import run_bass_kernel_spmd

dt = mybir.dt
F32, BF16, I32 = dt.float32, dt.bfloat16, dt.int32
AF = mybir.ActivationFunctionType
ALU = mybir.AluOpType
COMPUTE = ("pe", "act", "dve", "pool")

D = 1024
S = 2048
NT = 16
NSEQ = 2
DEPTH = 4
FFN = 2816
NFC = 22
EPS = 1e-6
N_CORES = 8
PI = math.pi
EPOCH = 1024


class Buf:
    __slots__ = ("name", "w", "r")

    def __init__(self, name):
        self.name = name
        self.w = {}
        self.r = {}


class Op:
    __slots__ = ("eng", "fn", "deps", "sig", "val", "key", "is_dma", "order", "epoch")

    def __init__(self, eng, fn, is_dma=False, key=None):
        self.eng = eng
        self.fn = fn
        self.deps = {}
        self.sig = False
        self.val = 0
        self.key = key if key is not None else eng
        self.is_dma = is_dma


class Prog:
    def __init__(self):
        self.streams = {e: [] for e in ("pe", "act", "dve", "pool", "sp")}
        self.dma_cnt = {}
        self.all_ops = []

    def op(self, eng, fn, R=(), W=(), WA=()):
        o = Op(eng, fn)
        self._track(o, R, W, WA)
        return o

    def dma(self, eng, fn, key, R=(), W=(), WA=()):
        o = Op(eng, fn, is_dma=True, key="dma:" + key)
        self.dma_cnt[key] = self.dma_cnt.get(key, 0) + 1
        o.val = 16 * self.dma_cnt[key]
        o.sig = True
        self._track(o, R, W, WA)
        return o

    def _track(self, o, R, W, WA):
        o.order = len(self.all_ops)
        self.all_ops.append(o)
        self.streams[o.eng].append(o)
        for b in R:
            for s in b.w.values():
                self._add(o, s, True)
        for b in list(W) + list(WA):
            for s in b.w.values():
                self._add(o, s, False)
            for s in b.r.values():
                self._add(o, s, False)
        for b in R:
            b.r[o.key] = o
        for b in W:
            b.w = {o.key: o}
            b.r = {}
        for b in WA:
            b.w[o.key] = o

    def _add(self, o, s, raw):
        if s is o:
            return
        if (not s.is_dma) and (not o.is_dma) and s.eng == o.eng:
            if not raw or s.eng == "pe":
                return
        cur = o.deps.get(s.key)
        if cur is None or cur.order < s.order:
            o.deps[s.key] = s

    def finalize(self):
        for o in self.all_ops:
            for s in o.deps.values():
                s.sig = True
        cnt = {e: 0 for e in COMPUTE}
        for o in self.all_ops:
            if not o.is_dma and o.sig:
                cnt[o.eng] += 1
                o.epoch = (cnt[o.eng] - 1) // EPOCH
                o.val = (cnt[o.eng] - 1) % EPOCH + 1
        self.n_epochs = {e: (cnt[e] + EPOCH - 1) // EPOCH for e in COMPUTE}

    def replay(self, name, eng, sems, dma_sems):
        waited = {}
        for o in self.streams[name]:
            for k, s in o.deps.items():
                if s.is_dma:
                    if waited.get(k, 0) < s.val:
                        eng.wait_ge(dma_sems[k[4:]], s.val)
                        waited[k] = s.val
                else:
                    tv = (s.epoch, s.val)
                    if waited.get(k, (-1, 0)) < tv:
                        eng.wait_ge(sems[(s.eng, s.epoch)], s.val)
                        waited[k] = tv
            if o.fn is None:
                continue
            ins = o.fn(eng)
            if o.is_dma:
                ins.then_inc(dma_sems[o.key[4:]], 16)
            elif o.sig:
                ins.then_inc(sems[(o.eng, o.epoch)], 1)


class Builder:
    def __init__(self, layers, nseq, final_norm):
        self.layers = layers
        self.nseq = nseq
        self.final_norm = final_norm
        self.P = Prog()
        self.nc = bass.Bass("TRN2", target_bir_lowering=False)
        self.bufs = {}

    def B(self, name):
        b = self.bufs.get(name)
        if b is None:
            b = self.bufs[name] = Buf(name)
        return b

    def Bs(self, names):
        return [self.B(n) for n in names]

    def xb(self, *aps):
        out = []
        for a in aps:
            try:
                if a.tensor.name != "psum":
                    continue
            except AttributeError:
                continue
            es = 4 if a.dtype in (F32, I32) else 2
            b = (a.offset * es) // 2048
            bb = self.B(f"xbank{b}")
            if bb not in out:
                out.append(bb)
        return out

    def dram_in(self, name, shape, d=F32):
        return self.nc.dram_tensor(name, list(shape), d, kind="ExternalInput").ap()

    def MM(self, out, lhsT, rhs, start, stop, R, W=(), WA=()):
        self.P.op("pe", lambda e: e.matmul(out, lhsT=lhsT, rhs=rhs, start=start, stop=stop,
                                           skip_group_check=True), R=R, W=list(W) + self.xb(out), WA=WA)

    def TR(self, out, in_, R, W=(), WA=()):
        ident = self.ident
        self.P.op("pe", lambda e: e.transpose(out=out, in_=in_, identity=ident), R=list(R) + [self.B("consts")], W=list(W) + self.xb(out), WA=WA)

    def ACT(self, out, in_, func, R, W=(), WA=(), scale=None, bias=None, accum=None):
        kw = {}
        if scale is not None:
            kw["scale"] = scale
        if bias is not None:
            kw["bias"] = bias
        if accum is not None:
            kw["accum_out"] = accum
        self.P.op("act", lambda e: e.activation(out=out, in_=in_, func=func, **kw), R=R, W=list(W) + self.xb(out, in_), WA=WA)

    def TT(self, eng, out, in0, in1, op, R, W=(), WA=()):
        self.P.op(eng, lambda e: e.tensor_tensor(out=out, in0=in0, in1=in1, op=op), R=R, W=list(W) + self.xb(out, in0, in1), WA=WA)

    def TS(self, eng, out, in0, s1, s2, op0, op1, R, W=(), WA=()):
        if op1 is None:
            self.P.op(eng, lambda e: e.tensor_scalar(out=out, in0=in0, scalar1=s1, scalar2=None, op0=op0), R=R, W=list(W) + self.xb(out, in0), WA=WA)
        else:
            self.P.op(eng, lambda e: e.tensor_scalar(out=out, in0=in0, scalar1=s1, scalar2=s2, op0=op0, op1=op1), R=R, W=list(W) + self.xb(out, in0), WA=WA)

    def STT(self, out, in0, scalar, in1, op0, op1, R, W=(), WA=(), accum=None):
        if accum is None:
            self.P.op("dve", lambda e: e.scalar_tensor_tensor(out=out, in0=in0, scalar=scalar, in1=in1, op0=op0, op1=op1), R=R, W=list(W) + self.xb(out, in0, in1), WA=WA)
        else:
            self.P.op("dve", lambda e: e.scalar_tensor_tensor(out=out, in0=in0, scalar=scalar, in1=in1, op0=op0, op1=op1, accum_out=accum), R=R, W=list(W) + self.xb(out, in0, in1), WA=WA)

    def CP(self, eng, out, in_, R, W=(), WA=()):
        if eng == "act":
            self.P.op("act", lambda e: e.copy(out=out, in_=in_), R=R, W=list(W) + self.xb(out, in_), WA=WA)
        else:
            self.P.op(eng, lambda e: e.tensor_copy(out=out, in_=in_), R=R, W=list(W) + self.xb(out, in_), WA=WA)

    def MEMSET(self, eng, ap, val, W=(), WA=()):
        self.P.op(eng, lambda e: e.memset(ap, val), W=W, WA=WA)

    def DMA(self, eng, out, in_, key, R=(), W=(), WA=()):
        self.P.dma(eng, lambda e: e.dma_start(out=out, in_=in_), key, R=R, W=W, WA=WA)

    def barrier(self):
        allb = list(self.bufs.values())
        P = self.P
        for e in ("pe", "act", "dve", "pool", "sp"):
            o = Op(e, None)
            o.order = len(P.all_ops)
            for b in allb:
                for src in list(b.w.values()) + list(b.r.values()):
                    if src.fn is None:
                        continue
                    if (not src.is_dma) and src.eng == e:
                        continue
                    cur = o.deps.get(src.key)
                    if cur is None or cur.order < src.order:
                        o.deps[src.key] = src
            P.all_ops.append(o)
            P.streams[e].append(o)

    def arena_reset(self):
        self.aoff = 0

    def carve(self, shape, d):
        n = 1
        for v in shape:
            n *= v
        nbytes = n * (4 if d in (F32, I32) else 2)
        nbytes = (nbytes + 31) // 32 * 32
        o2 = self.aoff // 2
        assert self.aoff + nbytes <= self.arena_bytes, (self.aoff, nbytes, self.arena_bytes)
        v = self.arena[:, o2:o2 + nbytes // 2]
        self.aoff += nbytes
        if d != BF16:
            v = v.bitcast(d)
        v = v[:, 0:n]
        if len(shape) == 2:
            v = v.rearrange("p (a b) -> p a b", b=shape[1])
        elif len(shape) == 3:
            v = v.rearrange("p (a b c) -> p a b c", b=shape[1], c=shape[2])
        return v

    def pbank(self, i, d=F32):
        v = self.psum[:, i * 512:(i + 1) * 512]
        if d == BF16:
            v = v.bitcast(BF16)
        return v

    def build(self):
        nc = self.nc
        ns = self.nseq
        self.x_d = self.dram_in("x", [ns, S, D])
        self.pos_d = self.dram_in("pos", [ns, 128, NT], I32)
        self.nmg_d = self.dram_in("nmg", [128, DEPTH * 8])
        self.nfg_d = self.dram_in("nfg", [128, DEPTH * 8])
        self.fng_d = self.dram_in("fng", [128, D])
        self.lam_d = self.dram_in("lam_in", [128, 2 * 256])
        self.subln_d = self.dram_in("subln", [128, 2])
        self.cw_d = self.dram_in("f_cw", [128, DEPTH * NFC * 3])
        self.cb_d = self.dram_in("f_cb", [128, DEPTH * NFC])
        ents = [e if isinstance(e, tuple) else (e, True, True) for e in self.layers]
        self.attn_js = sorted({li // 2 for li, m, f in ents if m and li % 2 == 0})
        self.ret_js = sorted({li // 2 for li, m, f in ents if m and li % 2 == 1})
        self.ffn_ls = sorted({li for li, m, f in ents if f})
        if self.attn_js:
            self.a_wqkv = self.dram_in("a_wqkv", [len(self.attn_js), D, 3 * D])
            self.a_wo = self.dram_in("a_wo", [len(self.attn_js), D, D])
        if self.ret_js:
            self.r_wqkvg = self.dram_in("r_wqkvg", [len(self.ret_js), D, 6144])
            self.r_wo = self.dram_in("r_wo", [len(self.ret_js), 2048, D])
        if self.ffn_ls:
            self.f_win = self.dram_in("f_win", [len(self.ffn_ls), D, 2 * FFN])
            self.f_wout = self.dram_in("f_wout", [len(self.ffn_ls), FFN, D])
        self.cbf_d = self.dram_in("c_bf", [128, 256], BF16)
        self.cf_d = self.dram_in("c_f32", [128, 8 + 128 + 512 + 16])
        self.out_d = nc.dram_tensor("out", [ns, S, D], F32, kind="ExternalOutput").ap()

        self.X = nc.alloc_sbuf_tensor("X", [128, NT, D], F32)[:]
        self.HT = nc.alloc_sbuf_tensor("HT", [128, 8, S], BF16)[:]
        self.cbf = nc.alloc_sbuf_tensor("cbf", [128, 256], BF16)[:]
        self.cf = nc.alloc_sbuf_tensor("cf", [128, 664], F32)[:]
        self.ident = self.cbf[:, 0:128]
        self.cmask = self.cbf[:, 128:256]
        self.afreq = self.cf[:, 0:8]
        self.rfreq = self.cf[:, 8:136]
        self.rmask = self.cf[:, 136:648].rearrange("p (h i) -> p h i", i=128)
        self.rcol = self.cf[:, 648:664]
        self.params = nc.alloc_sbuf_tensor("params", [128, 32 + 32 + 512 + 2 + 264 + 88], F32)[:]
        o = 0
        self.nmg = self.params[:, o:o + 32]; o += 32
        self.nfg = self.params[:, o:o + 32]; o += 32
        self.lamin = self.params[:, o:o + 512]; o += 512
        self.subln = self.params[:, o:o + 2]; o += 2
        self.cw = self.params[:, o:o + 264]; o += 264
        self.cb = self.params[:, o:o + 88]; o += 88
        self.rcs = nc.alloc_sbuf_tensor("rcs", [128, 2, NT, 128], F32)[:]
        self.acs = nc.alloc_sbuf_tensor("acs", [128, 2, NT, 8], F32)[:]
        self.posf = nc.alloc_sbuf_tensor("posf", [128, NT], F32)[:]
        self.posi = nc.alloc_sbuf_tensor("posi", [128, NT], I32)[:]
        self.small = nc.alloc_sbuf_tensor("small", [128, 64], F32)[:]
        self.arena_bytes = (nc.sbuf_bytes_remaining - 256) // 64 * 64
        self.arena = nc.alloc_sbuf_tensor("arena", [128, self.arena_bytes // 2], BF16)[:]
        self.psum = nc.alloc_psum_tensor("psum", [128, 4096], F32)[:]

        cB = self.B("consts")
        self.DMA("sp", self.cbf, self.cbf_d, "consts", W=[cB])
        self.DMA("sp", self.cf, self.cf_d, "consts", WA=[cB])
        self.DMA("sp", self.nmg, self.nmg_d, "consts", WA=[cB])
        self.DMA("sp", self.nfg, self.nfg_d, "consts", WA=[cB])
        self.DMA("sp", self.lamin, self.lam_d, "consts", WA=[cB])
        self.DMA("sp", self.subln, self.subln_d, "consts", WA=[cB])
        self.DMA("sp", self.cw, self.cw_d, "consts", WA=[cB])
        self.DMA("sp", self.cb, self.cb_d, "consts", WA=[cB])
        self.MEMSET("dve", self.small[:, 0:1], EPS, WA=[cB])

        for s in range(ns):
            self.emit_seq(s)
        self.P.op("sp", None, R=[self.B("out")])
        self.P.finalize()

        from contextlib import ExitStack
        with ExitStack() as es:
            sems = {(e, k): es.enter_context(nc.semaphore(f"s_{e}{k}")) for e in COMPUTE for k in range(self.P.n_epochs[e])}
            dsems = {k: es.enter_context(nc.semaphore("d_" + k)) for k in self.P.dma_cnt}
            P = self.P
            with nc.Block() as block:
                @block.tensor
                def _(e):
                    P.replay("pe", e, sems, dsems)

                @block.scalar
                def _(e):
                    P.replay("act", e, sems, dsems)

                @block.vector
                def _(e):
                    P.replay("dve", e, sems, dsems)

                @block.gpsimd
                def _(e):
                    P.replay("pool", e, sems, dsems)

                @block.sync
                def _(e):
                    P.replay("sp", e, sems, dsems)
        return nc

    def emit_seq(self, s):
        BX = [self.B(f"X{t}") for t in range(NT)]
        self.barrier()
        for t in range(NT):
            self.DMA("sp", self.X[:, t, :], self.x_d[s, t * 128:(t + 1) * 128, :], f"X{t}", W=[BX[t]])
        self.DMA("sp", self.posi, self.pos_d[s], "pos", W=[self.B("posi")])
        self.CP("dve", self.posf, self.posi, R=[self.B("posi")], W=[self.B("posf")])
        self.arena_reset()
        self.emit_sincos(self.afreq, 8, self.acs, "acs")
        self.arena_reset()
        self.emit_sincos(self.rfreq, 128, self.rcs, "rcs")
        for ent in self.layers:
            li, do_mix, do_ffn = ent if isinstance(ent, tuple) else (ent, True, True)
            if do_mix:
                self.barrier()
                self.arena_reset()
                if li % 2 == 0:
                    self.emit_attn(li)
                else:
                    self.emit_ret(li)
            if do_ffn:
                self.barrier()
                self.arena_reset()
                self.emit_ffn(li)
        self.barrier()
        self.arena_reset()
        self.emit_out(s)

    def emit_sincos(self, freq, F, dst, name):
        cB = self.B("consts")
        Bt = self.B("sc_tmp")
        Bd = self.B(name)
        ang = self.carve([NT, F], F32)
        ki = self.carve([NT, F], I32)
        kf = self.carve([NT, F], F32)
        y = self.carve([NT, F], F32)
        fb = freq.unsqueeze(1).to_broadcast([128, NT, F])
        pb = self.posf.unsqueeze(2).to_broadcast([128, NT, F])
        self.TT("dve", ang, fb, pb, ALU.mult, R=[cB, self.B("posf")], W=[Bt])
        self.TS("dve", ki, ang, 1.0 / (2 * PI), None, ALU.mult, None, R=[Bt], WA=[Bt])
        self.CP("dve", kf, ki, R=[Bt], WA=[Bt])
        self.STT(ang, kf, -2 * PI, ang, ALU.mult, ALU.add, R=[Bt], WA=[Bt])
        for idx, shift in ((1, 0.0), (0, PI / 2)):
            self.TS("dve", y, ang, shift, None, ALU.add, None, R=[Bt], WA=[Bt])
            self.TS("dve", kf, y, PI, -2 * PI, ALU.is_gt, ALU.mult, R=[Bt], WA=[Bt])
            self.TT("dve", y, y, kf, ALU.add, R=[Bt], WA=[Bt])
            self.TS("dve", kf, y, -PI, 2 * PI, ALU.is_lt, ALU.mult, R=[Bt], WA=[Bt])
            self.TT("dve", y, y, kf, ALU.add, R=[Bt], WA=[Bt])
            self.ACT(dst[:, idx], y, AF.Sin, R=[Bt], WA=[Bd])

    def emit_rstd(self, out, in_, scale, R, W):
        self.ACT(out, in_, AF.Ln, R=list(R) + [self.B("consts")], W=W, scale=scale, bias=self.small[:, 0:1])
        self.ACT(out, out, AF.Exp, R=W, WA=W, scale=-0.5)

    def emit_norm(self, gcol):
        cB = self.B("consts")
        junk = self.carve([D], BF16)
        junk2 = self.carve([D], BF16)
        xn = [self.carve([D], BF16) for _ in range(2)]
        ss = self.carve([2 * NT], F32)
        gb = gcol.unsqueeze(2).to_broadcast([128, 8, 128])
        Bss = self.B("n_ss")
        for t in range(NT):
            BXt = self.B(f"X{t}")
            first = (t == 0)
            if t % 2 == 0:
                self.ACT(junk, self.X[:, t, :], AF.Square, R=[BXt], W=[Bss] if first else (), WA=[self.B("n_junk")] + ([] if first else [Bss]),
                         accum=ss[:, t:t + 1])
            else:
                self.STT(junk2, self.X[:, t, :], 1.0, self.X[:, t, :], ALU.mult, ALU.mult, R=[BXt], WA=[self.B("n_junk2"), Bss], accum=ss[:, t:t + 1])
        Brs = self.B("n_rs")
        self.emit_rstd(ss[:, NT:2 * NT], ss[:, 0:NT], 1.0 / D, R=[Bss], W=[Brs])
        for t in range(NT):
            p = t % 2
            BXt = self.B(f"X{t}")
            Bxn = self.B(f"n_xn{p}")
            Bpt = self.B(f"bank{4 + p}")
            self.ACT(xn[p], self.X[:, t, :], AF.Copy, R=[BXt, Brs], W=[Bxn], scale=ss[:, NT + t:NT + t + 1])
            pt = self.pbank(4 + p, BF16).rearrange("p (c t) -> p c t", t=128)
            for c in range(8):
                self.TR(pt[:, c, :], xn[p][:, c * 128:(c + 1) * 128], R=[Bxn], W=[Bpt] if c == 0 else (), WA=() if c == 0 else [Bpt])
            self.TT("dve", self.HT[:, :, t * 128:(t + 1) * 128], pt, gb, ALU.mult, R=[Bpt, cB], W=[self.B(f"HT{t}")])

    def emit_attn(self, li):
        j = li // 2
        lambda_init = 0.8 - 0.6 * math.exp(-0.3 * li)
        cB = self.B("consts")
        X, HT = self.X, self.HT
        WA = [self.carve([8, 768], BF16) for _ in range(2)]
        WB = [self.carve([2, D], BF16) for _ in range(2)]
        qT = self.carve([2, S], BF16)
        kT = self.carve([2, S], BF16)
        V = self.carve([NT, 2, 129], BF16)
        onT = self.carve([2, S], BF16)
        Pt = [self.carve([2, 512], BF16) for _ in range(2)]
        qkf = [self.carve([512], F32) for _ in range(2)]
        qkb = [self.carve([512], BF16) for _ in range(2)]
        rt = [self.carve([8, 8], F32) for _ in range(4)]
        onb4 = [self.carve([4, 128], BF16) for _ in range(2)]
        qkf.append(Pt[0].rearrange("p c n -> p (c n)").bitcast(F32))
        qkb.append(onb4[0].rearrange("p q e -> p (q e)"))
        qkf_names = ["a_qkf0", "a_qkf1", "a_P0"]
        qkb_names = ["a_qkb0", "a_qkb1", "a_on0"]
        sm = self.carve([32], F32)
        lamj = self.carve([64], F32)
        Bl = self.B("a_lam")
        li0 = self.lamin[:, j * 256:(j + 1) * 256]
        self.STT(lamj, li0[:, 0:64], 1.0, li0[:, 64:128], ALU.mult, ALU.mult, R=[cB], W=[Bl], accum=sm[:, 0:1])
        self.STT(lamj, li0[:, 128:192], 1.0, li0[:, 192:256], ALU.mult, ALU.mult, R=[cB], WA=[Bl], accum=sm[:, 1:2])
        self.ACT(sm[:, 2:4], sm[:, 0:2], AF.Exp, R=[Bl], WA=[Bl])
        self.TT("dve", sm[:, 4:5], sm[:, 3:4], sm[:, 2:3], ALU.subtract, R=[Bl], WA=[Bl])
        self.TS("dve", sm[:, 5:6], sm[:, 4:5], -lambda_init, None, ALU.add, None, R=[Bl], WA=[Bl])
        neglam = sm[:, 5:6]
        self.TS("dve", sm[:, 6:7], self.subln[:, j:j + 1], 1.0 - lambda_init, None, ALU.mult, None, R=[cB], WA=[Bl])
        sgcol = sm[:, 6:7]
        self.MEMSET("pool", V[:, :, :, 128:129], 1.0, W=[self.B("a_Vones")])

        mark = self.aoff
        self.emit_norm(self.nmg[:, li * 8:(li + 1) * 8])
        self.aoff = mark
        accs = self.carve([3, 387], F32)

        wq = self.a_wqkv[self.attn_js.index(j)]
        wo = self.a_wo[self.attn_js.index(j)]

        def load_w(g):
            sl = g % 2
            Bw = self.B(f"a_WA{sl}")
            for blk in range(3):
                src = wq[:, blk * D + g * 256: blk * D + (g + 1) * 256].rearrange("(k p) c -> p k c", p=128)
                self.DMA("pool", WA[sl][:, :, blk * 256:(blk + 1) * 256], src, f"a_WA{sl}", W=[Bw] if blk == 0 else (), WA=() if blk == 0 else [Bw])
            src = wo[g * 256:(g + 1) * 256, :].rearrange("(k p) c -> p k c", p=128)
            self.DMA("pool", WB[sl], src, f"a_WB{sl}", W=[self.B(f"a_WB{sl}")])

        acos = self.acs[:, 0]
        asin = self.acs[:, 1]
        load_w(0)
        for g in range(4):
            sl = g % 2
            if g + 1 < 4:
                load_w(g + 1)
            Bw = self.B(f"a_WA{sl}")
            Bwo = self.B(f"a_WB{sl}")
            def p1_mm(t):
                p = t % 3
                tsl = slice(t * 128, (t + 1) * 128)
                BHt = self.B(f"HT{t}")
                ps_qk = self.pbank(2 * p)
                ps_v = self.pbank(2 * p + 1)[:, 0:256]
                Bqk = self.B(f"bank{2 * p}")
                Bv = self.B(f"bank{2 * p + 1}")
                for k in range(8):
                    self.MM(ps_qk, HT[:, k, tsl], WA[sl][:, k, 0:512], k == 0, k == 7, R=[BHt, Bw], W=[Bqk] if k == 0 else (), WA=() if k == 0 else [Bqk])
                for k in range(8):
                    self.MM(ps_v, HT[:, k, tsl], WA[sl][:, k, 512:768], k == 0, k == 7, R=[BHt, Bw], W=[Bv] if k == 0 else (), WA=() if k == 0 else [Bv])

            def p1_post(t):
                p = t % 3
                tsl = slice(t * 128, (t + 1) * 128)
                ps_qk = self.pbank(2 * p)
                ps_v = self.pbank(2 * p + 1)[:, 0:256]
                Bqk = self.B(f"bank{2 * p}")
                Bv = self.B(f"bank{2 * p + 1}")
                BVt = self.B(f"a_V{t}")
                self.CP("act", V[:, t, :, 0:128], ps_v.rearrange("p (h e) -> p h e", e=128), R=[Bv, self.B("a_Vones")], W=[BVt])
                Bf = self.B(qkf_names[p])
                self.ACT(qkf[p][:, 0:256], ps_qk[:, 0:256], AF.Copy, R=[Bqk], W=[Bf], scale=0.125)
                self.CP("act", qkf[p][:, 256:512], ps_qk[:, 256:512], R=[Bqk], WA=[Bf])
                v3 = qkf[p].rearrange("p (g d) -> p g d", d=64)
                x1 = v3[:, :, 0:8]
                x2 = v3[:, :, 8:16]
                cb_ = acos[:, t, :].unsqueeze(1).to_broadcast([128, 8, 8])
                sb_ = asin[:, t, :].unsqueeze(1).to_broadcast([128, 8, 8])
                Brt = self.B("a_rt")
                Bacs = self.B("acs")
                self.TT("dve", rt[0], x1, cb_, ALU.mult, R=[Bf, Bacs], W=[Brt])
                self.TT("dve", rt[1], x2, sb_, ALU.mult, R=[Bf, Bacs], WA=[Brt])
                self.TT("dve", rt[2], x2, cb_, ALU.mult, R=[Bf, Bacs], WA=[Brt])
                self.TT("dve", rt[3], x1, sb_, ALU.mult, R=[Bf, Bacs], WA=[Brt])
                self.TT("dve", x1, rt[0], rt[1], ALU.subtract, R=[Brt], WA=[Bf])
                self.TT("dve", x2, rt[2], rt[3], ALU.add, R=[Brt], WA=[Bf])
                Bb = self.B(qkb_names[p])
                self.CP("pool", qkb[p], qkf[p], R=[Bf], W=[Bb])

            def p1_post_b(t):
                p = t % 3
                tsl = slice(t * 128, (t + 1) * 128)
                Bb = self.B(qkb_names[p])
                ptb = self.pbank(7, BF16)[:, 0:512].rearrange("p (i t) -> p i t", t=128)
                Bpt = self.B("bank7")
                for i in range(4):
                    self.TR(ptb[:, i, :], qkb[p][:, i * 128:(i + 1) * 128], R=[Bb], W=[Bpt] if i == 0 else (), WA=() if i == 0 else [Bpt])
                self.CP("dve", qT[:, :, tsl], ptb[:, 0:2, :], R=[Bpt], W=[self.B(f"a_qT{t}")])
                self.CP("act", kT[:, :, tsl], ptb[:, 2:4, :], R=[Bpt], W=[self.B(f"a_kT{t}")])

            p1_mm(0)
            p1_mm(1)
            p1_post(0)
            for t in range(2, NT):
                p1_mm(t)
                p1_post(t - 1)
                p1_post_b(t - 2)
            p1_post(NT - 1)
            p1_post_b(NT - 2)
            p1_post_b(NT - 1)
            steps = [(hl, qt, kb) for hl in range(2) for qt in range(4) for kb in range(4 * qt + 4)]
            nst = len(steps)
            Bacc = [self.B(f"bank{4 + b}") for b in range(3)]
            Baccs = self.B("a_accs")
            accv = accs.rearrange("p b n -> p (b n)")[:, 0:1032].rearrange("p (a n) -> p a n", n=129)
            of4 = qkf[1].rearrange("p (q e) -> p q e", e=128)
            sq4 = qkf[0].rearrange("p (q e) -> p q e", e=128)
            Bof, Bsq = self.B("a_qkf1"), self.B("a_qkf0")
            state = {"started": [False] * 3}

            def acc_ap(c, qb):
                a = c * 4 + qb
                return self.pbank(4 + a // 3)[:, (a % 3) * 129:(a % 3) * 129 + 129], a // 3

            def st_S(i):
                hl, qt, kb = steps[i]
                par = i % 2
                c0 = max(kb - 4 * qt, 0) * 128
                BqTs = [self.B(f"a_qT{4 * qt + q}") for q in range(4)]
                diag = kb - 4 * qt >= 0
                for c in range(2):
                    self.MM(self.pbank(2 * par + c)[:, c0:512], kT[c * 64:(c + 1) * 64, hl, kb * 128:(kb + 1) * 128],
                            qT[c * 64:(c + 1) * 64, hl, qt * 512 + c0:(qt + 1) * 512], True, not diag,
                            R=[self.B(f"a_kT{kb}")] + BqTs, W=[self.B(f"bank{2 * par + c}")])
                if diag:
                    for c in range(2):
                        self.MM(self.pbank(2 * par + c)[:, c0:c0 + 128], self.ident, self.cmask, False, True,
                                R=[cB], WA=[self.B(f"bank{2 * par + c}")])

            def st_exp(i):
                hl, qt, kb = steps[i]
                par = i % 2
                jd = kb - 4 * qt
                c0 = max(jd, 0) * 128
                src = self.psum[:, 2 * par * 512:(2 * par + 2) * 512].rearrange("p (c n) -> p c n", n=512)[:, :, c0:512]
                self.ACT(Pt[par][:, :, c0:512], src, AF.Exp, R=[self.B(f"bank{2 * par}"), self.B(f"bank{2 * par + 1}")],
                         W=[self.B(f"a_P{par}"), self.B(f"xbank{2 * par + 1}")])

            def st_PV(i):
                hl, qt, kb = steps[i]
                par = i % 2
                jd = kb - 4 * qt
                if kb == 0:
                    state["started"] = [False] * 3
                for c in range(2):
                    Bp = self.B(f"a_P{par}")
                    for qb in range(max(jd, 0), 4):
                        ap_, b = acc_ap(c, qb)
                        st = not state["started"][b]
                        state["started"][b] = True
                        self.MM(ap_, Pt[par][:, c, qb * 128:(qb + 1) * 128], V[:, kb, hl, :], st, kb == 4 * qt + qb,
                                R=[Bp, self.B(f"a_V{kb}")], W=[Bacc[b]] if st else (), WA=() if st else [Bacc[b]])

            def st_norm(hl, qt, dp):
                for b in range(3):
                    n_ = 387 if b < 2 else 258
                    self.CP("dve", accs[:, b, 0:n_], self.pbank(4 + b)[:, 0:n_], R=[Bacc[b]], W=[Baccs] if b == 0 else (), WA=() if b == 0 else [Baccs])
                Bsm = self.B("a_sm")
                rec = sm[:, 8:16]
                self.P.op("dve", lambda e: e.reciprocal(out=rec, in_=accv[:, :, 128]), R=[Baccs], W=[Bsm])
                self.TS("dve", sm[:, 12:16], sm[:, 12:16], neglam, None, ALU.mult, None, R=[Bsm, Bl], WA=[Bsm])
                r1 = sm[:, 8:12].unsqueeze(2).to_broadcast([128, 4, 128])
                r2 = sm[:, 12:16].unsqueeze(2).to_broadcast([128, 4, 128])
                self.TT("dve", of4, accv[:, 0:4, 0:128], r1, ALU.mult, R=[Baccs, Bsm], W=[Bof])
                self.TT("dve", sq4, accv[:, 4:8, 0:128], r2, ALU.mult, R=[Baccs, Bsm], W=[Bsq])
                self.TT("dve", of4, of4, sq4, ALU.add, R=[Bsq], WA=[Bof])
                self.TT("dve", sq4, of4, of4, ALU.mult, R=[Bof], W=[Bsq])
                self.P.op("dve", lambda e: e.tensor_reduce(out=sm[:, 16:20], in_=sq4, axis=mybir.AxisListType.X, op=ALU.add), R=[Bsq], WA=[Bsm])

            def st_norm_b(hl, qt, dp):
                Bsm = self.B("a_sm")
                Brs = self.B("a_rs")
                self.emit_rstd(sm[:, 20:24], sm[:, 16:20], 1.0 / 128, R=[Bsm], W=[Brs])
                rs = sm[:, 20:24].unsqueeze(2).to_broadcast([128, 4, 128])
                self.TT("dve", onb4[dp], of4, rs, ALU.mult, R=[Bof, Brs], W=[self.B(f"a_on{dp}")])

            def st_norm_pe(hl, qt, dp):
                pt2 = self.pbank(7, BF16)[:, 512:1024].rearrange("p (q t) -> p q t", t=128)
                Bp2 = self.B("bank7b")
                for qb in range(4):
                    self.TR(pt2[:, qb, :], onb4[dp][:, qb, :], R=[self.B(f"a_on{dp}")], W=[Bp2] if qb == 0 else (), WA=() if qb == 0 else [Bp2])
                dst = onT[:, hl, qt * 512:(qt + 1) * 512].rearrange("p (q t) -> p q t", t=128)
                self.TS("dve", dst, pt2, sgcol, None, ALU.mult, None, R=[Bp2, Bl],
                        W=[self.B(f"a_onT{hl}_{4 * qt + q}") for q in range(4)])

            pending = []
            st_S(0)
            for i in range(nst):
                if i + 1 < nst:
                    st_S(i + 1)
                st_exp(i)
                st_PV(i)
                hl, qt, kb = steps[i]
                if kb == 4 * qt + 3:
                    dp = (hl * 4 + qt) % 2
                    st_norm(hl, qt, dp)
                    pending.append((i + 2, st_norm_b, hl, qt, dp))
                    pending.append((i + 4, st_norm_pe, hl, qt, dp))
                    pending.sort(key=lambda x: x[0])
                while pending and pending[0][0] <= i:
                    _, f_, h_, q_, d_ = pending.pop(0)
                    f_(h_, q_, d_)
            for _, f_, h_, q_, d_ in pending:
                f_(h_, q_, d_)
            for t in range(NT):
                p = t % 2
                tsl = slice(t * 128, (t + 1) * 128)
                for dh in range(2):
                    Bk = self.B(f"bank{2 * p + dh}")
                    for hl in range(2):
                        self.MM(self.pbank(2 * p + dh), onT[:, hl, tsl], WB[sl][:, hl, dh * 512:(dh + 1) * 512], hl == 0, hl == 1,
                                R=[self.B(f"a_onT{hl}_{t}"), Bwo], W=[Bk] if hl == 0 else (), WA=() if hl == 0 else [Bk])
                    BXt = self.B(f"X{t}")
                    xs = X[:, t, dh * 512:(dh + 1) * 512]
                    self.TT("dve", xs, xs, self.pbank(2 * p + dh), ALU.add, R=[Bk, BXt], WA=[BXt])

    def emit_ffn(self, li):
        cB = self.B("consts")
        X, HT = self.X, self.HT
        Win = [self.carve([8, 1024], BF16) for _ in range(2)]
        Wout = [self.carve([4, D], BF16) for _ in range(2)]
        gb = [self.carve([516], F32) for _ in range(2)]
        tb = [self.carve([512], F32) for _ in range(2)]
        sl_ = [self.carve([512], F32) for _ in range(2)]
        actT = [self.carve([4, 512], BF16) for _ in range(2)]
        carry = self.carve([4, 2], F32)
        self.emit_norm(self.nfg[:, li * 8:(li + 1) * 8])
        win = self.f_win[self.ffn_ls.index(li)]
        wout = self.f_wout[self.ffn_ls.index(li)]
        groups = [(0, 4), (512, 4), (1024, 4), (1536, 4), (2048, 4), (2560, 2)]
        cw = self.cw[:, li * 66:(li + 1) * 66].rearrange("p (f j) -> p f j", j=3)
        cb = self.cb[:, li * 22:(li + 1) * 22]

        def load_w(gi):
            f0, nch = groups[gi]
            nf = nch * 128
            s_ = gi % 2
            Bw = self.B(f"f_Win{s_}")
            self.DMA("pool", Win[s_][:, :, 0:nf], win[:, f0:f0 + nf].rearrange("(k p) c -> p k c", p=128), f"f_Win{s_}", W=[Bw])
            self.DMA("pool", Win[s_][:, :, 512:512 + nf], win[:, FFN + f0:FFN + f0 + nf].rearrange("(k p) c -> p k c", p=128), f"f_Win{s_}", WA=[Bw])
            self.DMA("pool", Wout[s_][:, 0:nch, :], wout[f0:f0 + nf, :].rearrange("(c p) d -> p c d", p=128), f"f_Wout{s_}", W=[self.B(f"f_Wout{s_}")])

        def emit_wout(gi, T):
            f0, nch = groups[gi]
            s_ = gi % 2
            ap_ = T % 2
            Bact = self.B(f"f_act{ap_}")
            Bwo = self.B(f"f_Wout{s_}")
            for tb_ in range(4):
                t = 4 * T + tb_
                p2 = t % 2
                BXt = self.B(f"X{t}")
                for dh in range(2):
                    bk = 4 + 2 * p2 + dh
                    Bk = self.B(f"bank{bk}")
                    for fc in range(nch):
                        self.MM(self.pbank(bk), actT[ap_][:, fc, tb_ * 128:(tb_ + 1) * 128], Wout[s_][:, fc, dh * 512:(dh + 1) * 512],
                                fc == 0, fc == nch - 1, R=[Bact, Bwo], W=[Bk] if fc == 0 else (), WA=() if fc == 0 else [Bk])
                    xs = X[:, t, dh * 512:(dh + 1) * 512]
                    self.TT("dve", xs, xs, self.pbank(bk), ALU.add, R=[Bk, BXt], WA=[BXt])

        load_w(0)
        it = 0
        pending = None
        for gi, (f0, nch) in enumerate(groups):
            s_ = gi % 2
            Bw = self.B(f"f_Win{s_}")
            Bwo = self.B(f"f_Wout{s_}")
            for T in range(4):
                ap_ = T % 2
                BHs = [self.B(f"HT{4 * T + i}") for i in range(4)]
                Bact = self.B(f"f_act{ap_}")
                for fc in range(nch):
                    if fc == 1:
                        if pending is not None:
                            emit_wout(*pending)
                            pending = None
                        if T == 0 and gi + 1 < len(groups):
                            load_w(gi + 1)
                    par = it % 2
                    it += 1
                    fi = f0 // 128 + fc
                    psG = self.pbank(2 * par)
                    psU = self.pbank(2 * par + 1)
                    BG = self.B(f"bank{2 * par}")
                    BU = self.B(f"bank{2 * par + 1}")
                    for k in range(8):
                        self.MM(psG, Win[s_][:, k, fc * 128:(fc + 1) * 128], HT[:, k, T * 512:(T + 1) * 512], k == 0, k == 7,
                                R=BHs + [Bw], W=[BG] if k == 0 else (), WA=() if k == 0 else [BG])
                    for k in range(8):
                        self.MM(psU, Win[s_][:, k, 512 + fc * 128:512 + (fc + 1) * 128], HT[:, k, T * 512:(T + 1) * 512], k == 0, k == 7,
                                R=BHs + [Bw], W=[BU] if k == 0 else (), WA=() if k == 0 else [BU])
                    Bgb = self.B(f"f_gb{par}")
                    Bc = self.B(f"f_carry{fc}")
                    if T == 0:
                        self.MEMSET("pool", gb[par][:, 0:2], 0.0, W=[Bgb])
                    else:
                        self.CP("pool", gb[par][:, 0:2], carry[:, fc, :], R=[Bc], W=[Bgb])
                    self.CP("act", gb[par][:, 2:514], psG, R=[BG], WA=[Bgb])
                    self.CP("pool", carry[:, fc, :], gb[par][:, 512:514], R=[Bgb], W=[Bc])
                    Btb = self.B(f"f_tb{par}")
                    self.TS("pool", tb[par], gb[par][:, 2:514], cw[:, fi, 2:3], cb[:, fi:fi + 1], ALU.mult, ALU.add, R=[Bgb, cB], W=[Btb])
                    self.STT(tb[par], gb[par][:, 1:513], cw[:, fi, 1:2], tb[par], ALU.mult, ALU.add, R=[Bgb, cB, Btb], WA=[Btb])
                    self.STT(tb[par], gb[par][:, 0:512], cw[:, fi, 0:1], tb[par], ALU.mult, ALU.add, R=[Bgb, cB, Btb], WA=[Btb])
                    Bsl = self.B(f"f_sl{par}")
                    self.ACT(sl_[par], tb[par], AF.Silu, R=[Btb], W=[Bsl])
                    self.TT("dve", actT[ap_][:, fc, :], sl_[par], psU, ALU.mult, R=[Bsl, BU], W=[Bact] if fc == 0 else (), WA=() if fc == 0 else [Bact])
                pending = (gi, T)
        emit_wout(*pending)

    def emit_ret(self, li):
        j = li // 2
        cB = self.B("consts")
        X, HT = self.X, self.HT
        WA = self.carve([8, 1536], BF16)
        WB = [self.carve([4, D], BF16) for _ in range(2)]
        St = self.carve([2, 512], F32)
        Sbf = [self.carve([2, 512], BF16) for _ in range(2)]
        rt = [self.carve([2, 128], F32) for _ in range(4)]
        qkr = [self.carve([2, 2, 128], BF16) for _ in range(2)]
        vbf = [self.carve([512], BF16) for _ in range(2)]
        sg = [self.carve([512], F32) for _ in range(2)]
        kd = [self.carve([256], BF16) for _ in range(2)]
        qkT = [self.carve([4, 128], BF16) for _ in range(2)]
        innT = [self.carve([128], BF16) for _ in range(2)]
        gated = [self.carve([512], BF16) for _ in range(2)]
        goT = [self.carve([4, 128], BF16) for _ in range(2)]
        junk = self.carve([512], BF16)
        sm = self.carve([16], F32)
        self.emit_norm(self.nmg[:, li * 8:(li + 1) * 8])
        wq = self.r_wqkvg[self.ret_js.index(j)]
        wo = self.r_wo[self.ret_js.index(j)]
        rcos = self.rcs[:, 0]
        rsin = self.rcs[:, 1]
        Brcs = self.B("rcs")
        gam = [1.0 - 2.0 ** (-5.0 - h) for h in range(4)]

        def load_w(h):
            blocks = [(h * 256, 256, 0), (1024 + h * 256, 256, 256), (2048 + h * 512, 512, 512), (4096 + h * 512, 512, 1024)]
            names = ["r_Wqk", "r_Wqk", "r_Wv", "r_Wg"]
            first = {"r_Wqk": True, "r_Wv": True, "r_Wg": True}
            for (c0, n, d0), nm in zip(blocks, names):
                src = wq[:, c0:c0 + n].rearrange("(k p) c -> p k c", p=128)
                Bw = self.B(nm)
                self.DMA("pool", WA[:, :, d0:d0 + n], src, nm, W=[Bw] if first[nm] else (), WA=() if first[nm] else [Bw])
                first[nm] = False
            s_ = h % 2
            self.DMA("pool", WB[s_], wo[h * 512:(h + 1) * 512, :].rearrange("(c p) d -> p c d", p=128), f"r_WB{s_}", W=[self.B(f"r_WB{s_}")])

        load_w(0)
        for h in range(4):
            s_ = h % 2
            Bwo = self.B(f"r_WB{s_}")
            cd = gam[h] ** 128
            qd = self.rcol[:, h:h + 1]
            qd2 = self.rcol[:, 4 + h:5 + h]
            kdc = self.rcol[:, 8 + h:9 + h]
            maskT = self.rmask[:, h, :]

            def stageA(n):
                p = n % 2
                tsl = slice(n * 128, (n + 1) * 128)
                BHt = self.B(f"HT{n}")
                names = ["r_Wqk", "r_Wv", "r_Wg"]
                for b in range(3):
                    Bk = self.B(f"bank{b}")
                    Bw = self.B(names[b])
                    for k in range(8):
                        self.MM(self.pbank(b), HT[:, k, tsl], WA[:, k, b * 512:(b + 1) * 512], k == 0, k == 7, R=[BHt, Bw],
                                W=[Bk] if k == 0 else (), WA=() if k == 0 else [Bk])
                B0, B1, B2 = self.B("bank0"), self.B("bank1"), self.B("bank2")
                v4 = self.pbank(0).rearrange("p (a i e) -> p a i e", i=128, e=2)
                E = v4[:, :, :, 0]
                O = v4[:, :, :, 1]
                cb_ = rcos[:, n, :].unsqueeze(1).to_broadcast([128, 2, 128])
                sb_ = rsin[:, n, :].unsqueeze(1).to_broadcast([128, 2, 128])
                Brt = self.B("r_rt")
                self.TT("dve", rt[0], E, cb_, ALU.mult, R=[B0, Brcs], W=[Brt])
                self.TT("dve", rt[1], O, sb_, ALU.mult, R=[B0, Brcs], WA=[Brt])
                self.TT("dve", rt[2], O, cb_, ALU.mult, R=[B0, Brcs], WA=[Brt])
                self.TT("dve", rt[3], E, sb_, ALU.mult, R=[B0, Brcs], WA=[Brt])
                Bqr = self.B(f"r_qkr{p}")
                self.TT("dve", qkr[p][:, :, 0, :], rt[0], rt[1], ALU.subtract, R=[Brt], W=[Bqr])
                self.TT("dve", qkr[p][:, :, 1, :], rt[2], rt[3], ALU.add, R=[Brt], WA=[Bqr])
                self.CP("act", vbf[p], self.pbank(1), R=[B1], W=[self.B(f"r_v{p}")])
                self.ACT(sg[p], self.pbank(2), AF.Silu, R=[B2], W=[self.B(f"r_sg{p}")])
                kflat = qkr[p][:, 1].rearrange("p a b -> p (a b)")
                self.ACT(kd[p], kflat, AF.Copy, R=[Bqr, cB], W=[self.B(f"r_kd{p}")], scale=kdc)

            def stageA2(n):
                p = n % 2
                Bqr = self.B(f"r_qkr{p}")
                ptb = self.pbank(3, BF16)[:, 0:512].rearrange("p (i t) -> p i t", t=128)
                Bpt = self.B("bank3a")
                qflat = qkr[p].rearrange("p a b c -> p (a b c)")
                for i in range(4):
                    self.TR(ptb[:, i, :], qflat[:, i * 128:(i + 1) * 128], R=[Bqr], W=[Bpt] if i == 0 else (), WA=() if i == 0 else [Bpt])
                self.CP("act", qkT[p], ptb, R=[Bpt], W=[self.B(f"r_qkT{p}")])

            def stageB(n):
                p = n % 2
                BqkT = self.B(f"r_qkT{p}")
                Bv = self.B(f"r_v{p}")
                pin = self.pbank(3)[:, 256:384]
                Bin = self.B("bank3b")
                for dc in range(2):
                    self.MM(pin, qkT[p][:, 2 + dc, :], qkT[p][:, dc, :], dc == 0, dc == 1, R=[BqkT], W=[Bin] if dc == 0 else (), WA=() if dc == 0 else [Bin])
                Bit = self.B(f"r_innT{p}")
                self.TT("dve", innT[p], pin, maskT, ALU.mult, R=[Bin, cB], W=[Bit])

            def stageB1b(n):
                p = n % 2
                BqkT = self.B(f"r_qkT{p}")
                Bv = self.B(f"r_v{p}")
                Bit = self.B(f"r_innT{p}")
                BO = self.B("bank4")
                pO = self.pbank(4)
                sp_ = (n - 1) % 2
                self.MM(pO, innT[p], vbf[p], True, n == 0, R=[Bit, Bv], W=[BO])
                if n > 0:
                    Bs = self.B(f"r_Sbf{sp_}")
                    for dc in range(2):
                        self.MM(pO, qkT[p][:, dc, :], Sbf[sp_][:, dc, :], False, dc == 1, R=[BqkT, Bs], WA=[BO])
                Bkd = self.B(f"r_kd{p}")
                BSt = self.B("r_St")
                if n < NT - 1:
                    for dc in range(2):
                        Bd = self.B(f"bank{5 + dc}")
                        self.MM(self.pbank(5 + dc), kd[p][:, dc * 128:(dc + 1) * 128], vbf[p], True, True, R=[Bkd, Bv], W=[Bd])
                        if n == 0:
                            self.CP("dve", St[:, dc, :], self.pbank(5 + dc), R=[Bd], W=[BSt] if dc == 0 else (), WA=() if dc == 0 else [BSt])
                        else:
                            self.STT(St[:, dc, :], St[:, dc, :], cd, self.pbank(5 + dc), ALU.mult, ALU.add, R=[Bd, BSt], WA=[BSt])
                    self.CP("pool", Sbf[p], St, R=[BSt], W=[self.B(f"r_Sbf{p}")])
                Bsm = self.B(f"r_sm{p}")
                o_ = p * 8
                self.ACT(junk, pO, AF.Square, R=[BO], W=[Bsm], WA=[self.B("r_junk")], accum=sm[:, o_:o_ + 1])
                self.TT("dve", sm[:, o_ + 1:o_ + 2], sm[:, o_:o_ + 1], qd2, ALU.mult, R=[Bsm, cB], WA=[Bsm])
                self.emit_rstd(sm[:, o_ + 2:o_ + 3], sm[:, o_ + 1:o_ + 2], 1.0, R=[Bsm], W=[self.B(f"r_rs{p}")])
                self.TT("dve", sm[:, o_ + 3:o_ + 4], sm[:, o_ + 2:o_ + 3], qd, ALU.mult, R=[self.B(f"r_rs{p}"), cB], W=[self.B(f"r_rq{p}")])
                Bg = self.B(f"r_gated{p}")
                self.STT(gated[p], pO, sm[:, o_ + 3:o_ + 4], sg[p], ALU.mult, ALU.mult, R=[BO, self.B(f"r_rq{p}"), self.B(f"r_sg{p}")], W=[Bg])

            def stageB2(n):
                p = n % 2
                Bg = self.B(f"r_gated{p}")
                ptb = self.pbank(7, BF16)[:, 0:512].rearrange("p (i t) -> p i t", t=128)
                Bpt = self.B("bank7")
                for i in range(4):
                    self.TR(ptb[:, i, :], gated[p][:, i * 128:(i + 1) * 128], R=[Bg], W=[Bpt] if i == 0 else (), WA=() if i == 0 else [Bpt])
                BgT = self.B(f"r_goT{p}")
                self.CP("act", goT[p], ptb, R=[Bpt], W=[BgT])

            def stageB2b(n):
                p = n % 2
                BgT = self.B(f"r_goT{p}")
                BXt = self.B(f"X{n}")
                tslx = n
                for dh in range(2):
                    Bk = self.B("bank6") if dh == 0 else self.B("bank5")
                    bk = 6 if dh == 0 else 5
                    for ec in range(4):
                        self.MM(self.pbank(bk), goT[p][:, ec, :], WB[s_][:, ec, dh * 512:(dh + 1) * 512], ec == 0, ec == 3, R=[BgT, Bwo],
                                W=[Bk] if ec == 0 else (), WA=() if ec == 0 else [Bk])
                    xs = X[:, tslx, dh * 512:(dh + 1) * 512]
                    self.TT("dve", xs, xs, self.pbank(bk), ALU.add, R=[Bk, BXt], WA=[BXt])

            stageA(0)
            stageA2(0)
            for n in range(NT):
                if n + 1 < NT:
                    stageA(n + 1)
                    if n + 1 == NT - 1 and h + 1 < 4:
                        load_w(h + 1)
                stageB(n)
                if n >= 1:
                    stageB2(n - 1)
                stageB1b(n)
                if n + 1 < NT:
                    stageA2(n + 1)
                if n >= 1:
                    stageB2b(n - 1)
            stageB2(NT - 1)
            stageB2b(NT - 1)

    def emit_out(self, s):
        junk = self.carve([D], BF16)
        ob = [self.carve([D], F32) for _ in range(2)]
        ss = self.carve([4], F32)
        gfull = self.carve([D], F32)
        Bg = self.B("o_g")
        self.DMA("sp", gfull, self.fng_d, "o_g", W=[Bg])
        for t in range(NT):
            p = t % 2
            BXt = self.B(f"X{t}")
            Bob = self.B(f"o_b{p}")
            if self.final_norm:
                Bss = self.B(f"o_ss{p}")
                self.ACT(junk, self.X[:, t, :], AF.Square, R=[BXt], W=[Bss], WA=[self.B("o_junk")], accum=ss[:, p:p + 1])
                self.emit_rstd(ss[:, 2 + p:3 + p], ss[:, p:p + 1], 1.0 / D, R=[Bss], W=[self.B(f"o_rs{p}")])
                self.STT(ob[p], self.X[:, t, :], ss[:, 2 + p:3 + p], gfull, ALU.mult, ALU.mult, R=[BXt, self.B(f"o_rs{p}"), Bg], W=[Bob])
            else:
                self.CP("dve", ob[p], self.X[:, t, :], R=[BXt], W=[Bob])
            self.DMA("sp", self.out_d[s, t * 128:(t + 1) * 128, :], ob[p], "out", R=[Bob], WA=[self.B("out")])


def _consts():
    ident = np.eye(128, dtype=np.float32)
    cm = np.where(np.arange(128)[:, None] <= np.arange(128)[None, :], 0.0, -30000.0).astype(np.float32)
    cbf = np.concatenate([ident, cm], axis=1).astype(ml_dtypes.bfloat16)
    afreq = (500000.0 ** (-np.arange(0, 16, 2, dtype=np.float32) / np.float32(16))).astype(np.float32)
    rfreq = (1.0 / (10000.0 ** np.linspace(0.0, 1.0, 128, dtype=np.float32))).astype(np.float32)
    cf = np.zeros((128, 664), np.float32)
    cf[:, 0:8] = afreq[None, :]
    cf[:, 8:136] = rfreq[None, :]
    idx = np.arange(128, dtype=np.float64)
    for h in range(4):
        gam = 1.0 - 2.0 ** (-5.0 - h)
        m = np.where(idx[None, :] >= idx[:, None], (gam ** (-(idx[:, None] + 1.0))) / 16.0, 0.0)
        cf[:, 136 + h * 128:136 + (h + 1) * 128] = m
        qd = gam ** (idx + 1.0)
        cf[:, 648 + h] = qd
        cf[:, 652 + h] = qd * qd / 512.0
        cf[:, 656 + h] = gam ** (127.0 - idx) / 16.0
    return cbf, cf


_NC_CACHE = {}


def _get_nc(layers, nseq, final_norm):
    key = (tuple(layers), nseq, final_norm)
    if key not in _NC_CACHE:
        _NC_CACHE[key] = Builder(list(layers), nseq, final_norm).build()
    return _NC_CACHE[key]


PLAN = [[0, 1, 2, 3]]


def _norm_layers(layers):
    return tuple(e if isinstance(e, tuple) else (e, True, True) for e in layers)


def _run(inputs, layers=None, final_norm=True, plan=None):
    f = lambda a: np.ascontiguousarray(np.asarray(a, dtype=np.float32))
    x = f(inputs["x"])
    pos = np.ascontiguousarray(np.asarray(inputs["positions"], dtype=np.int32))
    cbf, cf = _consts()
    fm = lambda g: np.ascontiguousarray(f(g).reshape(DEPTH, 8, 128).transpose(2, 0, 1).reshape(128, DEPTH * 8))
    lam = np.concatenate([f(inputs["attn_lambda_q1"]), f(inputs["attn_lambda_k1"]),
                          f(inputs["attn_lambda_q2"]), f(inputs["attn_lambda_k2"])], axis=1)
    lam = np.ascontiguousarray(np.broadcast_to(lam.reshape(1, 512), (128, 512)))
    subln = np.ascontiguousarray(f(inputs["attn_subln_g"]).T)
    cw = f(inputs["ffn_conv_w"]).reshape(DEPTH, 3, NFC, 128).transpose(3, 0, 2, 1)
    cw = np.ascontiguousarray(cw.reshape(128, DEPTH * NFC * 3))
    cb = np.ascontiguousarray(f(inputs["ffn_conv_b"]).reshape(DEPTH, NFC, 128).transpose(2, 0, 1).reshape(128, DEPTH * NFC))
    small = dict(
        nmg=fm(inputs["norm_mix_g"]), nfg=fm(inputs["norm_ffn_g"]),
        fng=np.ascontiguousarray(np.broadcast_to(f(inputs["final_norm_g"]).reshape(1, D), (128, D))),
        lam_in=lam, subln=subln, f_cw=cw, f_cb=cb, c_bf=cbf, c_f32=cf,
    )
    if plan is None:
        plan = [list(layers)] if layers is not None else PLAN
    pcs = [np.ascontiguousarray(pos[c * NSEQ:(c + 1) * NSEQ].reshape(NSEQ, NT, 128).transpose(0, 2, 1)) for c in range(N_CORES)]
    cur = x
    for li_, lay in enumerate(plan):
        lay = _norm_layers(lay)
        fn = final_norm and (li_ == len(plan) - 1)
        key = (lay, NSEQ, fn)
        if key not in _NC_CACHE:
            b = Builder(list(lay), NSEQ, fn)
            _NC_CACHE[key] = (b.build(), b)
        nc, b = _NC_CACHE[key]
        shared = dict(small)
        if b.attn_js:
            shared["a_wqkv"] = f(inputs["attn_w_qkv"])[b.attn_js]
            shared["a_wo"] = f(inputs["attn_w_o"])[b.attn_js]
        if b.ret_js:
            shared["r_wqkvg"] = f(inputs["ret_w_qkvg"])[b.ret_js]
            shared["r_wo"] = f(inputs["ret_w_o"])[b.ret_js]
        if b.ffn_ls:
            shared["f_win"] = f(inputs["ffn_w_in"])[b.ffn_ls]
            shared["f_wout"] = f(inputs["ffn_w_out"])[b.ffn_ls]
        in_maps = []
        for c in range(N_CORES):
            m = dict(shared)
            m["x"] = np.ascontiguousarray(cur[c * NSEQ:(c + 1) * NSEQ])
            m["pos"] = pcs[c]
            in_maps.append(m)
        res = run_bass_kernel_spmd(nc, in_maps, core_ids=list(range(N_CORES)))
        cur = np.concatenate([r["out"] for r in res.results], axis=0)
    return cur


def kernel(**inputs):
    return _run(inputs)
```
